# Optimizing a Trainium2 kernel written in Bass

```python
import jax, jax.numpy as jnp
from jax import lax
import numpy as np

D_MODEL = 1024
BATCH = 4
SEQ = 4096
DEPTH = 4

CHUNK = 64
Q_BLOCK = 128
HEAD_DIM = 64
N_DIFF_HEADS = D_MODEL // 4 // HEAD_DIM
N_FOX_HEADS = D_MODEL // 2 // HEAD_DIM
DIFF_WIDTH = N_DIFF_HEADS * 2 * HEAD_DIM
FOX_WIDTH = N_FOX_HEADS * HEAD_DIM
MIX_WIDTH = DIFF_WIDTH + FOX_WIDTH
IN_COLS = 3 * DIFF_WIDTH + 3 * FOX_WIDTH + N_FOX_HEADS
ROT_DIM = HEAD_DIM // 4
ROPE_THETA = 500000.0
N_GROUPS = 4
EXPERTS_PER_GROUP = 8
N_EXPERTS = N_GROUPS * EXPERTS_PER_GROUP
TOP_K = 2
D_EXPERT = D_MODEL // 2
EXPERT_BLOCK = 128
EPS = 1e-6

kernel_name = "hybrid_diff_fox_hmoe_adaln"


def rms_norm(x, g):
    xf = x.astype(jnp.float32)
    y = xf * lax.rsqrt(jnp.mean(xf * xf, axis=-1, keepdims=True) + EPS)
    return (y * g.astype(jnp.float32)).astype(x.dtype)


def rope_tables(positions):
    inv = ROPE_THETA ** (-jnp.arange(0, ROT_DIM, 2, dtype=jnp.float32) / ROT_DIM)
    ang = positions.astype(jnp.float32)[..., None] * inv
    return jnp.cos(ang), jnp.sin(ang)


def partial_rope(x, cos, sin):
    half = ROT_DIM // 2
    cos = cos.astype(x.dtype)
    sin = sin.astype(x.dtype)
    x1 = x[..., :half]
    x2 = x[..., half:ROT_DIM]
    return jnp.concatenate([x1 * cos - x2 * sin, x2 * cos + x1 * sin, x[..., ROT_DIM:]], axis=-1)


def diff_attention(q, k, v, lam):
    B, S, H = q.shape[:3]
    nb = S // Q_BLOCK
    scale = HEAD_DIM ** -0.5
    kf = k.astype(jnp.float32)
    vf = v.astype(jnp.float32)
    qb = q.reshape(B, nb, Q_BLOCK, H, 2, HEAD_DIM).transpose(1, 0, 2, 3, 4, 5)
    key_chunk = jnp.arange(S) // CHUNK

    def block(args):
        qi, bi = args
        s = jnp.einsum('bqhcd,bkhcd->bhcqk', qi.astype(jnp.float32), kf) * scale
        q_chunk = (bi * Q_BLOCK + jnp.arange(Q_BLOCK)) // CHUNK
        mask = key_chunk[None, :] <= q_chunk[:, None]
        p = jax.nn.softmax(jnp.where(mask, s, -jnp.inf), axis=-1)
        a = p[:, :, 0] - lam * p[:, :, 1]
        return jnp.einsum('bhqk,bkhe->bqhe', a, vf)

    o = lax.map(block, (qb, jnp.arange(nb)))
    return o.transpose(1, 0, 2, 3, 4).reshape(B, S, H, 2 * HEAD_DIM).astype(v.dtype)


def forgetting_attention(q, k, v, log_f):
    B, S, H = q.shape[:3]
    nb = S // Q_BLOCK
    scale = HEAD_DIM ** -0.5
    cf = jnp.cumsum(log_f, axis=1)
    cf_k = cf.transpose(0, 2, 1)
    kf = k.astype(jnp.float32)
    vf = v.astype(jnp.float32)
    qb = q.reshape(B, nb, Q_BLOCK, H, HEAD_DIM).transpose(1, 0, 2, 3, 4)
    cqb = cf.reshape(B, nb, Q_BLOCK, H).transpose(1, 0, 3, 2)
    key_idx = jnp.arange(S)

    def block(args):
        qi, cq, bi = args
        s = jnp.einsum('bqhd,bkhd->bhqk', qi.astype(jnp.float32), kf) * scale
        s = s + (cq[..., :, None] - cf_k[:, :, None, :])
        t = bi * Q_BLOCK + jnp.arange(Q_BLOCK)
        mask = key_idx[None, :] <= t[:, None]
        p = jax.nn.softmax(jnp.where(mask, s, -jnp.inf), axis=-1)
        return jnp.einsum('bhqk,bkhd->bqhd', p, vf)

    o = lax.map(block, (qb, cqb, jnp.arange(nb)))
    return o.transpose(1, 0, 2, 3, 4).reshape(B, S, H, HEAD_DIM).astype(v.dtype)


def token_mixer(h, cos, sin, layer, w_in_l, b_forget_l, lq1, lk1, lq2, lk2, g_subln_l, g_fox_l, w_out_l):
    B, S, _ = h.shape
    proj = h @ w_in_l
    cuts = [DIFF_WIDTH, 2 * DIFF_WIDTH, 3 * DIFF_WIDTH,
            3 * DIFF_WIDTH + FOX_WIDTH, 3 * DIFF_WIDTH + 2 * FOX_WIDTH, 3 * DIFF_WIDTH + 3 * FOX_WIDTH]
    dq, dk, dv, fq, fk, fv, ff = jnp.split(proj, cuts, axis=-1)

    rc, rs = cos[:, :, None, None, :], sin[:, :, None, None, :]
    dq = partial_rope(dq.reshape(B, S, N_DIFF_HEADS, 2, HEAD_DIM), rc, rs)
    dk = partial_rope(dk.reshape(B, S, N_DIFF_HEADS, 2, HEAD_DIM), rc, rs)
    dv = dv.reshape(B, S, N_DIFF_HEADS, 2 * HEAD_DIM)
    lambda_init = 0.8 - 0.6 * float(np.exp(-0.3 * layer))
    lam = (jnp.exp(jnp.sum(lq1.astype(jnp.float32) * lk1.astype(jnp.float32)))
           - jnp.exp(jnp.sum(lq2.astype(jnp.float32) * lk2.astype(jnp.float32))) + lambda_init)
    o_diff = diff_attention(dq, dk, dv, lam)
    o_diff = (rms_norm(o_diff, g_subln_l) * (1.0 - lambda_init)).reshape(B, S, DIFF_WIDTH)

    fq = fq.reshape(B, S, N_FOX_HEADS, HEAD_DIM)
    fk = fk.reshape(B, S, N_FOX_HEADS, HEAD_DIM)
    fv = fv.reshape(B, S, N_FOX_HEADS, HEAD_DIM)
    log_f = jax.nn.log_sigmoid(ff.astype(jnp.float32) + b_forget_l.astype(jnp.float32))
    o_fox = forgetting_attention(fq, fk, fv, log_f)
    o_fox = rms_norm(o_fox, g_fox_l).reshape(B, S, FOX_WIDTH)

    return jnp.concatenate([o_diff, o_fox], axis=-1) @ w_out_l


def hierarchical_moe(h, wg, bg, we, be, w_gate, w_up, w_down):
    B, S, D = h.shape
    N = B * S
    hf = h.reshape(N, D)
    hr = hf.astype(jnp.float32)
    p_group = jax.nn.softmax(hr @ wg.astype(jnp.float32) + bg.astype(jnp.float32), axis=-1)
    g_sel = jnp.argmax(p_group, axis=-1)
    p_g = jnp.take_along_axis(p_group, g_sel[:, None], axis=-1)
    logits_e = (hr @ we.astype(jnp.float32) + be.astype(jnp.float32)).reshape(N, N_GROUPS, EXPERTS_PER_GROUP)
    logits_sel = jnp.take_along_axis(logits_e, g_sel[:, None, None], axis=1)[:, 0]
    top_l, top_i = lax.top_k(logits_sel, TOP_K)
    weights = p_g * jax.nn.softmax(top_l, axis=-1)
    expert_id = g_sel[:, None] * EXPERTS_PER_GROUP + top_i

    A = N * TOP_K
    e_flat = expert_id.reshape(A).astype(jnp.int32)
    tok_flat = jnp.repeat(jnp.arange(N, dtype=jnp.int32), TOP_K)
    w_flat = weights.reshape(A)
    order = jnp.argsort(e_flat)
    e_sorted = e_flat[order]
    counts = jax.ops.segment_sum(jnp.ones((A,), jnp.int32), e_flat, num_segments=N_EXPERTS)
    padded = ((counts + EXPERT_BLOCK - 1) // EXPERT_BLOCK) * EXPERT_BLOCK
    start = jnp.cumsum(counts) - counts
    pend = jnp.cumsum(padded)
    pstart = pend - padded
    dest = pstart[e_sorted] + (jnp.arange(A, dtype=jnp.int32) - start[e_sorted])
    P = A + N_EXPERTS * EXPERT_BLOCK
    n_blocks = P // EXPERT_BLOCK
    slot_tok = jnp.zeros((P,), jnp.int32).at[dest].set(tok_flat[order])
    slot_w = jnp.zeros((P,), jnp.float32).at[dest].set(w_flat[order])
    block_expert = jnp.minimum(
        jnp.searchsorted(pend, jnp.arange(n_blocks, dtype=jnp.int32) * EXPERT_BLOCK, side='right'),
        N_EXPERTS - 1)
    xs = hf[slot_tok].reshape(n_blocks, EXPERT_BLOCK, D)

    def run_block(args):
        xb, e = args
        return (jax.nn.silu(xb @ w_gate[e]) * (xb @ w_up[e])) @ w_down[e]

    ys = lax.map(run_block, (xs, block_expert)).reshape(P, D)
    out = jnp.zeros((N, D), ys.dtype).at[slot_tok].add(ys * slot_w[:, None].astype(ys.dtype))
    return out.reshape(B, S, D)


def setup_inputs(seed: int = 0) -> dict:
    key = jax.random.key(seed)
    ks = jax.random.split(key, 26)
    D = D_MODEL

    def nrm(k, shape, fan_in, mult=1.0):
        return jax.random.normal(k, shape, jnp.float32) * (mult * fan_in ** -0.5)

    def gain(k, shape):
        return 1.0 + 0.02 * jax.random.normal(k, shape, jnp.float32)

    offsets = jax.random.randint(ks[2], (BATCH, 1), 0, 4096)
    positions = (offsets + jnp.arange(SEQ)[None, :]).astype(jnp.int32)
    return {
        "x": jax.random.normal(ks[0], (BATCH, SEQ, D), jnp.float32),
        "c": jax.random.normal(ks[1], (BATCH, D), jnp.float32),
        "positions": positions,
        "w_ada": nrm(ks[3], (DEPTH, D, 6 * D), D, 0.5),
        "b_ada": 0.02 * jax.random.normal(ks[4], (DEPTH, 6 * D), jnp.float32),
        "g_mix": gain(ks[5], (DEPTH, D)),
        "w_in": nrm(ks[6], (DEPTH, D, IN_COLS), D),
        "b_forget": 1.0 + 4.0 * jax.random.uniform(ks[7], (DEPTH, N_FOX_HEADS), jnp.float32),
        "lambda_q1": 0.1 * jax.random.normal(ks[8], (DEPTH, HEAD_DIM), jnp.float32),
        "lambda_k1": 0.1 * jax.random.normal(ks[9], (DEPTH, HEAD_DIM), jnp.float32),
        "lambda_q2": 0.1 * jax.random.normal(ks[10], (DEPTH, HEAD_DIM), jnp.float32),
        "lambda_k2": 0.1 * jax.random.normal(ks[11], (DEPTH, HEAD_DIM), jnp.float32),
        "g_subln": gain(ks[12], (DEPTH, 2 * HEAD_DIM)),
        "g_fox_out": gain(ks[13], (DEPTH, HEAD_DIM)),
        "w_out": nrm(ks[14], (DEPTH, MIX_WIDTH, D), MIX_WIDTH),
        "g_ffn": gain(ks[15], (DEPTH, D)),
        "w_router_group": nrm(ks[16], (DEPTH, D, N_GROUPS), D),
        "b_router_group": 0.01 * jax.random.normal(ks[17], (DEPTH, N_GROUPS), jnp.float32),
        "w_router_expert": nrm(ks[18], (DEPTH, D, N_EXPERTS), D),
        "b_router_expert": 0.01 * jax.random.normal(ks[19], (DEPTH, N_EXPERTS), jnp.float32),
        "w_expert_gate": nrm(ks[20], (DEPTH, N_EXPERTS, D, D_EXPERT), D),
        "w_expert_up": nrm(ks[21], (DEPTH, N_EXPERTS, D, D_EXPERT), D),
        "w_expert_down": nrm(ks[22], (DEPTH, N_EXPERTS, D_EXPERT, D), D_EXPERT),
        "g_final": gain(ks[23], (D,)),
    }


def reference(x, c, positions, w_ada, b_ada, g_mix, w_in, b_forget, lambda_q1, lambda_k1,
              lambda_q2, lambda_k2, g_subln, g_fox_out, w_out, g_ffn, w_router_group,
              b_router_group, w_router_expert, b_router_expert, w_expert_gate, w_expert_up,
              w_expert_down, g_final):
    cond = jax.nn.silu(c)
    cos, sin = rope_tables(positions)
    for l in range(DEPTH):
        mod = (cond @ w_ada[l] + b_ada[l])[:, None, :]
        sh1, sc1, gt1, sh2, sc2, gt2 = jnp.split(mod, 6, axis=-1)
        h = rms_norm(x, g_mix[l]) * (1.0 + sc1) + sh1
        mix = token_mixer(h, cos, sin, l, w_in[l], b_forget[l], lambda_q1[l], lambda_k1[l],
                          lambda_q2[l], lambda_k2[l], g_subln[l], g_fox_out[l], w_out[l])
        x = x + gt1 * mix
        h = rms_norm(x, g_ffn[l]) * (1.0 + sc2) + sh2
        moe = hierarchical_moe(h, w_router_group[l], b_router_group[l], w_router_expert[l],
                               b_router_expert[l], w_expert_gate[l], w_expert_up[l], w_expert_down[l])
        x = x + gt2 * moe
    return rms_norm(x, g_final)
```

```python
import math, contextlib
import numpy as np
import concourse.bass as bass
import concourse.mybir as mybir
from concourse.bass_utils import run_bass_kernel_spmd

F32 = mybir.dt.float32
BF16 = mybir.dt.bfloat16
I32 = mybir.dt.int32
AF = mybir.ActivationFunctionType
ALU = mybir.AluOpType
AX = mybir.AxisListType

MAXOPS = None
NLIN = 4
USE_COND = True
COND_THR = None
ENGS = ("pe", "act", "dve", "pool", "sp")


class Res:
    __slots__ = ("name", "w", "r", "excl")

    def __init__(self, name, w=None, excl=False):
        self.name = name
        self.w = w
        self.r = []
        self.excl = excl


class Prog:
    def __init__(self, nc):
        self.nc = nc
        self.ops = []
        self.es = contextlib.ExitStack()
        self.engsem = {e: self.es.enter_context(nc.semaphore("s_" + e)) for e in ENGS}
        self.keysem = {}
        self.semByName = {}
        self.cnt = {}
        self.known = {e: {} for e in ENGS}
        self.lastkey = {}
        self.allres = []
        self.barrier_op = None
        self.nflush = 0
        self.ninstr = 0
        self.cur_cond = None
        self.ncond = 0

    def res(self, name, excl=False):
        r = Res(name, self.barrier_op, excl)
        self.allres.append(r)
        return r

    def op(self, eng, fn, reads=(), writes=(), dmakey=None, ndma=1):
        self.total_ops = getattr(self, "total_ops", 0) + 1
        if MAXOPS is not None and self.total_ops > MAXOPS:
            return None
        o = dict(eng=eng, fn=fn, reads=list(reads), writes=list(writes), dmakey=dmakey, ndma=ndma,
                 deps=[], sig=False, done=False, cond=self.cur_cond)
        self.ops.append(o)
        return o

    def cond_begin(self, flag_ap):
        self.ncond += 1
        self.cur_cond = (self.ncond, flag_ap)

    def cond_end(self):
        self.cur_cond = None

    def chain(self, eng, fns, reads=(), writes=()):
        for f in fns:
            self.op(eng, f, reads=reads, writes=writes)

    def flush(self, scratch):
        nc = self.nc
        global MAXOPS
        _mo = MAXOPS
        MAXOPS = None
        bar = self.op("pool", lambda e: e.memset(scratch, 0.0), writes=[self.scratch_res])
        MAXOPS = _mo
        bar["isbar"] = True
        ops = self.ops
        for o in ops:
            deps = {}
            if o.get("isbar"):
                for r in self.allres:
                    if r.w is not None:
                        deps[id(r.w)] = r.w
                    for q in r.r:
                        deps[id(q)] = q
            for r in o["reads"]:
                if r.w is not None:
                    deps[id(r.w)] = r.w
                if r.excl:
                    for q in r.r:
                        if q["eng"] != o["eng"]:
                            deps[id(q)] = q
            for w in o["writes"]:
                if w.w is not None:
                    deps[id(w.w)] = w.w
                for q in w.r:
                    deps[id(q)] = q
            k = o["dmakey"]
            if k is not None:
                if k in self.lastkey:
                    p = self.lastkey[k]
                    deps[id(p)] = p
                self.lastkey[k] = o
            deps.pop(id(o), None)
            o["deps"] = list(deps.values())
            for r in o["reads"]:
                r.r.append(o)
            for w in o["writes"]:
                w.w = o
                w.r = []
        for o in ops:
            for p in o["deps"]:
                if p["eng"] == "pe" and o["eng"] == "pe" and p["dmakey"] is None:
                    continue
                p["sig"] = True
        bar["sig"] = True
        for o in ops:
            k = o["dmakey"]
            if k is not None:
                if k not in self.keysem:
                    pref = "q" if o["eng"] == "pool" else "d"
                    n_ = sum(1 for v in self.keysem.values() if v[0] == pref)
                    name = f"{pref}_{n_}"
                    if name not in self.semByName:
                        self.semByName[name] = self.es.enter_context(nc.semaphore(name))
                    self.keysem[k] = (pref, name)
                else:
                    assert (self.keysem[k][0] == "q") == (o["eng"] == "pool"), k
                o["semname"] = self.keysem[k][1]
                o["sem"] = self.semByName[o["semname"]]
                self.cnt[o["semname"]] = self.cnt.get(o["semname"], 0) + 16 * o["ndma"]
                o["val"] = self.cnt[o["semname"]]
            else:
                o["sem"] = self.engsem[o["eng"]]
                o["semname"] = "s_" + o["eng"]
                if o["sig"]:
                    self.cnt[o["semname"]] = self.cnt.get(o["semname"], 0) + 1
                    o["val"] = self.cnt[o["semname"]]
        per = {e: [] for e in ENGS}
        for o in ops:
            per[o["eng"]].append(o)

        def emit_op(engname, eng, o, known):
            need = {}
            for p in o["deps"]:
                if p["eng"] == "pe" and engname == "pe" and p["dmakey"] is None:
                    continue
                sn = p["semname"]
                if p["val"] > need.get(sn, (None, 0))[1]:
                    need[sn] = (p["sem"], p["val"])
            for sn, (sem, val) in need.items():
                if known.get(sn, 0) >= val:
                    continue
                eng.wait_ge(sem, val)
                known[sn] = val
                self.ninstr += 1
            res = o["fn"](eng)
            if o["dmakey"] is not None:
                lst = res if isinstance(res, (list, tuple)) else [res]
                assert len(lst) == o["ndma"], (len(lst), o["ndma"], o["dmakey"])
                for ins in lst:
                    ins.then_inc(o["sem"], 16)
            elif o["sig"]:
                ins = res[-1] if isinstance(res, (list, tuple)) else res
                ins.then_inc(o["sem"], 1)

        def run(engname, eng):
            known = self.known[engname]
            lst = per[engname]
            i = 0
            while i < len(lst):
                o = lst[i]
                if o["cond"] is None:
                    emit_op(engname, eng, o, known)
                    i += 1
                    continue
                cid = o["cond"]
                j = i
                while j < len(lst) and lst[j]["cond"] is cid:
                    j += 1
                region = lst[i:j]
                incs = {}
                for o2 in region:
                    n_ = 16 * o2["ndma"] if o2["dmakey"] is not None else (1 if o2["sig"] else 0)
                    if n_:
                        sn = o2["semname"]
                        if sn not in incs:
                            incs[sn] = [o2["sem"], o2["val"] - n_, 0]
                        incs[sn][2] += n_
                self._nreg = getattr(self, "_nreg", 0) + 1
                reg = eng.alloc_register(f"cf{self._nreg}")
                eng.reg_load(reg, cid[1])
                saved = dict(known)
                with eng.If_eq(reg, 1):
                    for o2 in region:
                        emit_op(engname, eng, o2, known)
                with eng.Else():
                    for sn, (sem, base, tot) in incs.items():
                        if base > 0:
                            eng.wait_ge(sem, base)
                        eng.sem_inc(sem, tot)
                eng.free_register(reg)
                known.clear()
                known.update(saved)
                i = j

        with nc.Block() as block:
            @block.tensor
            def _(e):
                run("pe", e)

            @block.scalar
            def _(e):
                run("act", e)

            @block.vector
            def _(e):
                run("dve", e)

            @block.gpsimd
            def _(e):
                run("pool", e)

            @block.sync
            def _(e):
                run("sp", e)
        for r in self.allres:
            r.w = bar
            r.r = []
        pass
        self.barrier_op = bar
        self.lastkey = {}
        self.keysem = {}
        self.ops = []
        self.nflush += 1

    def finish(self, scratch):
        self.flush(scratch)
        nc = self.nc
        fin = [(sem, self.cnt[name]) for name, sem in self.semByName.items()]
        bar = self.barrier_op
        with nc.Block() as block:
            @block.sync
            def _(e):
                e.wait_ge(bar["sem"], bar["val"])
                for sem, val in fin:
                    e.wait_ge(sem, val)
        self.es.close()

NT = 32
T = NT * 128
D = 1024
NL = 4
NE = 32
CAP = 1536
SEG = 256
NSLOT = NE * CAP
INC = 3080
EPS = 1e-6
NEGM = -240000.0
TWO_PI = 2.0 * math.pi
MAGIC = 12582912.0
INV_FREQ = [500000.0 ** (-(2 * i) / 16.0) for i in range(8)]


class K:
    def __init__(self, nlayers=NL, dbg=False, stop=None):
        self.nl = nlayers
        self.dbg = dbg
        self.stop = stop
        nc = self.nc = bass.Bass("TRN2", target_bir_lowering=False)
        self.P = Prog(nc)
        self.pes = contextlib.ExitStack()
        self.ins = {}
        self.dr = {}
        self.build()

    def din(self, name, shape, dt=F32):
        self.ins[name] = self.nc.dram_tensor(name, list(shape), dt, kind="ExternalInput").ap()
        return self.ins[name]

    def dscr(self, name, shape, dt):
        kind = "ExternalOutput" if self.dbg else "Internal"
        t = self.nc.dram_tensor(name, list(shape), dt, kind=kind).ap()
        self.dr[name] = t
        return t

    def sb(self, es, name, shape, dt):
        self._uid = getattr(self, "_uid", 0) + 1
        name = f"{name}_u{self._uid}"
        t = es.enter_context(self.nc.sbuf_tensor(name, list(shape), dt))
        return t, self.P.res(name)

    def ps(self, es, name, shape, dt):
        self._uid = getattr(self, "_uid", 0) + 1
        name = f"{name}_u{self._uid}"
        n = 1
        for d_ in shape[1:]:
            n *= d_
        full = 512 if dt == F32 else 1024
        assert n <= full
        if n == full:
            t = es.enter_context(self.nc.psum_tensor(name, list(shape), dt))
        else:
            assert len(shape) == 2
            tb = es.enter_context(self.nc.psum_tensor(name, [shape[0], full], dt))
            t = tb[:, 0:shape[1]]
        return t, self.P.res(name, excl=True)

    def indirect(self, e, **kw):
        self._nreg = getattr(self, "_nreg", 0) + 1
        reg = e.alloc_register(f"bnd{self._nreg}")
        e.reg_mov(reg, NSLOT - 1)
        r = e.indirect_dma_start(bounds_check=reg, oob_is_err=False, **kw)
        e.free_register(reg)
        return r

    def dump(self, name, ap, res, shape, dt):
        if not self.dbg:
            return
        t = self.nc.dram_tensor("dbg_" + name, list(shape), dt, kind="ExternalOutput").ap()
        self.P.op("sp", lambda e: e.dma_start(out=t, in_=ap), reads=[res], dmakey="dbg_" + name)

    def build(self):
        nc, P = self.nc, self.P
        I = self.ins
        self.din("x", [T, D]); self.din("cT", [128, 8]); self.din("pos", [128, NT], I32)
        self.din("w_ada", [NLIN, D, 6 * D]); self.din("b_ada", [NLIN, 6 * D]); self.din("g_mix", [NLIN, D])
        self.din("w_in", [NLIN, D, INC]); self.din("b_forget", [NLIN, 8])
        for n in ("lambda_q1", "lambda_k1", "lambda_q2", "lambda_k2"):
            self.din(n, [NLIN, 64])
        self.din("g_subln", [NLIN, 128]); self.din("g_fox_out", [NLIN, 64]); self.din("w_out", [NLIN, D, D])
        self.din("g_ffn", [NLIN, D]); self.din("w_router_group", [NLIN, D, 4]); self.din("b_router_group", [NLIN, 4])
        self.din("w_router_expert", [NLIN, D, 32]); self.din("b_router_expert", [NLIN, 32])
        self.din("w_expert_gate", [NLIN, NE, D, 512]); self.din("w_expert_up", [NLIN, NE, D, 512])
        self.din("w_expert_down", [NLIN, NE, 512, D]); self.din("g_final", [D])
        self.y = nc.dram_tensor("y", [T, D], F32, kind="ExternalOutput").ap()
        self.Ry = P.res("y")
        self.xres = self.dscr("xres", [T, D], F32); self.Rxres = [P.res("xres0"), P.res("xres1")]
        self.qktd = self.dscr("qktd", [8, 128, T], BF16); self.Rqktd = [P.res("qktd0"), P.res("qktd1")]
        self.ktf = self.dscr("ktf", [4, 128, T], BF16); self.Rktf = [P.res("ktf0"), P.res("ktf1")]
        self.qtf = self.dscr("qtf", [8, 67, T], BF16); self.Rqtf = [P.res("qtf0"), P.res("qtf1")]
        self.vd = self.dscr("vd", [T, 512], BF16); self.Rvd = [P.res("vd0"), P.res("vd1")]
        self.vf = self.dscr("vf", [T, 512], BF16); self.Rvf = [P.res("vf0"), P.res("vf1")]
        self.ocat = self.dscr("ocat", [T, D], BF16); self.Rocat = [P.res("ocat0"), P.res("ocat1")]
        self.xsd = self.dscr("xsd", [NSLOT, D], BF16); self.Rxsd = P.res("xsd")
        self.ysd = self.dscr("ysd", [NSLOT, D], BF16); self.Rysd = P.res("ysd")
        self.setup()
        for l in range(self.nl):
            self.phase0(l)
            if l == 0:
                self.dump("modt", self.modt[:], self.Rmodt, [128, 6, D], F32)
                self.dump("cosT", self.cosT[:], self.RcosT, [128, NT, 8], F32)
                self.dump("sinT", self.sinT[:], self.RsinT, [128, NT, 8], F32)
                self.dump("neglam", self.neglam[:], self.Rneglam, [128, 1], F32)
            if self.stop == ("p0", l): break
            self.phase1(l)
            if self.stop == ("p1", l): break
            self.phase2(l)
            if self.stop == ("p2", l): break
            self.phase3a(l)
            if self.stop == ("p3a", l): break
            self.phase3b(l)
            if self.stop == ("p3b", l): break
        else:
            self.phase_final()
        P.finish(self.scr[:])
        self.pes.close()

    def setup(self):
        nc, P, I = self.nc, self.P, self.ins
        pes = self.pes
        self.scr, P.scratch_res = self.sb(pes, "scr", [128, 8], F32)
        self.ident, self.Rident = self.sb(pes, "ident", [128, 128], BF16)
        self.identf, self.Ridentf = self.sb(pes, "identf", [128, 128], F32)
        self.triU, self.RtriU = self.sb(pes, "triU", [128, 128], F32)
        self.triS, self.RtriS = self.sb(pes, "triS", [128, 128], F32)
        self.e127, self.Re127 = self.sb(pes, "e127", [128, 128], F32)
        self.ones, self.Rones = self.sb(pes, "ones", [128, 128], F32)
        self.maskF, self.RmaskF = self.sb(pes, "maskF", [128, 128], BF16)
        self.maskD, self.RmaskD = self.sb(pes, "maskD", [128, 128], BF16)
        self.cosT, self.RcosT = self.sb(pes, "cosT", [128, NT, 8], F32)
        self.sinT, self.RsinT = self.sb(pes, "sinT", [128, NT, 8], F32)
        self.condB, self.RcondB = self.sb(pes, "condB", [128, 8, 128], F32)
        self.modt, self.Rmodt = self.sb(pes, "modt", [128, 6, D], F32)
        self.ncf, self.Rncf = self.sb(pes, "ncf", [128, NT, 8], F32)
        self.dest, self.Rdest = self.sb(pes, "dest", [128, NT, 2], I32)
        self.wts, self.Rwts = self.sb(pes, "wts", [128, NT, 2], F32)
        self.sbase, self.Rsbase = self.sb(pes, "sbase", [128, NE], F32)
        self.neglam, self.Rneglam = self.sb(pes, "neglam", [128, 1], F32)
        self.gt2p, self.Rgt2p = self.sb(pes, "gt2p", [128, D], F32)
        self.flags, self.Rflags = self.sb(pes, "flags", [128, NE * (CAP // SEG)], I32)
        with contextlib.ExitStack() as es:
            tmpf, Rtmpf = self.sb(es, "su_tmpf", [128, 128], F32)
            posi, Rposi = self.sb(es, "su_posi", [128, NT], I32)
            posf, Rposf = self.sb(es, "su_posf", [128, NT], F32)
            ang, Rang = self.sb(es, "su_ang", [128, NT, 8], F32)
            a2, Ra2 = self.sb(es, "su_a2", [128, NT, 8], F32)
            kk, Rkk = self.sb(es, "su_kk", [128, NT, 8], F32)
            cs, Rcs = self.sb(es, "su_cs", [128, 8], F32)

            W = [self.Rident, self.Ridentf, self.RtriU, self.RtriS, self.Re127, self.Rones,
                 self.RmaskF, self.RmaskD, self.Rsbase, Rtmpf, P.scratch_res]
            fns = [
                lambda e: e.memset(self.identf[:], 0.0),
                lambda e: e.affine_select(out=self.identf[:], in_=self.identf[:], pattern=[[-1, 128]], compare_op=ALU.not_equal,
                                          fill=1.0, base=0, channel_multiplier=1),
                lambda e: e.tensor_copy(out=self.ident[:], in_=self.identf[:]),
                lambda e: e.memset(self.triU[:], 1.0),
                lambda e: e.affine_select(out=self.triU[:], in_=self.triU[:], pattern=[[1, 128]], compare_op=ALU.is_ge,
                                          fill=0.0, base=0, channel_multiplier=-1),
                lambda e: e.memset(self.triS[:], 1.0),
                lambda e: e.affine_select(out=self.triS[:], in_=self.triS[:], pattern=[[1, 128]], compare_op=ALU.is_gt,
                                          fill=0.0, base=0, channel_multiplier=-1),
                lambda e: e.memset(self.e127[:], 0.0),
                lambda e: e.affine_select(out=self.e127[:], in_=self.e127[:], pattern=[[0, 128]], compare_op=ALU.not_equal,
                                          fill=1.0, base=-127, channel_multiplier=1),
                lambda e: e.memset(self.ones[:], 1.0),
                lambda e: e.memset(tmpf[:], 0.0),
                lambda e: e.affine_select(out=tmpf[:], in_=tmpf[:], pattern=[[1, 128]], compare_op=ALU.is_ge,
                                          fill=NEGM, base=0, channel_multiplier=-1),
                lambda e: e.tensor_copy(out=self.maskF[:], in_=tmpf[:]),
                lambda e: e.memset(self.maskD[:], 0.0),
                lambda e: e.memset(self.maskD[64:128, 0:64], NEGM),
                lambda e: e.memset(self.scr[:], 0.0),
            ]
            for j in range(NE):
                fns.append(lambda e, j=j: e.memset(self.sbase[:, j:j + 1], float(j * CAP)))
            P.chain("pool", fns, writes=W)
            zt, Rzt = self.sb(es, "su_zt", [128, 8192], BF16)
            P.op("pool", lambda e: e.memset(zt[:], 0.0), writes=[Rzt])
            nz = NSLOT // 1024 if self.dbg else 0
            P.op("sp", lambda e: [e.dma_start(out=self.xsd[c * 1024:(c + 1) * 1024, :].rearrange("(p a) d -> p (a d)", p=128), in_=zt[:]) for c in range(nz)],
                 reads=[Rzt], writes=[self.Rxsd], dmakey="su0", ndma=nz) if nz else None
            P.op("sp", lambda e: [e.dma_start(out=self.ysd[c * 1024:(c + 1) * 1024, :].rearrange("(p a) d -> p (a d)", p=128), in_=zt[:]) for c in range(nz)],
                 reads=[Rzt], writes=[self.Rysd], dmakey="su0y", ndma=nz) if nz else None
            P.op("sp", lambda e: e.dma_start(out=posi[:], in_=I["pos"]), writes=[Rposi], dmakey="su1")
            P.op("sp", lambda e: e.dma_start(out=cs[:], in_=I["cT"]), writes=[Rcs], dmakey="su2")

            P.op("dve", lambda e: e.tensor_copy(out=posf[:], in_=posi[:]), reads=[Rposi], writes=[Rposf])

            def angles(e):
                r = None
                for i in range(8):
                    r = e.tensor_scalar(out=ang[:, :, i], in0=posf[:], scalar1=float(INV_FREQ[i]), scalar2=None, op0=ALU.mult)
                return r
            P.op("dve", angles, reads=[Rposf], writes=[Rang])

            def reduce_sin(dst, Rdst, shift):
                P.op("dve", lambda e: e.tensor_scalar(out=a2[:], in0=ang[:], scalar1=float(shift), scalar2=None, op0=ALU.add),
                     reads=[Rang], writes=[Ra2])
                P.op("dve", lambda e: e.tensor_scalar(out=kk[:], in0=a2[:], scalar1=1.0 / TWO_PI, scalar2=MAGIC, op0=ALU.mult, op1=ALU.add),
                     reads=[Ra2], writes=[Rkk])
                P.op("dve", lambda e: e.tensor_scalar(out=kk[:], in0=kk[:], scalar1=MAGIC, scalar2=None, op0=ALU.subtract),
                     reads=[Rkk], writes=[Rkk])
                P.op("dve", lambda e: e.scalar_tensor_tensor(out=a2[:], in0=kk[:], scalar=-TWO_PI, in1=a2[:], op0=ALU.mult, op1=ALU.add),
                     reads=[Rkk, Ra2], writes=[Ra2])
                P.op("dve", lambda e: e.tensor_scalar(out=kk[:], in0=a2[:], scalar1=math.pi, scalar2=-TWO_PI, op0=ALU.is_gt, op1=ALU.mult),
                     reads=[Ra2], writes=[Rkk])
                P.op("dve", lambda e: e.tensor_tensor(out=a2[:], in0=a2[:], in1=kk[:], op=ALU.add), reads=[Ra2, Rkk], writes=[Ra2])
                P.op("dve", lambda e: e.tensor_scalar(out=kk[:], in0=a2[:], scalar1=-math.pi, scalar2=TWO_PI, op0=ALU.is_lt, op1=ALU.mult),
                     reads=[Ra2], writes=[Rkk])
                P.op("dve", lambda e: e.tensor_tensor(out=a2[:], in0=a2[:], in1=kk[:], op=ALU.add), reads=[Ra2, Rkk], writes=[Ra2])
                P.op("dve", lambda e: e.tensor_scalar(out=a2[:], in0=a2[:], scalar1=math.pi, scalar2=-math.pi, op0=ALU.min, op1=ALU.max),
                     reads=[Ra2], writes=[Ra2])
                P.op("act", lambda e: e.activation(out=dst[:], in_=a2[:], func=AF.Sin), reads=[Ra2], writes=[Rdst])
            reduce_sin(self.sinT, self.RsinT, 0.0)
            reduce_sin(self.cosT, self.RcosT, math.pi / 2)
            P.op("act", lambda e: e.activation(out=cs[:], in_=cs[:], func=AF.Silu), reads=[Rcs], writes=[Rcs])
            P.op("dve", lambda e: e.tensor_copy(out=self.condB[:], in_=cs[:].unsqueeze(2).to_broadcast([128, 8, 128])),
                 reads=[Rcs], writes=[self.RcondB])
            P.flush(self.scr[:])

    def phase0(self, l):
        nc, P, I = self.nc, self.P, self.ins
        with contextlib.ExitStack() as es:
            wa = [self.sb(es, f"p0_wa{j}", [128, 8, 512], F32) for j in range(2)]
            bb = [self.sb(es, f"p0_bb{j}", [128, 512], F32) for j in range(2)]
            pm = [self.ps(es, f"p0_pm{j}", [128, 512], F32) for j in range(2)]
            gb, Rgb = self.sb(es, "p0_gb", [128, D], F32)
            lam4, Rlam4 = self.sb(es, "p0_lam", [128, 4, 64], F32)
            ltmp, Rltmp = self.sb(es, "p0_ltmp", [128, 2, 64], F32)
            ls, Rls = self.sb(es, "p0_ls", [128, 2], F32)
            if l > 0:
                P.op("pool", lambda e: e.tensor_copy(out=self.gt2p[:], in_=self.modt[:, 5, :]), reads=[self.Rmodt], writes=[self.Rgt2p])
            for g in range(12):
                def grp0_(g):
                    j = g % 2
                    (wt, Rw), (bt, Rb), (pt, Rp) = wa[j], bb[j], pm[j]
                    src = I["w_ada"][l, :, g * 512:(g + 1) * 512].rearrange("(k p) n -> p k n", p=128)
                    P.op("sp", lambda e, wt=wt, src=src: e.dma_start(out=wt[:], in_=src), writes=[Rw], dmakey=f"p0w{j}")
                    bsrc = I["b_ada"][l, g * 512:(g + 1) * 512].partition_broadcast(128)
                    P.op("sp", lambda e, bt=bt, bsrc=bsrc: e.dma_start(out=bt[:], in_=bsrc), writes=[Rb], dmakey=f"p0b{j}")

                    def mm(e, wt=wt, pt=pt):
                        r = None
                        for k in range(8):
                            r = e.matmul(pt[:], lhsT=self.condB[:, k, :], rhs=wt[:, k, :], start=(k == 0), stop=(k == 7))
                        return r
                    P.op("pe", mm, reads=[Rw, self.RcondB], writes=[Rp])
                    dst = self.modt[:, g // 2, (g % 2) * 512:(g % 2 + 1) * 512]
                    P.op("dve", lambda e, dst=dst, pt=pt, bt=bt: e.tensor_tensor(out=dst, in0=pt[:], in1=bt[:], op=ALU.add),
                         reads=[Rp, Rb], writes=[self.Rmodt])
                grp0_(g)
            for (gname, idx) in (("g_mix", 1), ("g_ffn", 4)):
                gsrc = I[gname][l, :].partition_broadcast(128)
                P.op("sp", lambda e, gsrc=gsrc: e.dma_start(out=gb[:], in_=gsrc), writes=[Rgb], dmakey="p0g")
                P.op("dve", lambda e, idx=idx: e.scalar_tensor_tensor(out=self.modt[:, idx, :], in0=self.modt[:, idx, :], scalar=1.0,
                                                                      in1=gb[:], op0=ALU.add, op1=ALU.mult),
                     reads=[Rgb, self.Rmodt], writes=[self.Rmodt])
            for qi, n in enumerate(("lambda_q1", "lambda_k1", "lambda_q2", "lambda_k2")):
                lsrc = I[n][l, :].partition_broadcast(128)
                P.op("sp", lambda e, qi=qi, lsrc=lsrc: e.dma_start(out=lam4[:, qi, :], in_=lsrc), writes=[Rlam4], dmakey=f"p0l{qi}")
            lam_init = 0.8 - 0.6 * math.exp(-0.3 * l)

            def lamf(e):
                e.tensor_tensor(out=ltmp[:, 0, :], in0=lam4[:, 0, :], in1=lam4[:, 1, :], op=ALU.mult)
                return e.tensor_tensor(out=ltmp[:, 1, :], in0=lam4[:, 2, :], in1=lam4[:, 3, :], op=ALU.mult)
            P.op("dve", lamf, reads=[Rlam4], writes=[Rltmp])
            P.op("dve", lambda e: e.tensor_reduce(out=ls[:], in_=ltmp[:], axis=AX.X, op=ALU.add), reads=[Rltmp], writes=[Rls])
            P.op("act", lambda e: e.activation(out=ls[:], in_=ls[:], func=AF.Exp), reads=[Rls], writes=[Rls])
            P.op("dve", lambda e: e.scalar_tensor_tensor(out=self.neglam[:], in0=ls[:, 1:2], scalar=-lam_init, in1=ls[:, 0:1],
                                                         op0=ALU.add, op1=ALU.subtract),
                 reads=[Rls], writes=[self.Rneglam])
            P.flush(self.scr[:])

    def rstd_ops(self, xt, Rx, junk, Rjunk, ss, Rss, n):
        P = self.P
        P.op("act", lambda e: e.activation(out=junk[:], in_=xt, func=AF.Square, accum_out=ss[:]), reads=[Rx], writes=[Rjunk, Rss])
        P.op("act", lambda e: e.activation(out=ss[:], in_=ss[:], func=AF.Ln, scale=1.0 / n, bias=EPS), reads=[Rss], writes=[Rss])
        P.op("act", lambda e: e.activation(out=ss[:], in_=ss[:], func=AF.Exp, scale=-0.5), reads=[Rss], writes=[Rss])

    def phase1(self, l):
        nc, P, I = self.nc, self.P, self.ins
        with contextlib.ExitStack() as es:
            win = [self.sb(es, f"p1_win{k}", [128, INC], BF16) for k in range(8)]
            Rwin = [w[1] for w in win]
            P.op("pool", lambda e: [e.dma_start(out=win[k][0][:], in_=I["w_in"][l, k * 128:(k + 1) * 128, :]) for k in range(8)],
                 writes=Rwin, dmakey="p1win", ndma=8)
            bfb, Rbfb = self.sb(es, "p1_bfb", [128, 8], F32)
            P.op("sp", lambda e: e.dma_start(out=bfb[:], in_=I["b_forget"][l, :].partition_broadcast(128)), writes=[Rbfb], dmakey="p1bfb")
            xs = [self.sb(es, f"p1_xs{j}", [128, D], F32) for j in range(2)]
            tmp, Rtmp = self.sb(es, "p1_tmp", [128, D], F32)
            junk, Rjunk = self.sb(es, "p1_junk", [128, D], BF16)
            hb = [self.sb(es, f"p1_hb{j}", [128, D], BF16) for j in range(2)]
            hT = [self.sb(es, f"p1_hT{j}", [128, 8, 128], BF16) for j in range(2)]
            qk = [self.sb(es, f"p1_qk{j}", [128, 1024], BF16) for j in range(2)]
            vds = [self.sb(es, f"p1_vds{j}", [128, 512], BF16) for j in range(2)]
            vfs = [self.sb(es, f"p1_vfs{j}", [128, 512], BF16) for j in range(2)]
            kf = [self.sb(es, f"p1_kf{j}", [128, 512], BF16) for j in range(2)]
            qfa = [self.sb(es, f"p1_qfa{j}", [128, 8, 67], BF16) for j in range(2)]
            stqk = [self.sb(es, f"p1_stqk{j}", [128, 8, 128], BF16) for j in range(2)]
            stkf = [self.sb(es, f"p1_stkf{j}", [128, 4, 128], BF16) for j in range(2)]
            stqf = [self.sb(es, f"p1_stqf{j}", [128, 8, 128], BF16) for j in range(2)]
            ss = [self.sb(es, f"p1_ss{j}", [128, 1], F32) for j in range(2)]
            rt, Rrt = self.sb(es, "p1_rt", [128, 4, 8, 8], F32)
            fz, Rfz = self.sb(es, "p1_fz", [128, 8], F32)
            fnz, Rfnz = self.sb(es, "p1_fnz", [128, 8], F32)
            fa, Rfa = self.sb(es, "p1_fa", [128, 8], F32)
            fl, Rfl = self.sb(es, "p1_fl", [128, 8], F32)
            cf = [self.sb(es, f"p1_cf{j}", [128, 8], F32) for j in range(2)]
            g8, Rg8 = self.sb(es, "p1_g8", [128, 8], F32)
            r1, Rr1 = self.sb(es, "p1_r1", [128, 8], F32)
            if l > 0:
                y0 = [self.sb(es, f"p1_y0{j}", [128, D], BF16) for j in range(2)]
                y1 = [self.sb(es, f"p1_y1{j}", [128, D], BF16) for j in range(2)]
            tp = [self.ps(es, f"p1_tp{j}", [128, 8, 128], BF16) for j in range(2)]
            pg = [self.ps(es, f"p1_pg{j}", [128, 512], F32) for j in range(3)]
            pff, Rpff = self.ps(es, "p1_pff", [128, 8], F32)
            pcs, Rpcs = self.ps(es, "p1_pcs", [128, 8], F32)
            tpi = [0]
            pgi = [0]

            def stageA(i):
                b = i % 2
                xt, Rx = xs[b]
                rows = slice(i * 128, (i + 1) * 128)
                if l == 0:
                    P.op("sp", lambda e: e.dma_start(out=xt[:], in_=I["x"][rows, :]), writes=[Rx], dmakey=f"p1x{b}")
                    return
                self.combine(i, xt, Rx, y0[b], y1[b], tmp, Rtmp, self.gt2p[:], self.Rgt2p, f"p1x{b}", f"p1y{b}")
                P.op("sp", lambda e: e.dma_start(out=self.xres[rows, :], in_=xt[:]), reads=[Rx], writes=[self.Rxres[b]], dmakey=f"p1xo{b}")

            stageA(0)
            for i in range(NT):
                def tile1_(i):
                    b = i % 2
                    if i + 1 < NT:
                        stageA(i + 1)
                    xt, Rx = xs[b]
                    rows = slice(i * 128, (i + 1) * 128)
                    sst, Rss = ss[b]
                    self.rstd_ops(xt[:], Rx, junk, Rjunk, sst, Rss, D)
                    P.op("dve", lambda e, xt=xt, sst=sst: e.scalar_tensor_tensor(out=tmp[:], in0=xt[:], scalar=sst[:, 0:1], in1=self.modt[:, 1, :],
                                                                               op0=ALU.mult, op1=ALU.mult),
                         reads=[Rx, Rss, self.Rmodt], writes=[Rtmp])
                    hbt, Rhb = hb[b]
                    P.op("dve", lambda e, hbt=hbt: e.tensor_tensor(out=hbt[:], in0=tmp[:], in1=self.modt[:, 0, :], op=ALU.add),
                         reads=[Rtmp, self.Rmodt], writes=[Rhb])
                    tpt, Rtp = tp[tpi[0] % 2]; tpi[0] += 1
                    hTt, RhT = hT[b]

                    def trh(e, hbt=hbt, tpt=tpt):
                        r = None
                        for k in range(8):
                            r = e.transpose(out=tpt[:, k, :], in_=hbt[:, k * 128:(k + 1) * 128], identity=self.ident[:])
                        return r
                    P.op("pe", trh, reads=[Rhb, self.Rident], writes=[Rtp])
                    P.op("act", lambda e, hTt=hTt, tpt=tpt: e.activation(out=hTt[:], in_=tpt[:], func=AF.Copy), reads=[Rtp], writes=[RhT])
                    qkt, Rqk = qk[b]
                    qk3 = qkt[:].rearrange("p (m d) -> p m d", d=64)
                    qfat, Rqfa = qfa[b]
                    for g in range(6):
                        pgt, Rpg = pg[pgi[0] % 3]; pgi[0] += 1

                        def mm(e, g=g, pgt=pgt, hTt=hTt):
                            r = None
                            for k in range(8):
                                r = e.matmul(pgt[:], lhsT=hTt[:, k, :], rhs=win[k][0][:, g * 512:(g + 1) * 512], start=(k == 0), stop=(k == 7))
                            return r
                        P.op("pe", mm, reads=[RhT] + Rwin, writes=[Rpg])
                        pg3 = pgt[:].rearrange("p (m d) -> p m d", d=64)
                        if g < 2:
                            m0 = g * 8
                            P.op("act", lambda e, pg3=pg3, qk3=qk3, m0=m0: e.activation(out=qk3[:, m0:m0 + 8, 16:64], in_=pg3[:, :, 16:64], func=AF.Copy),
                                 reads=[Rpg], writes=[Rqk])
                            cosb = self.cosT[:, i, :].unsqueeze(1).to_broadcast([128, 8, 8])
                            sinb = self.sinT[:, i, :].unsqueeze(1).to_broadcast([128, 8, 8])

                            def rope1(e, pg3=pg3, cosb=cosb, sinb=sinb):
                                e.tensor_tensor(out=rt[:, 0], in0=pg3[:, :, 0:8], in1=cosb, op=ALU.mult)
                                e.tensor_tensor(out=rt[:, 1], in0=pg3[:, :, 8:16], in1=sinb, op=ALU.mult)
                                e.tensor_tensor(out=rt[:, 2], in0=pg3[:, :, 8:16], in1=cosb, op=ALU.mult)
                                return e.tensor_tensor(out=rt[:, 3], in0=pg3[:, :, 0:8], in1=sinb, op=ALU.mult)
                            P.op("dve", rope1, reads=[Rpg, self.RcosT, self.RsinT], writes=[Rrt])

                            def rope2(e, qk3=qk3, m0=m0):
                                e.tensor_tensor(out=qk3[:, m0:m0 + 8, 0:8], in0=rt[:, 0], in1=rt[:, 1], op=ALU.subtract)
                                return e.tensor_tensor(out=qk3[:, m0:m0 + 8, 8:16], in0=rt[:, 2], in1=rt[:, 3], op=ALU.add)
                            P.op("dve", rope2, reads=[Rrt], writes=[Rqk])
                        elif g == 2:
                            vt, Rv = vds[b]
                            P.op("act", lambda e, vt=vt, pgt=pgt: e.activation(out=vt[:], in_=pgt[:], func=AF.Copy), reads=[Rpg], writes=[Rv])
                            P.op("sp", lambda e, vt=vt, rows=rows: e.dma_start(out=self.vd[rows, :], in_=vt[:]), reads=[Rv], writes=[self.Rvd[b]], dmakey=f"p1vd{b}")
                        elif g == 3:
                            P.op("act", lambda e, qfat=qfat, pg3=pg3: e.activation(out=qfat[:, :, 0:64], in_=pg3, func=AF.Copy), reads=[Rpg], writes=[Rqfa])
                        elif g == 4:
                            kt, Rk = kf[b]
                            P.op("act", lambda e, kt=kt, pgt=pgt: e.activation(out=kt[:], in_=pgt[:], func=AF.Copy), reads=[Rpg], writes=[Rk])
                        elif g == 5:
                            vt2, Rv2 = vfs[b]
                            P.op("act", lambda e, vt2=vt2, pgt=pgt: e.activation(out=vt2[:], in_=pgt[:], func=AF.Copy), reads=[Rpg], writes=[Rv2])
                            P.op("sp", lambda e, vt2=vt2, rows=rows: e.dma_start(out=self.vf[rows, :], in_=vt2[:]), reads=[Rv2], writes=[self.Rvf[b]], dmakey=f"p1vf{b}")
                    def mmf(e, hTt=hTt):
                        r = None
                        for k in range(8):
                            r = e.matmul(pff[:], lhsT=hTt[:, k, :], rhs=win[k][0][:, 3072:3080], start=(k == 0), stop=(k == 7))
                        return r
                    P.op("pe", mmf, reads=[RhT] + Rwin, writes=[Rpff])
                    P.op("dve", lambda e: e.tensor_tensor(out=fz[:], in0=pff[:], in1=bfb[:], op=ALU.add), reads=[Rpff, Rbfb], writes=[Rfz])
                    P.op("dve", lambda e: e.tensor_scalar(out=fnz[:], in0=fz[:], scalar1=-1.0, scalar2=None, op0=ALU.mult), reads=[Rfz], writes=[Rfnz])
                    P.op("dve", lambda e: e.tensor_tensor(out=fa[:], in0=fz[:], in1=fnz[:], op=ALU.max), reads=[Rfz, Rfnz], writes=[Rfa])
                    P.op("act", lambda e: e.activation(out=fa[:], in_=fa[:], func=AF.Exp, scale=-1.0), reads=[Rfa], writes=[Rfa])
                    P.op("act", lambda e: e.activation(out=fa[:], in_=fa[:], func=AF.Ln, bias=1.0), reads=[Rfa], writes=[Rfa])
                    P.op("dve", lambda e: e.scalar_tensor_tensor(out=fl[:], in0=fnz[:], scalar=0.0, in1=fa[:], op0=ALU.max, op1=ALU.add),
                         reads=[Rfnz, Rfa], writes=[Rfl])
                    P.op("dve", lambda e: e.tensor_scalar(out=fl[:], in0=fl[:], scalar1=-1.0, scalar2=None, op0=ALU.mult), reads=[Rfl], writes=[Rfl])
                    cft, Rcf = cf[b]
                    cfp, Rcfp = cf[1 - b]

                    def mmc(e, i=i, cfp=cfp):
                        r = e.matmul(pcs[:], lhsT=self.triU[:], rhs=fl[:], start=True, stop=(i == 0))
                        if i > 0:
                            r = e.matmul(pcs[:], lhsT=self.e127[:], rhs=cfp[:], start=False, stop=True)
                        return r
                    P.op("pe", mmc, reads=[Rfl, self.RtriU, self.Re127] + ([Rcfp] if i > 0 else []), writes=[Rpcs])

                    def cfev(e, cft=cft, i=i):
                        e.tensor_copy(out=cft[:], in_=pcs[:])
                        e.tensor_scalar(out=self.ncf[:, i, :], in0=pcs[:], scalar1=-1.0, scalar2=None, op0=ALU.mult)
                        return e.tensor_scalar(out=g8[:], in0=pcs[:], scalar1=8.0, scalar2=None, op0=ALU.mult)
                    P.op("dve", cfev, reads=[Rpcs], writes=[Rcf, self.Rncf, Rg8])
                    P.op("dve", lambda e, qfat=qfat: e.tensor_copy(out=qfat[:, :, 64], in_=g8[:]), reads=[Rg8], writes=[Rqfa])
                    P.op("dve", lambda e, qfat=qfat: e.tensor_tensor(out=r1[:], in0=g8[:], in1=qfat[:, :, 64], op=ALU.subtract), reads=[Rg8, Rqfa], writes=[Rr1])
                    P.op("dve", lambda e, qfat=qfat: e.tensor_copy(out=qfat[:, :, 65], in_=r1[:]), reads=[Rr1], writes=[Rqfa])
                    P.op("dve", lambda e, qfat=qfat: e.tensor_tensor(out=g8[:], in0=r1[:], in1=qfat[:, :, 65], op=ALU.subtract), reads=[Rr1, Rqfa], writes=[Rg8])
                    P.op("dve", lambda e, qfat=qfat: e.tensor_copy(out=qfat[:, :, 66], in_=g8[:]), reads=[Rg8], writes=[Rqfa])
                    tpt, Rtp = tp[tpi[0] % 2]; tpi[0] += 1

                    def trqk(e, qkt=qkt, tpt=tpt):
                        r = None
                        for j in range(8):
                            r = e.transpose(out=tpt[:, j, :], in_=qkt[:, j * 128:(j + 1) * 128], identity=self.ident[:])
                        return r
                    P.op("pe", trqk, reads=[Rqk, self.Rident], writes=[Rtp])
                    st, Rst = stqk[b]
                    P.op("dve", lambda e, st=st, tpt=tpt: e.tensor_copy(out=st[:], in_=tpt[:]), reads=[Rtp], writes=[Rst])
                    P.op("sp", lambda e, st=st, rows=rows: e.dma_start(out=self.qktd[:, :, rows].rearrange("j r t -> r j t"), in_=st[:]),
                         reads=[Rst], writes=[self.Rqktd[b]], dmakey=f"p1sq{b}")
                    tpt2, Rtp2 = tp[tpi[0] % 2]; tpi[0] += 1
                    kt, Rk = kf[b]

                    def trkf(e, kt=kt, tpt2=tpt2):
                        r = None
                        for j in range(4):
                            r = e.transpose(out=tpt2[:, j, :], in_=kt[:, j * 128:(j + 1) * 128], identity=self.ident[:])
                        return r
                    P.op("pe", trkf, reads=[Rk, self.Rident], writes=[Rtp2])
                    st2, Rst2 = stkf[b]
                    P.op("act", lambda e, st2=st2, tpt2=tpt2: e.activation(out=st2[:], in_=tpt2[:, 0:4, :], func=AF.Copy), reads=[Rtp2], writes=[Rst2])
                    P.op("sp", lambda e, st2=st2, rows=rows: e.dma_start(out=self.ktf[:, :, rows].rearrange("j r t -> r j t"), in_=st2[:]),
                         reads=[Rst2], writes=[self.Rktf[b]], dmakey=f"p1sk{b}")
                    tpt3, Rtp3 = tp[tpi[0] % 2]; tpi[0] += 1

                    def trqf(e, qfat=qfat, tpt3=tpt3):
                        r = None
                        for h in range(8):
                            r = e.transpose(out=tpt3[0:67, h, :], in_=qfat[:, h, :], identity=self.ident[:])
                        return r
                    P.op("pe", trqf, reads=[Rqfa, self.Rident], writes=[Rtp3])
                    st3, Rst3 = stqf[b]
                    P.op("dve", lambda e, st3=st3, tpt3=tpt3: e.tensor_copy(out=st3[0:67], in_=tpt3[0:67]), reads=[Rtp3], writes=[Rst3])
                    P.op("sp", lambda e, st3=st3, rows=rows: e.dma_start(out=self.qtf[:, :, rows].rearrange("h r t -> r h t"), in_=st3[0:67]),
                         reads=[Rst3], writes=[self.Rqtf[b]], dmakey=f"p1sf{b}")
                tile1_(i)
            P.flush(self.scr[:])

    def combine(self, i, xt, Rx, y0p, y1p, tmp, Rtmp, gt2, Rgt2, kx, ky):
        P = self.P
        rows = slice(i * 128, (i + 1) * 128)
        (y0t, Ry0), (y1t, Ry1) = y0p, y1p
        P.op("sp", lambda e: e.dma_start(out=xt[:], in_=self.xres[rows, :]), reads=self.Rxres, writes=[Rx], dmakey=kx)
        for k, (yt, Ry) in enumerate(((y0t, Ry0), (y1t, Ry1))):
            P.op("pool", lambda e, yt=yt: e.memset(yt[:], 0.0), writes=[Ry])
            P.op("pool", lambda e, yt=yt, k=k: self.indirect(
                e, out=yt[:, :], out_offset=None, in_=self.ysd[:, :],
                in_offset=bass.IndirectOffsetOnAxis(ap=self.dest[:, i, k:k + 1], axis=0)),
                reads=[self.Rysd, self.Rdest], writes=[Ry], dmakey=ky + str(k))
        P.op("dve", lambda e: e.tensor_scalar(out=tmp[:], in0=y0t[:], scalar1=self.wts[:, i, 0:1], scalar2=None, op0=ALU.mult),
             reads=[Ry0, self.Rwts], writes=[Rtmp])
        P.op("dve", lambda e: e.scalar_tensor_tensor(out=tmp[:], in0=y1t[:], scalar=self.wts[:, i, 1:2], in1=tmp[:], op0=ALU.mult, op1=ALU.add),
             reads=[Ry1, self.Rwts, Rtmp], writes=[Rtmp])
        P.op("dve", lambda e: e.tensor_tensor(out=tmp[:], in0=tmp[:], in1=gt2, op=ALU.mult), reads=[Rtmp, Rgt2], writes=[Rtmp])
        P.op("dve", lambda e: e.tensor_tensor(out=xt[:], in0=xt[:], in1=tmp[:], op=ALU.add), reads=[Rtmp, Rx], writes=[Rx])

    def phase2(self, l):
        nc, P, I = self.nc, self.P, self.ins
        lam_init = 0.8 - 0.6 * math.exp(-0.3 * l)
        with contextlib.ExitStack() as es:
            QT = [self.sb(es, f"p2_QT{j}", [67, T], BF16) for j in range(2)]
            KT = [self.sb(es, f"p2_KT{j}", [67, T], BF16) for j in range(2)]
            VV = [self.sb(es, f"p2_V{j}", [128, NT * 129], BF16) for j in range(2)]
            PT = [self.sb(es, f"p2_PT{j}", [128, 512], BF16) for j in range(4)]
            u1, Ru1 = self.sb(es, "p2_u1", [128, NT, 128], F32)
            ot, Rot = self.sb(es, "p2_ot", [128, 2, 128], F32)
            osq, Rosq = self.sb(es, "p2_osq", [128, 2, 128], F32)
            rden, Rrden = self.sb(es, "p2_rden", [128, 2], F32)
            ssq, Rssq = self.sb(es, "p2_ssq", [128, 2], F32)
            ost = [self.sb(es, f"p2_ost{j}", [128, 2, 128], BF16) for j in range(2)]
            gsub, Rgsub = self.sb(es, "p2_gsub", [128, 128], F32)
            gfox, Rgfox = self.sb(es, "p2_gfox", [128, 64], F32)
            SP_ = [self.ps(es, f"p2_S{j}", [128, 512], F32) for j in range(3)]
            OP = [self.ps(es, f"p2_O{j}", [128, 512], F32) for j in range(4)]
            for kt, Rk in KT:
                P.op("pool", lambda e, kt=kt: e.memset(kt[64:67, :], 1.0), writes=[Rk])
            P.op("sp", lambda e: e.dma_start(out=gsub[:], in_=I["g_subln"][l, :].partition_broadcast(128)), writes=[Rgsub], dmakey="p2g1")
            P.op("sp", lambda e: e.dma_start(out=gfox[:], in_=I["g_fox_out"][l, :].partition_broadcast(128)), writes=[Rgfox], dmakey="p2g2")
            P.op("dve", lambda e: e.tensor_scalar(out=gsub[:], in0=gsub[:], scalar1=float(1.0 - lam_init), scalar2=None, op0=ALU.mult),
                 reads=[Rgsub], writes=[Rgsub])
            jobs = []
            for h in range(4):
                jobs.append(("d", h, 0)); jobs.append(("d", h, 1))
            for h in range(8):
                jobs.append(("f", h, 0))
            vslot = {}
            vcount = [0]

            def load(jn):
                kind, h, c = jobs[jn]
                b = jn % 2
                qt, Rq = QT[b]; kt, Rk = KT[b]
                if kind == "d":
                    m = 2 * h + c
                    P.op("sp", lambda e: e.dma_start(out=qt[0:64, :], in_=self.qktd[m // 2, (m % 2) * 64:(m % 2) * 64 + 64, :]),
                         reads=self.Rqktd, writes=[Rq], dmakey=f"p2q{b}")
                    mk = 8 + m
                    P.op("sp", lambda e: e.dma_start(out=kt[0:64, :], in_=self.qktd[mk // 2, (mk % 2) * 64:(mk % 2) * 64 + 64, :]),
                         reads=self.Rqktd, writes=[Rk], dmakey=f"p2k{b}")
                    if c == 0:
                        vb = vcount[0] % 2; vcount[0] += 1
                        vslot[(kind, h)] = vb
                        vt, Rv = VV[vb]
                        v3 = vt[:].rearrange("p (n d) -> p n d", d=129)
                        P.op("sp", lambda e: e.dma_start(out=v3[:, :, 0:128], in_=self.vd[:, h * 128:(h + 1) * 128].rearrange("(n p) d -> p n d", p=128)),
                             reads=self.Rvd, writes=[Rv], dmakey=f"p2v{vb}")
                        P.op("pool", lambda e: e.memset(v3[:, :, 128:129], 1.0), writes=[Rv])
                else:
                    P.op("sp", lambda e: e.dma_start(out=qt[0:67, :], in_=self.qtf[h, :, :]), reads=self.Rqtf, writes=[Rq], dmakey=f"p2q{b}")
                    P.op("sp", lambda e: e.dma_start(out=kt[0:64, :], in_=self.ktf[h // 2, (h % 2) * 64:(h % 2) * 64 + 64, :]),
                         reads=self.Rktf, writes=[Rk], dmakey=f"p2k{b}")
                    vb = vcount[0] % 2; vcount[0] += 1
                    vslot[(kind, h)] = vb
                    vt, Rv = VV[vb]
                    v3 = vt[:, 0:NT * 65].rearrange("p (n d) -> p n d", d=65)
                    P.op("sp", lambda e: e.dma_start(out=v3[:, :, 0:64], in_=self.vf[:, h * 64:(h + 1) * 64].rearrange("(n p) d -> p n d", p=128)),
                         reads=self.Rvf, writes=[Rv], dmakey=f"p2v{vb}")
                    P.op("pool", lambda e: e.memset(v3[:, :, 64:65], 1.0), writes=[Rv])

            sidx = [0]; pidx = [0]; oidx = [0]; osti = [0]
            load(0)
            for jn, (kind, h, c) in enumerate(jobs):
                def job_(jn, kind, h, c):
                    if jn + 1 < len(jobs):
                        load(jn + 1)
                    b = jn % 2
                    qt, Rq = QT[b]; kt, Rk = KT[b]
                    vt, Rv = VV[vslot[(kind, h)]]
                    if kind == "d":
                        dv = 128; KK = 64; mask, Rmask = self.maskD, self.RmaskD
                        v3 = vt[:].rearrange("p (n d) -> p n d", d=129)
                    else:
                        dv = 64; KK = 67; mask, Rmask = self.maskF, self.RmaskF
                        v3 = vt[:, 0:NT * 65].rearrange("p (n d) -> p n d", d=65)
                    for qti in range(NT // 4):
                        def qtile_(qti):
                            Ob = [OP[(oidx[0] * 2) % 4], OP[(oidx[0] * 2 + 1) % 4]]; oidx[0] += 1
                            O3 = [o[0][:, 0:2 * (dv + 1)].rearrange("p (s d) -> p s d", d=dv + 1) for o in Ob]
                            nkb = 4 * qti + 4
                            steps = []
                            for kb in range(nkb):
                                j = kb - 4 * qti
                                qlo = max(j, 0) * 128
                                st, Rs = SP_[sidx[0] % 3]; sidx[0] += 1
                                pt, Rp = PT[pidx[0] % 4]; pidx[0] += 1
                                steps.append((kb, j, qlo, st, Rs, pt, Rp))

                            def rec_S(s):
                                kb, j, qlo, st, Rs, pt, Rp = s

                                def f(e):
                                    r = e.matmul(st[:, qlo:512], lhsT=kt[0:KK, kb * 128:(kb + 1) * 128],
                                                 rhs=qt[0:KK, qti * 512 + qlo:(qti + 1) * 512], start=True, stop=True, skip_group_check=True)
                                    if j >= 0:
                                        r = e.matmul(st[:, j * 128:(j + 1) * 128], lhsT=self.ident[:], rhs=mask[:], start=False, stop=True, skip_group_check=True)
                                    return r
                                P.op("pe", f, reads=[Rq, Rk, self.Rident, Rmask], writes=[Rs])

                            def rec_E(s):
                                kb, j, qlo, st, Rs, pt, Rp = s
                                bias = self.ncf[:, kb, h:h + 1] if kind == "f" else 0.0
                                P.op("act", lambda e: e.activation(out=pt[:, qlo:512], in_=st[:, qlo:512], func=AF.Exp, scale=0.125, bias=bias),
                                     reads=[Rs, self.Rncf], writes=[Rp])

                            def rec_PV(s):
                                kb, j, qlo, st, Rs, pt, Rp = s

                                def f(e):
                                    r = None
                                    for i4 in range(max(j, 0), 4):
                                        r = e.matmul(O3[i4 // 2][:, i4 % 2, :], lhsT=pt[:, i4 * 128:(i4 + 1) * 128], rhs=v3[:, kb, :],
                                                     start=(kb == 0 and i4 % 2 == 0), stop=(kb == 4 * qti + i4), skip_group_check=True)
                                    return r
                                P.op("pe", f, reads=[Rp, Rv], writes=[Ob[0][1], Ob[1][1]])
                            LA = 2
                            for s_i in range(min(LA, nkb)):
                                rec_S(steps[s_i])
                            for s_i in range(nkb):
                                rec_E(steps[s_i])
                                if s_i + LA < nkb:
                                    rec_S(steps[s_i + LA])
                                rec_PV(steps[s_i])
                            for half in range(2):
                                o3 = O3[half]; Ro = Ob[half][1]
                                rows0 = qti * 512 + half * 256
                                P.op("dve", lambda e, o3=o3: e.tensor_scalar(out=rden[:], in0=o3[:, :, dv], scalar1=1e-30, scalar2=None, op0=ALU.max),
                                     reads=[Ro], writes=[Rrden])
                                P.op("dve", lambda e: e.reciprocal(out=rden[:], in_=rden[:]), reads=[Rrden], writes=[Rrden])
                                rb = rden[:].unsqueeze(2).to_broadcast([128, 2, dv])
                                if kind == "d" and c == 0:
                                    P.op("dve", lambda e, o3=o3, rb=rb, half=half, qti=qti: e.tensor_tensor(out=u1[:, 4 * qti + 2 * half:4 * qti + 2 * half + 2, :], in0=o3[:, :, 0:dv], in1=rb, op=ALU.mult),
                                         reads=[Ro, Rrden], writes=[Ru1])
                                    continue
                                P.op("dve", lambda e, o3=o3, rb=rb: e.tensor_tensor(out=ot[:, :, 0:dv], in0=o3[:, :, 0:dv], in1=rb, op=ALU.mult),
                                     reads=[Ro, Rrden], writes=[Rot])
                                if kind == "d":
                                    P.op("dve", lambda e, half=half, qti=qti: e.scalar_tensor_tensor(out=ot[:], in0=ot[:], scalar=self.neglam[:, 0:1], in1=u1[:, 4 * qti + 2 * half:4 * qti + 2 * half + 2, :],
                                                                                           op0=ALU.mult, op1=ALU.add),
                                         reads=[Rot, Ru1, self.Rneglam], writes=[Rot])
                                P.op("dve", lambda e: e.tensor_tensor(out=osq[:, :, 0:dv], in0=ot[:, :, 0:dv], in1=ot[:, :, 0:dv], op=ALU.mult), reads=[Rot], writes=[Rosq])
                                P.op("dve", lambda e: e.tensor_reduce(out=ssq[:], in_=osq[:, :, 0:dv], axis=AX.X, op=ALU.add), reads=[Rosq], writes=[Rssq])
                                P.op("act", lambda e: e.activation(out=ssq[:], in_=ssq[:], func=AF.Ln, scale=1.0 / dv, bias=EPS), reads=[Rssq], writes=[Rssq])
                                P.op("act", lambda e: e.activation(out=ssq[:], in_=ssq[:], func=AF.Exp, scale=-0.5), reads=[Rssq], writes=[Rssq])
                                sb_ = ssq[:].unsqueeze(2).to_broadcast([128, 2, dv])
                                P.op("dve", lambda e, sb_=sb_: e.tensor_tensor(out=ot[:, :, 0:dv], in0=ot[:, :, 0:dv], in1=sb_, op=ALU.mult), reads=[Rot, Rssq], writes=[Rot])
                                gt = gsub if kind == "d" else gfox
                                Rg = Rgsub if kind == "d" else Rgfox
                                gb = gt[:].unsqueeze(1).to_broadcast([128, 2, dv])
                                ostt, Rost = ost[osti[0] % 2]; ob_ = osti[0] % 2; osti[0] += 1
                                P.op("dve", lambda e, gb=gb, ostt=ostt: e.tensor_tensor(out=ostt[:, :, 0:dv], in0=ot[:, :, 0:dv], in1=gb, op=ALU.mult),
                                     reads=[Rot, Rg], writes=[Rost])
                                col0 = h * 128 if kind == "d" else 512 + h * 64
                                dst = self.ocat[rows0:rows0 + 256, col0:col0 + dv].rearrange("(s p) d -> p s d", p=128)
                                P.op("sp", lambda e, dst=dst, ostt=ostt: e.dma_start(out=dst, in_=ostt[:, :, 0:dv]), reads=[Rost],
                                     writes=[self.Rocat[ob_]], dmakey=f"p2o{ob_}")
                        qtile_(qti)
                job_(jn, kind, h, c)
            P.flush(self.scr[:])

    def phase3a(self, l):
        nc, P, I = self.nc, self.P, self.ins
        xsrc = I["x"] if l == 0 else self.xres
        BIG = 1.0e4
        with contextlib.ExitStack() as es:
            wo = [self.sb(es, f"p3_wo{k}", [128, D], BF16) for k in range(8)]
            Rwo = [w[1] for w in wo]
            P.op("pool", lambda e: [e.dma_start(out=wo[k][0][:], in_=I["w_out"][l, k * 128:(k + 1) * 128, :]) for k in range(8)],
                 writes=Rwo, dmakey="p3wo", ndma=8)
            wr, Rwr = self.sb(es, "p3_wr", [128, 8, 36], F32)
            P.op("sp", lambda e: e.dma_start(out=wr[:, :, 0:4], in_=I["w_router_group"][l].rearrange("(k p) n -> p k n", p=128)), writes=[Rwr], dmakey="p3wr1")
            P.op("sp", lambda e: e.dma_start(out=wr[:, :, 4:36], in_=I["w_router_expert"][l].rearrange("(k p) n -> p k n", p=128)), writes=[Rwr], dmakey="p3wr2")
            rb, Rrb = self.sb(es, "p3_rb", [128, 36], F32)
            P.op("sp", lambda e: e.dma_start(out=rb[:, 0:4], in_=I["b_router_group"][l, :].partition_broadcast(128)), writes=[Rrb], dmakey="p3rb1")
            P.op("sp", lambda e: e.dma_start(out=rb[:, 4:36], in_=I["b_router_expert"][l, :].partition_broadcast(128)), writes=[Rrb], dmakey="p3rb2")
            xs = [self.sb(es, f"p3_xs{j}", [128, D], F32) for j in range(2)]
            oc = [self.sb(es, f"p3_oc{j}", [128, D], BF16) for j in range(2)]
            oT = [self.sb(es, f"p3_oT{j}", [128, 8, 128], BF16) for j in range(2)]
            tmp, Rtmp = self.sb(es, "p3_tmp", [128, D], F32)
            junk, Rjunk = self.sb(es, "p3_junk", [128, D], BF16)
            h2f = [self.sb(es, f"p3_h2f{j}", [128, D], F32) for j in range(2)]
            h2b = [self.sb(es, f"p3_h2b{j}", [128, D], BF16) for j in range(2)]
            h2T, Rh2T = self.sb(es, "p3_h2T", [128, 8, 128], F32)
            ss = [self.sb(es, f"p3_ss{j}", [128, 1], F32) for j in range(2)]
            lg, Rlg = self.sb(es, "p3_lg", [128, 36], F32)
            sm, Rsm = self.sb(es, "p3_sm", [128, 16], F32)
            ohg, Rohg = self.sb(es, "p3_ohg", [128, 4], F32)
            eg, Reg = self.sb(es, "p3_eg", [128, 4], F32)
            msk, Rmsk = self.sb(es, "p3_msk", [128, 32], F32)
            oh, Roh = self.sb(es, "p3_oh", [128, 2, 32], F32)
            ohs, Rohs = self.sb(es, "p3_ohs", [128, 32], F32)
            cum, Rcum = self.sb(es, "p3_cum", [128, 32], F32)
            pos, Rpos = self.sb(es, "p3_pos", [128, 32], F32)
            ovf, Rovf = self.sb(es, "p3_ovf", [128, 32], F32)
            dtmp, Rdtmp = self.sb(es, "p3_dtmp", [128, 2, 32], F32)
            dstf, Rdstf = self.sb(es, "p3_dstf", [128, 2], F32)
            tp, Rtp = self.ps(es, "p3_tp", [128, 8, 128], BF16)
            mx = [self.ps(es, f"p3_mx{j}", [128, 512], F32) for j in range(2)]
            tf = [self.ps(es, f"p3_tf{j}", [128, 4, 128], F32) for j in range(2)]
            plg, Rplg = self.ps(es, "p3_plg", [128, 36], F32)
            prk, Rprk = self.ps(es, "p3_prk", [128, 64], F32)
            P.op("pool", lambda e: e.memset(cum[:], 0.0), writes=[Rcum])

            def loadA(i):
                b = i % 2
                rows = slice(i * 128, (i + 1) * 128)
                P.op("sp", lambda e: e.dma_start(out=xs[b][0][:], in_=xsrc[rows, :]), reads=self.Rxres, writes=[xs[b][1]], dmakey=f"p3x{b}")
                P.op("sp", lambda e: e.dma_start(out=oc[b][0][:], in_=self.ocat[rows, :]), reads=self.Rocat, writes=[oc[b][1]], dmakey=f"p3o{b}")
            loadA(0)
            for i in range(NT):
                def tile3a_(i):
                    b = i % 2
                    rows = slice(i * 128, (i + 1) * 128)
                    if i + 1 < NT:
                        loadA(i + 1)
                    xt, Rx = xs[b]; oct_, Roc = oc[b]; oTt, RoT = oT[b]

                    def tro(e, oct_=oct_):
                        r = None
                        for k in range(8):
                            r = e.transpose(out=tp[:, k, :], in_=oct_[:, k * 128:(k + 1) * 128], identity=self.ident[:])
                        return r
                    P.op("pe", tro, reads=[Roc, self.Rident], writes=[Rtp])
                    P.op("act", lambda e, oTt=oTt: e.activation(out=oTt[:], in_=tp[:], func=AF.Copy), reads=[Rtp], writes=[RoT])
                    for n in range(2):
                        def mm(e, n=n, oTt=oTt):
                            r = None
                            for k in range(8):
                                r = e.matmul(mx[n][0][:], lhsT=oTt[:, k, :], rhs=wo[k][0][:, n * 512:(n + 1) * 512], start=(k == 0), stop=(k == 7))
                            return r
                        P.op("pe", mm, reads=[RoT] + Rwo, writes=[mx[n][1]])
                        P.op("dve", lambda e, n=n: e.tensor_tensor(out=tmp[:, n * 512:(n + 1) * 512], in0=mx[n][0][:], in1=self.modt[:, 2, n * 512:(n + 1) * 512], op=ALU.mult),
                             reads=[mx[n][1], self.Rmodt], writes=[Rtmp])
                    P.op("dve", lambda e, xt=xt: e.tensor_tensor(out=xt[:], in0=xt[:], in1=tmp[:], op=ALU.add), reads=[Rx, Rtmp], writes=[Rx])
                    P.op("sp", lambda e, xt=xt, rows=rows: e.dma_start(out=self.xres[rows, :], in_=xt[:]), reads=[Rx], writes=[self.Rxres[b]], dmakey=f"p3xo{b}")
                    sst, Rss = ss[b]
                    self.rstd_ops(xt[:], Rx, junk, Rjunk, sst, Rss, D)
                    P.op("dve", lambda e, xt=xt, sst=sst: e.scalar_tensor_tensor(out=tmp[:], in0=xt[:], scalar=sst[:, 0:1], in1=self.modt[:, 4, :], op0=ALU.mult, op1=ALU.mult),
                         reads=[Rx, Rss, self.Rmodt], writes=[Rtmp])
                    hf, Rhf = h2f[b]; hbt, Rhb = h2b[b]
                    P.op("dve", lambda e, hf=hf: e.tensor_tensor(out=hf[:], in0=tmp[:], in1=self.modt[:, 3, :], op=ALU.add), reads=[Rtmp, self.Rmodt], writes=[Rhf])
                    P.op("act", lambda e, hf=hf, hbt=hbt: e.activation(out=hbt[:], in_=hf[:], func=AF.Copy), reads=[Rhf], writes=[Rhb])
                    for half in range(2):
                        def trf(e, half=half, hf=hf):
                            r = None
                            for k in range(4):
                                kk = half * 4 + k
                                r = e.transpose(out=tf[half][0][:, k, :], in_=hf[:, kk * 128:(kk + 1) * 128], identity=self.identf[:])
                            return r
                        P.op("pe", trf, reads=[Rhf, self.Ridentf], writes=[tf[half][1]])
                        P.op("dve", lambda e, half=half: e.tensor_copy(out=h2T[:, half * 4:half * 4 + 4, :], in_=tf[half][0][:]), reads=[tf[half][1]], writes=[Rh2T])

                    def mml(e):
                        r = None
                        for k in range(8):
                            r = e.matmul(plg[:], lhsT=h2T[:, k, :], rhs=wr[:, k, :], start=(k == 0), stop=(k == 7))
                        return r
                    P.op("pe", mml, reads=[Rh2T, Rwr], writes=[Rplg])
                    P.op("dve", lambda e: e.tensor_tensor(out=lg[:], in0=plg[:], in1=rb[:], op=ALU.add), reads=[Rplg, Rrb], writes=[Rlg])
                    P.op("dve", lambda e: e.tensor_reduce(out=sm[:, 0:1], in_=lg[:, 0:4], axis=AX.X, op=ALU.max), reads=[Rlg], writes=[Rsm])
                    P.op("dve", lambda e: e.tensor_scalar(out=ohg[:], in0=lg[:, 0:4], scalar1=sm[:, 0:1], scalar2=None, op0=ALU.is_equal), reads=[Rlg, Rsm], writes=[Rohg])
                    P.op("dve", lambda e: e.tensor_scalar(out=sm[:, 1:2], in0=sm[:, 0:1], scalar1=-1.0, scalar2=None, op0=ALU.mult), reads=[Rsm], writes=[Rsm])
                    P.op("act", lambda e: e.activation(out=eg[:], in_=lg[:, 0:4], func=AF.Exp, bias=sm[:, 1:2], accum_out=sm[:, 2:3]), reads=[Rlg, Rsm], writes=[Reg, Rsm])
                    P.op("dve", lambda e: e.reciprocal(out=sm[:, 3:4], in_=sm[:, 2:3]), reads=[Rsm], writes=[Rsm])
                    P.op("dve", lambda e: e.tensor_scalar(out=ohg[:], in0=ohg[:], scalar1=-1.0, scalar2=BIG, op0=ALU.add, op1=ALU.mult), reads=[Rohg], writes=[Rohg])
                    P.op("dve", lambda e: e.tensor_tensor(out=msk[:].rearrange("p (g j) -> p g j", j=8), in0=lg[:, 4:36].rearrange("p (g j) -> p g j", j=8),
                                                          in1=ohg[:].unsqueeze(2).to_broadcast([128, 4, 8]), op=ALU.add), reads=[Rlg, Rohg], writes=[Rmsk])
                    P.op("dve", lambda e: e.tensor_reduce(out=sm[:, 4:5], in_=msk[:], axis=AX.X, op=ALU.max), reads=[Rmsk], writes=[Rsm])
                    P.op("dve", lambda e: e.tensor_scalar(out=oh[:, 0, :], in0=msk[:], scalar1=sm[:, 4:5], scalar2=None, op0=ALU.is_equal), reads=[Rmsk, Rsm], writes=[Roh])
                    P.op("dve", lambda e: e.scalar_tensor_tensor(out=msk[:], in0=oh[:, 0, :], scalar=-BIG, in1=msk[:], op0=ALU.mult, op1=ALU.add), reads=[Roh, Rmsk], writes=[Rmsk])
                    P.op("dve", lambda e: e.tensor_reduce(out=sm[:, 5:6], in_=msk[:], axis=AX.X, op=ALU.max), reads=[Rmsk], writes=[Rsm])
                    P.op("dve", lambda e: e.tensor_scalar(out=oh[:, 1, :], in0=msk[:], scalar1=sm[:, 5:6], scalar2=None, op0=ALU.is_equal), reads=[Rmsk, Rsm], writes=[Roh])
                    P.op("dve", lambda e: e.tensor_tensor(out=sm[:, 6:7], in0=sm[:, 5:6], in1=sm[:, 4:5], op=ALU.subtract), reads=[Rsm], writes=[Rsm])
                    P.op("act", lambda e: e.activation(out=sm[:, 7:8], in_=sm[:, 6:7], func=AF.Exp), reads=[Rsm], writes=[Rsm])
                    P.op("dve", lambda e: e.tensor_scalar(out=sm[:, 8:9], in0=sm[:, 7:8], scalar1=1.0, scalar2=None, op0=ALU.add), reads=[Rsm], writes=[Rsm])
                    P.op("dve", lambda e: e.reciprocal(out=sm[:, 9:10], in_=sm[:, 8:9]), reads=[Rsm], writes=[Rsm])
                    P.op("dve", lambda e: e.tensor_tensor(out=sm[:, 10:11], in0=sm[:, 9:10], in1=sm[:, 7:8], op=ALU.mult), reads=[Rsm], writes=[Rsm])
                    P.op("dve", lambda e, i=i: e.tensor_tensor(out=self.wts[:, i, 0:1], in0=sm[:, 9:10], in1=sm[:, 3:4], op=ALU.mult), reads=[Rsm], writes=[self.Rwts])
                    P.op("dve", lambda e, i=i: e.tensor_tensor(out=self.wts[:, i, 1:2], in0=sm[:, 10:11], in1=sm[:, 3:4], op=ALU.mult), reads=[Rsm], writes=[self.Rwts])
                    P.op("dve", lambda e: e.tensor_tensor(out=ohs[:], in0=oh[:, 0, :], in1=oh[:, 1, :], op=ALU.add), reads=[Roh], writes=[Rohs])

                    def mmr(e):
                        e.matmul(prk[:, 0:32], lhsT=self.triS[:], rhs=ohs[:], start=True, stop=True, skip_group_check=True)
                        return e.matmul(prk[:, 32:64], lhsT=self.ones[:], rhs=ohs[:], start=True, stop=True, skip_group_check=True)
                    P.op("pe", mmr, reads=[Rohs, self.RtriS, self.Rones], writes=[Rprk])
                    P.op("dve", lambda e: e.tensor_tensor(out=pos[:], in0=prk[:, 0:32], in1=cum[:], op=ALU.add), reads=[Rprk, Rcum], writes=[Rpos])
                    P.op("dve", lambda e: e.tensor_tensor(out=cum[:], in0=prk[:, 32:64], in1=cum[:], op=ALU.add), reads=[Rprk, Rcum], writes=[Rcum])
                    P.op("dve", lambda e: e.tensor_scalar(out=ovf[:], in0=pos[:], scalar1=float(CAP), scalar2=1.0e6, op0=ALU.is_ge, op1=ALU.mult), reads=[Rpos], writes=[Rovf])
                    P.op("dve", lambda e: e.tensor_tensor(out=pos[:], in0=pos[:], in1=self.sbase[:], op=ALU.add), reads=[Rpos, self.Rsbase], writes=[Rpos])
                    P.op("dve", lambda e: e.tensor_tensor(out=pos[:], in0=pos[:], in1=ovf[:], op=ALU.add), reads=[Rpos, Rovf], writes=[Rpos])
                    P.op("dve", lambda e: e.tensor_tensor(out=dtmp[:], in0=oh[:], in1=pos[:].unsqueeze(1).to_broadcast([128, 2, 32]), op=ALU.mult), reads=[Roh, Rpos], writes=[Rdtmp])
                    P.op("dve", lambda e: e.tensor_reduce(out=dstf[:], in_=dtmp[:], axis=AX.X, op=ALU.add), reads=[Rdtmp], writes=[Rdstf])
                    P.op("dve", lambda e, i=i: e.tensor_copy(out=self.dest[:, i, :], in_=dstf[:]), reads=[Rdstf], writes=[self.Rdest])
                    for k in range(2):
                        P.op("pool", lambda e, k=k, i=i, hbt=hbt: self.indirect(
                            e, out=self.xsd[:, :], out_offset=bass.IndirectOffsetOnAxis(ap=self.dest[:, i, k:k + 1], axis=0),
                            in_=hbt[:, :], in_offset=None),
                            reads=[Rhb, self.Rdest], writes=[self.Rxsd], dmakey=f"p3sc{k}")
                tile3a_(i)
            NHh = CAP // SEG
            flf, Rflf = self.sb(es, "p3_flf", [128, NE, NHh], F32)
            for hh in range(NHh):
                thr = float(SEG * hh) if COND_THR is None else float(COND_THR * hh)
                P.op("dve", lambda e, hh=hh, thr=thr: e.tensor_scalar(out=flf[:, :, hh], in0=cum[:], scalar1=thr, scalar2=None, op0=ALU.is_gt),
                     reads=[Rcum], writes=[Rflf])
            P.op("dve", lambda e: e.tensor_copy(out=self.flags[:], in_=flf[:].rearrange("p e h -> p (e h)")), reads=[Rflf], writes=[self.Rflags])
            P.flush(self.scr[:])

    def phase3b(self, l):
        nc, P, I = self.nc, self.P, self.ins
        NB = CAP // 128
        NH = CAP // SEG
        BPS = SEG // 128
        with contextlib.ExitStack() as es:
            wg = [[self.sb(es, f"p4_wg{j}_{k}", [128, 512], BF16) for k in range(8)] for j in range(2)]
            wu = [[self.sb(es, f"p4_wu{j}_{k}", [128, 512], BF16) for k in range(8)] for j in range(2)]
            wd = [[self.sb(es, f"p4_wd{j}_{k}", [128, D], BF16) for k in range(4)] for j in range(2)]
            Xt = [self.sb(es, f"p4_X{j}", [128, NB, D], BF16) for j in range(2)]
            XT, RXT = self.sb(es, "p4_XT", [128, 8, CAP], BF16)
            AT, RAT = self.sb(es, "p4_AT", [128, 4, CAP], BF16)
            sg = [self.sb(es, f"p4_sg{j}", [128, 512], F32) for j in range(2)]
            Ys = [self.sb(es, f"p4_Y{j}", [128, D], BF16) for j in range(2)]
            tp = [self.ps(es, f"p4_tp{j}", [128, 8, 128], BF16) for j in range(2)]
            pgt = [self.ps(es, f"p4_pg{j}", [128, 512], F32) for j in range(2)]
            put = [self.ps(es, f"p4_pu{j}", [128, 512], F32) for j in range(2)]
            py = [self.ps(es, f"p4_py{j}", [128, 512], F32) for j in range(2)]
            cnt = dict(tp=0, gu=0, y=0, ys=0)

            def load(e_):
                j = e_ % 2
                P.op("pool", lambda e: [e.dma_start(out=wg[j][k][0][:], in_=I["w_expert_gate"][l, e_, k * 128:(k + 1) * 128, :]) for k in range(8)],
                     writes=[w[1] for w in wg[j]], dmakey=f"p4g{j}", ndma=8)
                P.op("pool", lambda e: [e.dma_start(out=wu[j][k][0][:], in_=I["w_expert_up"][l, e_, k * 128:(k + 1) * 128, :]) for k in range(8)],
                     writes=[w[1] for w in wu[j]], dmakey=f"p4u{j}", ndma=8)
                P.op("pool", lambda e: [e.dma_start(out=wd[j][k][0][:], in_=I["w_expert_down"][l, e_, k * 128:(k + 1) * 128, :]) for k in range(4)],
                     writes=[w[1] for w in wd[j]], dmakey=f"p4d{j}", ndma=4)
                P.op("sp", lambda e: e.dma_start(out=Xt[j][0][:], in_=self.xsd[e_ * CAP:(e_ + 1) * CAP, :].rearrange("(b p) d -> p b d", p=128)),
                     reads=[self.Rxsd], writes=[Xt[j][1]], dmakey=f"p4x{j}")
            load(0)
            for e_ in range(NE):
                def expert_(e_):
                    j = e_ % 2
                    if e_ + 1 < NE:
                        load(e_ + 1)
                    xt, Rx = Xt[j]
                    Rwg = [w[1] for w in wg[j]]; Rwu = [w[1] for w in wu[j]]; Rwd = [w[1] for w in wd[j]]
                    for half_ in range(NH):
                        if half_ > 0 and USE_COND:
                            P.cond_begin(self.flags[0:1, e_ * NH + half_:e_ * NH + half_ + 1])
                        part_(e_, j, xt, Rx, Rwg, Rwu, Rwd, half_)
                        if half_ > 0 and USE_COND:
                            P.cond_end()

                def part_(e_, j, xt, Rx, Rwg, Rwu, Rwd, half_):
                    blks = range(half_ * BPS, half_ * BPS + BPS)
                    for blk in blks:
                        tpt, Rtp = tp[cnt["tp"] % 2]; cnt["tp"] += 1

                        def trx(e, blk=blk, tpt=tpt):
                            r = None
                            for k in range(8):
                                r = e.transpose(out=tpt[:, k, :], in_=xt[:, blk, k * 128:(k + 1) * 128], identity=self.ident[:])
                            return r
                        P.op("pe", trx, reads=[Rx, self.Rident], writes=[Rtp])
                        eng = "dve" if blk % 2 == 0 else "act"
                        if eng == "dve":
                            P.op("dve", lambda e, blk=blk, tpt=tpt: e.tensor_copy(out=XT[:, :, blk * 128:(blk + 1) * 128], in_=tpt[:]), reads=[Rtp], writes=[RXT])
                        else:
                            P.op("act", lambda e, blk=blk, tpt=tpt: e.activation(out=XT[:, :, blk * 128:(blk + 1) * 128], in_=tpt[:], func=AF.Copy), reads=[Rtp], writes=[RXT])
                    for half in (half_,):
                        cs = slice(half * SEG, (half + 1) * SEG)
                        for m in range(4):
                            (g_, Rg), (u_, Ru) = pgt[cnt["gu"] % 2], put[cnt["gu"] % 2]
                            sgt, Rsg = sg[cnt["gu"] % 2]; cnt["gu"] += 1

                            def mmg(e, m=m, cs=cs, g_=g_):
                                r = None
                                for k in range(8):
                                    r = e.matmul(g_[:, 0:SEG], lhsT=wg[j][k][0][:, m * 128:(m + 1) * 128], rhs=XT[:, k, cs], start=(k == 0), stop=(k == 7))
                                return r
                            P.op("pe", mmg, reads=[RXT] + Rwg, writes=[Rg])

                            def mmu(e, m=m, cs=cs, u_=u_):
                                r = None
                                for k in range(8):
                                    r = e.matmul(u_[:, 0:SEG], lhsT=wu[j][k][0][:, m * 128:(m + 1) * 128], rhs=XT[:, k, cs], start=(k == 0), stop=(k == 7))
                                return r
                            P.op("pe", mmu, reads=[RXT] + Rwu, writes=[Ru])
                            P.op("act", lambda e, g_=g_, sgt=sgt: e.activation(out=sgt[:, 0:SEG], in_=g_[:, 0:SEG], func=AF.Silu), reads=[Rg], writes=[Rsg])
                            P.op("dve", lambda e, m=m, cs=cs, u_=u_, sgt=sgt: e.tensor_tensor(out=AT[:, m, cs], in0=sgt[:, 0:SEG], in1=u_[:, 0:SEG], op=ALU.mult),
                                 reads=[Rsg, Ru], writes=[RAT])
                    for blk in blks:
                        yst, Rys = Ys[cnt["ys"] % 2]; yb = cnt["ys"] % 2; cnt["ys"] += 1
                        for n in range(2):
                            y_, Ry = py[cnt["y"] % 2]; cnt["y"] += 1

                            def mmd(e, blk=blk, n=n, y_=y_):
                                r = None
                                for k in range(4):
                                    r = e.matmul(y_[:], lhsT=AT[:, k, blk * 128:(blk + 1) * 128], rhs=wd[j][k][0][:, n * 512:(n + 1) * 512], start=(k == 0), stop=(k == 3))
                                return r
                            P.op("pe", mmd, reads=[RAT] + Rwd, writes=[Ry])
                            if n == 0:
                                P.op("dve", lambda e, n=n, y_=y_, yst=yst: e.tensor_copy(out=yst[:, n * 512:(n + 1) * 512], in_=y_[:]), reads=[Ry], writes=[Rys])
                            else:
                                P.op("act", lambda e, n=n, y_=y_, yst=yst: e.activation(out=yst[:, n * 512:(n + 1) * 512], in_=y_[:], func=AF.Copy), reads=[Ry], writes=[Rys])
                        r0 = e_ * CAP + blk * 128
                        P.op("sp", lambda e, r0=r0, yst=yst: e.dma_start(out=self.ysd[r0:r0 + 128, :], in_=yst[:]), reads=[Rys], writes=[self.Rysd], dmakey=f"p4y{yb}")
                expert_(e_)
            P.flush(self.scr[:])

    def phase_final(self):
        nc, P, I = self.nc, self.P, self.ins
        with contextlib.ExitStack() as es:
            xs = [self.sb(es, f"pf_xs{j}", [128, D], F32) for j in range(2)]
            y0 = [self.sb(es, f"pf_y0{j}", [128, D], BF16) for j in range(2)]
            y1 = [self.sb(es, f"pf_y1{j}", [128, D], BF16) for j in range(2)]
            tmp, Rtmp = self.sb(es, "pf_tmp", [128, D], F32)
            junk, Rjunk = self.sb(es, "pf_junk", [128, D], BF16)
            ss = [self.sb(es, f"pf_ss{j}", [128, 1], F32) for j in range(2)]
            gf, Rgf = self.sb(es, "pf_gf", [128, D], F32)
            P.op("sp", lambda e: e.dma_start(out=gf[:], in_=I["g_final"].partition_broadcast(128)), writes=[Rgf], dmakey="pfg")
            for i in range(NT):
                def tilef_(i):
                    b = i % 2
                    rows = slice(i * 128, (i + 1) * 128)
                    xt, Rx = xs[b]
                    self.combine(i, xt, Rx, y0[b], y1[b], tmp, Rtmp, self.modt[:, 5, :], self.Rmodt, f"pfx{b}", f"pfy{b}")
                    sst, Rss = ss[b]
                    self.rstd_ops(xt[:], Rx, junk, Rjunk, sst, Rss, D)
                    P.op("dve", lambda e, xt=xt, sst=sst: e.scalar_tensor_tensor(out=xt[:], in0=xt[:], scalar=sst[:, 0:1], in1=gf[:], op0=ALU.mult, op1=ALU.mult),
                         reads=[Rx, Rss, Rgf], writes=[Rx])
                    P.op("sp", lambda e, xt=xt, rows=rows: e.dma_start(out=self.y[rows, :], in_=xt[:]), reads=[Rx], writes=[self.Ry], dmakey=f"pfo{b}")
                tilef_(i)
            P.flush(self.scr[:])


def _layout_inputs(inputs, b):
    m = {}
    m["x"] = np.ascontiguousarray(inputs["x"][b])
    m["cT"] = np.ascontiguousarray(inputs["c"][b].reshape(8, 128).T)
    m["pos"] = np.ascontiguousarray(inputs["positions"][b].reshape(NT, 128).T).astype(np.int32)
    for k in ("w_ada", "b_ada", "g_mix", "w_in", "b_forget", "lambda_q1", "lambda_k1", "lambda_q2", "lambda_k2",
              "g_subln", "g_fox_out", "w_out", "g_ffn", "w_router_group", "b_router_group", "w_router_expert",
              "b_router_expert", "w_expert_gate", "w_expert_up", "w_expert_down", "g_final"):
        m[k] = np.ascontiguousarray(inputs[k])
    return m


def kernel(**inputs):
    kb = K()
    in_maps = [_layout_inputs(inputs, b) for b in range(4)]
    res = run_bass_kernel_spmd(kb.nc, in_maps, core_ids=list(range(4)))
    return np.stack([np.asarray(r["y"]) for r in res.results], axis=0).astype(np.float32)
```

```python
import math, contextlib
import numpy as np
import concourse.bass as bass
import concourse.mybir as mybir
from concourse.bass_utils import run_bass_kernel_spmd

F32 = mybir.dt.float32
BF16 = mybir.dt.bfloat16
I32 = mybir.dt.int32
AF = mybir.ActivationFunctionType
ALU = mybir.AluOpType
AX = mybir.AxisListType

MAXOPS = None
NLIN = 4
USE_COND = True
COND_THR = None
ENGS = ("pe", "act", "dve", "pool", "sp")


class Res:
    __slots__ = ("name", "w", "r", "excl")

    def __init__(self, name, w=None, excl=False):
        self.name = name
        self.w = w
        self.r = []
        self.excl = excl


class Prog:
    def __init__(self, nc):
        self.nc = nc
        self.ops = []
        self.es = contextlib.ExitStack()
        self.engsem = {e: self.es.enter_context(nc.semaphore("s_" + e)) for e in ENGS}
        self.keysem = {}
        self.semByName = {}
        self.cnt = {}
        self.known = {e: {} for e in ENGS}
        self.lastkey = {}
        self.allres = []
        self.barrier_op = None
        self.nflush = 0
        self.ninstr = 0
        self.cur_cond = None
        self.ncond = 0

    def res(self, name, excl=False):
        r = Res(name, self.barrier_op, excl)
        self.allres.append(r)
        return r

    def op(self, eng, fn, reads=(), writes=(), dmakey=None, ndma=1):
        self.total_ops = getattr(self, "total_ops", 0) + 1
        if MAXOPS is not None and self.total_ops > MAXOPS:
            return None
        o = dict(eng=eng, fn=fn, reads=list(reads), writes=list(writes), dmakey=dmakey, ndma=ndma,
                 deps=[], sig=False, done=False, cond=self.cur_cond)
        self.ops.append(o)
        return o

    def cond_begin(self, flag_ap):
        self.ncond += 1
        self.cur_cond = (self.ncond, flag_ap)

    def cond_end(self):
        self.cur_cond = None

    def chain(self, eng, fns, reads=(), writes=()):
        for f in fns:
            self.op(eng, f, reads=reads, writes=writes)

    def flush(self, scratch):
        nc = self.nc
        global MAXOPS
        _mo = MAXOPS
        MAXOPS = None
        bar = self.op("pool", lambda e: e.memset(scratch, 0.0), writes=[self.scratch_res])
        MAXOPS = _mo
        bar["isbar"] = True
        ops = self.ops
        for o in ops:
            deps = {}
            if o.get("isbar"):
                for r in self.allres:
                    if r.w is not None:
                        deps[id(r.w)] = r.w
                    for q in r.r:
                        deps[id(q)] = q
            for r in o["reads"]:
                if r.w is not None:
                    deps[id(r.w)] = r.w
                if r.excl:
                    for q in r.r:
                        if q["eng"] != o["eng"]:
                            deps[id(q)] = q
            for w in o["writes"]:
                if w.w is not None:
                    deps[id(w.w)] = w.w
                for q in w.r:
                    deps[id(q)] = q
            k = o["dmakey"]
            if k is not None:
                if k in self.lastkey:
                    p = self.lastkey[k]
                    deps[id(p)] = p
                self.lastkey[k] = o
            deps.pop(id(o), None)
            o["deps"] = list(deps.values())
            for r in o["reads"]:
                r.r.append(o)
            for w in o["writes"]:
                w.w = o
                w.r = []
        for o in ops:
            for p in o["deps"]:
                if p["eng"] == "pe" and o["eng"] == "pe" and p["dmakey"] is None:
                    continue
                p["sig"] = True
        bar["sig"] = True
        for o in ops:
            k = o["dmakey"]
            if k is not None:
                if k not in self.keysem:
                    pref = "q" if o["eng"] == "pool" else "d"
                    n_ = sum(1 for v in self.keysem.values() if v[0] == pref)
                    name = f"{pref}_{n_}"
                    if name not in self.semByName:
                        self.semByName[name] = self.es.enter_context(nc.semaphore(name))
                    self.keysem[k] = (pref, name)
                else:
                    assert (self.keysem[k][0] == "q") == (o["eng"] == "pool"), k
                o["semname"] = self.keysem[k][1]
                o["sem"] = self.semByName[o["semname"]]
                self.cnt[o["semname"]] = self.cnt.get(o["semname"], 0) + 16 * o["ndma"]
                o["val"] = self.cnt[o["semname"]]
            else:
                o["sem"] = self.engsem[o["eng"]]
                o["semname"] = "s_" + o["eng"]
                if o["sig"]:
                    self.cnt[o["semname"]] = self.cnt.get(o["semname"], 0) + 1
                    o["val"] = self.cnt[o["semname"]]
        per = {e: [] for e in ENGS}
        for o in ops:
            per[o["eng"]].append(o)

        def emit_op(engname, eng, o, known):
            need = {}
            for p in o["deps"]:
                if p["eng"] == "pe" and engname == "pe" and p["dmakey"] is None:
                    continue
                sn = p["semname"]
                if p["val"] > need.get(sn, (None, 0))[1]:
                    need[sn] = (p["sem"], p["val"])
            for sn, (sem, val) in need.items():
                if known.get(sn, 0) >= val:
                    continue
                eng.wait_ge(sem, val)
                known[sn] = val
                self.ninstr += 1
            res = o["fn"](eng)
            if o["dmakey"] is not None:
                lst = res if isinstance(res, (list, tuple)) else [res]
                assert len(lst) == o["ndma"], (len(lst), o["ndma"], o["dmakey"])
                for ins in lst:
                    ins.then_inc(o["sem"], 16)
            elif o["sig"]:
                ins = res[-1] if isinstance(res, (list, tuple)) else res
                ins.then_inc(o["sem"], 1)

        def run(engname, eng):
            known = self.known[engname]
            lst = per[engname]
            i = 0
            while i < len(lst):
                o = lst[i]
                if o["cond"] is None:
                    emit_op(engname, eng, o, known)
                    i += 1
                    continue
                cid = o["cond"]
                j = i
                while j < len(lst) and lst[j]["cond"] is cid:
                    j += 1
                region = lst[i:j]
                incs = {}
                for o2 in region:
                    n_ = 16 * o2["ndma"] if o2["dmakey"] is not None else (1 if o2["sig"] else 0)
                    if n_:
                        sn = o2["semname"]
                        if sn not in incs:
                            incs[sn] = [o2["sem"], o2["val"] - n_, 0]
                        incs[sn][2] += n_
                self._nreg = getattr(self, "_nreg", 0) + 1
                reg = eng.alloc_register(f"cf{self._nreg}")
                eng.reg_load(reg, cid[1])
                saved = dict(known)
                with eng.If_eq(reg, 1):
                    for o2 in region:
                        emit_op(engname, eng, o2, known)
                with eng.Else():
                    for sn, (sem, base, tot) in incs.items():
                        if base > 0:
                            eng.wait_ge(sem, base)
                        eng.sem_inc(sem, tot)
                eng.free_register(reg)
                known.clear()
                known.update(saved)
                i = j

        with nc.Block() as block:
            @block.tensor
            def _(e):
                run("pe", e)

            @block.scalar
            def _(e):
                run("act", e)

            @block.vector
            def _(e):
                run("dve", e)

            @block.gpsimd
            def _(e):
                run("pool", e)

            @block.sync
            def _(e):
                run("sp", e)
        for r in self.allres:
            r.w = bar
            r.r = []
        pass
        self.barrier_op = bar
        self.lastkey = {}
        self.keysem = {}
        self.ops = []
        self.nflush += 1

    def finish(self, scratch):
        self.flush(scratch)
        nc = self.nc
        fin = [(sem, self.cnt[name]) for name, sem in self.semByName.items()]
        bar = self.barrier_op
        with nc.Block() as block:
            @block.sync
            def _(e):
                e.wait_ge(bar["sem"], bar["val"])
                for sem, val in fin:
                    e.wait_ge(sem, val)
        self.es.close()

NT = 32
T = NT * 128
D = 1024
NL = 4
NE = 32
CAP = 1536
SEG = 256
NSLOT = NE * CAP
INC = 3080
EPS = 1e-6
NEGM = -240000.0
TWO_PI = 2.0 * math.pi
MAGIC = 12582912.0
INV_FREQ = [500000.0 ** (-(2 * i) / 16.0) for i in range(8)]


class K:
    def __init__(self, nlayers=NL, dbg=False, stop=None):
        self.nl = nlayers
        self.dbg = dbg
        self.stop = stop
        nc = self.nc = bass.Bass("TRN2", target_bir_lowering=False)
        self.P = Prog(nc)
        self.pes = contextlib.ExitStack()
        self.ins = {}
        self.dr = {}
        self.build()

    def din(self, name, shape, dt=F32):
        self.ins[name] = self.nc.dram_tensor(name, list(shape), dt, kind="ExternalInput").ap()
        return self.ins[name]

    def dscr(self, name, shape, dt):
        kind = "ExternalOutput" if self.dbg else "Internal"
        t = self.nc.dram_tensor(name, list(shape), dt, kind=kind).ap()
        self.dr[name] = t
        return t

    def sb(self, es, name, shape, dt):
        self._uid = getattr(self, "_uid", 0) + 1
        name = f"{name}_u{self._uid}"
        t = es.enter_context(self.nc.sbuf_tensor(name, list(shape), dt))
        return t, self.P.res(name)

    def ps(self, es, name, shape, dt):
        self._uid = getattr(self, "_uid", 0) + 1
        name = f"{name}_u{self._uid}"
        n = 1
        for d_ in shape[1:]:
            n *= d_
        full = 512 if dt == F32 else 1024
        assert n <= full
        if n == full:
            t = es.enter_context(self.nc.psum_tensor(name, list(shape), dt))
        else:
            assert len(shape) == 2
            tb = es.enter_context(self.nc.psum_tensor(name, [shape[0], full], dt))
            t = tb[:, 0:shape[1]]
        return t, self.P.res(name, excl=True)

    def indirect(self, e, **kw):
        self._nreg = getattr(self, "_nreg", 0) + 1
        reg = e.alloc_register(f"bnd{self._nreg}")
        e.reg_mov(reg, NSLOT - 1)
        r = e.indirect_dma_start(bounds_check=reg, oob_is_err=False, **kw)
        e.free_register(reg)
        return r

    def dump(self, name, ap, res, shape, dt):
        if not self.dbg:
            return
        t = self.nc.dram_tensor("dbg_" + name, list(shape), dt, kind="ExternalOutput").ap()
        self.P.op("sp", lambda e: e.dma_start(out=t, in_=ap), reads=[res], dmakey="dbg_" + name)

    def build(self):
        nc, P = self.nc, self.P
        I = self.ins
        self.din("x", [T, D]); self.din("cT", [128, 8]); self.din("pos", [128, NT], I32)
        self.din("w_ada", [NLIN, D, 6 * D]); self.din("b_ada", [NLIN, 6 * D]); self.din("g_mix", [NLIN, D])
        self.din("w_in", [NLIN, D, INC]); self.din("b_forget", [NLIN, 8])
        for n in ("lambda_q1", "lambda_k1", "lambda_q2", "lambda_k2"):
            self.din(n, [NLIN, 64])
        self.din("g_subln", [NLIN, 128]); self.din("g_fox_out", [NLIN, 64]); self.din("w_out", [NLIN, D, D])
        self.din("g_ffn", [NLIN, D]); self.din("w_router_group", [NLIN, D, 4]); self.din("b_router_group", [NLIN, 4])
        self.din("w_router_expert", [NLIN, D, 32]); self.din("b_router_expert", [NLIN, 32])
        self.din("w_expert_gate", [NLIN, NE, D, 512]); self.din("w_expert_up", [NLIN, NE, D, 512])
        self.din("w_expert_down", [NLIN, NE, 512, D]); self.din("g_final", [D])
        self.y = nc.dram_tensor("y", [T, D], F32, kind="ExternalOutput").ap()
        self.Ry = P.res("y")
        self.xres = self.dscr("xres", [T, D], F32); self.Rxres = [P.res("xres0"), P.res("xres1")]
        self.qktd = self.dscr("qktd", [8, 128, T], BF16); self.Rqktd = [P.res("qktd0"), P.res("qktd1")]
        self.ktf = self.dscr("ktf", [4, 128, T], BF16); self.Rktf = [P.res("ktf0"), P.res("ktf1")]
        self.qtf = self.dscr("qtf", [8, 67, T], BF16); self.Rqtf = [P.res("qtf0"), P.res("qtf1")]
        self.vd = self.dscr("vd", [T, 512], BF16); self.Rvd = [P.res("vd0"), P.res("vd1")]
        self.vf = self.dscr("vf", [T, 512], BF16); self.Rvf = [P.res("vf0"), P.res("vf1")]
        self.ocat = self.dscr("ocat", [T, D], BF16); self.Rocat = [P.res("ocat0"), P.res("ocat1")]
        self.xsd = self.dscr("xsd", [NSLOT, D], BF16); self.Rxsd = P.res("xsd")
        self.ysd = self.dscr("ysd", [NSLOT, D], BF16); self.Rysd = P.res("ysd")
        self.setup()
        for l in range(self.nl):
            self.phase0(l)
            if l == 0:
                self.dump("modt", self.modt[:], self.Rmodt, [128, 6, D], F32)
                self.dump("cosT", self.cosT[:], self.RcosT, [128, NT, 8], F32)
                self.dump("sinT", self.sinT[:], self.RsinT, [128, NT, 8], F32)
                self.dump("neglam", self.neglam[:], self.Rneglam, [128, 1], F32)
            if self.stop == ("p0", l): break
            self.phase1(l)
            if self.stop == ("p1", l): break
            self.phase2(l)
            if self.stop == ("p2", l): break
            self.phase3a(l)
            if self.stop == ("p3a", l): break
            self.phase3b(l)
            if self.stop == ("p3b", l): break
        else:
            self.phase_final()
        P.finish(self.scr[:])
        self.pes.close()

    def setup(self):
        nc, P, I = self.nc, self.P, self.ins
        pes = self.pes
        self.scr, P.scratch_res = self.sb(pes, "scr", [128, 8], F32)
        self.ident, self.Rident = self.sb(pes, "ident", [128, 128], BF16)
        self.identf, self.Ridentf = self.sb(pes, "identf", [128, 128], F32)
        self.triU, self.RtriU = self.sb(pes, "triU", [128, 128], F32)
        self.triS, self.RtriS = self.sb(pes, "triS", [128, 128], F32)
        self.e127, self.Re127 = self.sb(pes, "e127", [128, 128], F32)
        self.ones, self.Rones = self.sb(pes, "ones", [128, 128], F32)
        self.maskF, self.RmaskF = self.sb(pes, "maskF", [128, 128], BF16)
        self.maskD, self.RmaskD = self.sb(pes, "maskD", [128, 128], BF16)
        self.cosT, self.RcosT = self.sb(pes, "cosT", [128, NT, 8], F32)
        self.sinT, self.RsinT = self.sb(pes, "sinT", [128, NT, 8], F32)
        self.condB, self.RcondB = self.sb(pes, "condB", [128, 8, 128], F32)
        self.modt, self.Rmodt = self.sb(pes, "modt", [128, 6, D], F32)
        self.ncf, self.Rncf = self.sb(pes, "ncf", [128, NT, 8], F32)
        self.dest, self.Rdest = self.sb(pes, "dest", [128, NT, 2], I32)
        self.wts, self.Rwts = self.sb(pes, "wts", [128, NT, 2], F32)
        self.sbase, self.Rsbase = self.sb(pes, "sbase", [128, NE], F32)
        self.neglam, self.Rneglam = self.sb(pes, "neglam", [128, 1], F32)
        self.gt2p, self.Rgt2p = self.sb(pes, "gt2p", [128, D], F32)
        self.flags, self.Rflags = self.sb(pes, "flags", [128, NE * (CAP // SEG)], I32)
        with contextlib.ExitStack() as es:
            tmpf, Rtmpf = self.sb(es, "su_tmpf", [128, 128], F32)
            posi, Rposi = self.sb(es, "su_posi", [128, NT], I32)
            posf, Rposf = self.sb(es, "su_posf", [128, NT], F32)
            ang, Rang = self.sb(es, "su_ang", [128, NT, 8], F32)
            a2, Ra2 = self.sb(es, "su_a2", [128, NT, 8], F32)
            kk, Rkk = self.sb(es, "su_kk", [128, NT, 8], F32)
            cs, Rcs = self.sb(es, "su_cs", [128, 8], F32)

            W = [self.Rident, self.Ridentf, self.RtriU, self.RtriS, self.Re127, self.Rones,
                 self.RmaskF, self.RmaskD, self.Rsbase, Rtmpf, P.scratch_res]
            fns = [
                lambda e: e.memset(self.identf[:], 0.0),
                lambda e: e.affine_select(out=self.identf[:], in_=self.identf[:], pattern=[[-1, 128]], compare_op=ALU.not_equal,
                                          fill=1.0, base=0, channel_multiplier=1),
                lambda e: e.tensor_copy(out=self.ident[:], in_=self.identf[:]),
                lambda e: e.memset(self.triU[:], 1.0),
                lambda e: e.affine_select(out=self.triU[:], in_=self.triU[:], pattern=[[1, 128]], compare_op=ALU.is_ge,
                                          fill=0.0, base=0, channel_multiplier=-1),
                lambda e: e.memset(self.triS[:], 1.0),
                lambda e: e.affine_select(out=self.triS[:], in_=self.triS[:], pattern=[[1, 128]], compare_op=ALU.is_gt,
                                          fill=0.0, base=0, channel_multiplier=-1),
                lambda e: e.memset(self.e127[:], 0.0),
                lambda e: e.affine_select(out=self.e127[:], in_=self.e127[:], pattern=[[0, 128]], compare_op=ALU.not_equal,
                                          fill=1.0, base=-127, channel_multiplier=1),
                lambda e: e.memset(self.ones[:], 1.0),
                lambda e: e.memset(tmpf[:], 0.0),
                lambda e: e.affine_select(out=tmpf[:], in_=tmpf[:], pattern=[[1, 128]], compare_op=ALU.is_ge,
                                          fill=NEGM, base=0, channel_multiplier=-1),
                lambda e: e.tensor_copy(out=self.maskF[:], in_=tmpf[:]),
                lambda e: e.memset(self.maskD[:], 0.0),
                lambda e: e.memset(self.maskD[64:128, 0:64], NEGM),
                lambda e: e.memset(self.scr[:], 0.0),
            ]
            for j in range(NE):
                fns.append(lambda e, j=j: e.memset(self.sbase[:, j:j + 1], float(j * CAP)))
            P.chain("pool", fns, writes=W)
            zt, Rzt = self.sb(es, "su_zt", [128, 8192], BF16)
            P.op("pool", lambda e: e.memset(zt[:], 0.0), writes=[Rzt])
            nz = NSLOT // 1024 if self.dbg else 0
            P.op("sp", lambda e: [e.dma_start(out=self.xsd[c * 1024:(c + 1) * 1024, :].rearrange("(p a) d -> p (a d)", p=128), in_=zt[:]) for c in range(nz)],
                 reads=[Rzt], writes=[self.Rxsd], dmakey="su0", ndma=nz) if nz else None
            P.op("sp", lambda e: [e.dma_start(out=self.ysd[c * 1024:(c + 1) * 1024, :].rearrange("(p a) d -> p (a d)", p=128), in_=zt[:]) for c in range(nz)],
                 reads=[Rzt], writes=[self.Rysd], dmakey="su0y", ndma=nz) if nz else None
            P.op("sp", lambda e: e.dma_start(out=posi[:], in_=I["pos"]), writes=[Rposi], dmakey="su1")
            P.op("sp", lambda e: e.dma_start(out=cs[:], in_=I["cT"]), writes=[Rcs], dmakey="su2")

            P.op("dve", lambda e: e.tensor_copy(out=posf[:], in_=posi[:]), reads=[Rposi], writes=[Rposf])

            def angles(e):
                r = None
                for i in range(8):
                    r = e.tensor_scalar(out=ang[:, :, i], in0=posf[:], scalar1=float(INV_FREQ[i]), scalar2=None, op0=ALU.mult)
                return r
            P.op("dve", angles, reads=[Rposf], writes=[Rang])

            def reduce_sin(dst, Rdst, shift):
                P.op("dve", lambda e: e.tensor_scalar(out=a2[:], in0=ang[:], scalar1=float(shift), scalar2=None, op0=ALU.add),
                     reads=[Rang], writes=[Ra2])
                P.op("dve", lambda e: e.tensor_scalar(out=kk[:], in0=a2[:], scalar1=1.0 / TWO_PI, scalar2=MAGIC, op0=ALU.mult, op1=ALU.add),
                     reads=[Ra2], writes=[Rkk])
                P.op("dve", lambda e: e.tensor_scalar(out=kk[:], in0=kk[:], scalar1=MAGIC, scalar2=None, op0=ALU.subtract),
                     reads=[Rkk], writes=[Rkk])
                P.op("dve", lambda e: e.scalar_tensor_tensor(out=a2[:], in0=kk[:], scalar=-TWO_PI, in1=a2[:], op0=ALU.mult, op1=ALU.add),
                     reads=[Rkk, Ra2], writes=[Ra2])
                P.op("dve", lambda e: e.tensor_scalar(out=kk[:], in0=a2[:], scalar1=math.pi, scalar2=-TWO_PI, op0=ALU.is_gt, op1=ALU.mult),
                     reads=[Ra2], writes=[Rkk])
                P.op("dve", lambda e: e.tensor_tensor(out=a2[:], in0=a2[:], in1=kk[:], op=ALU.add), reads=[Ra2, Rkk], writes=[Ra2])
                P.op("dve", lambda e: e.tensor_scalar(out=kk[:], in0=a2[:], scalar1=-math.pi, scalar2=TWO_PI, op0=ALU.is_lt, op1=ALU.mult),
                     reads=[Ra2], writes=[Rkk])
                P.op("dve", lambda e: e.tensor_tensor(out=a2[:], in0=a2[:], in1=kk[:], op=ALU.add), reads=[Ra2, Rkk], writes=[Ra2])
                P.op("dve", lambda e: e.tensor_scalar(out=a2[:], in0=a2[:], scalar1=math.pi, scalar2=-math.pi, op0=ALU.min, op1=ALU.max),
                     reads=[Ra2], writes=[Ra2])
                P.op("act", lambda e: e.activation(out=dst[:], in_=a2[:], func=AF.Sin), reads=[Ra2], writes=[Rdst])
            reduce_sin(self.sinT, self.RsinT, 0.0)
            reduce_sin(self.cosT, self.RcosT, math.pi / 2)
            P.op("act", lambda e: e.activation(out=cs[:], in_=cs[:], func=AF.Silu), reads=[Rcs], writes=[Rcs])
            P.op("dve", lambda e: e.tensor_copy(out=self.condB[:], in_=cs[:].unsqueeze(2).to_broadcast([128, 8, 128])),
                 reads=[Rcs], writes=[self.RcondB])
            P.flush(self.scr[:])

    def phase0(self, l):
        nc, P, I = self.nc, self.P, self.ins
        with contextlib.ExitStack() as es:
            wa = [self.sb(es, f"p0_wa{j}", [128, 8, 512], F32) for j in range(2)]
            bb = [self.sb(es, f"p0_bb{j}", [128, 512], F32) for j in range(2)]
            pm = [self.ps(es, f"p0_pm{j}", [128, 512], F32) for j in range(2)]
            gb, Rgb = self.sb(es, "p0_gb", [128, D], F32)
            lam4, Rlam4 = self.sb(es, "p0_lam", [128, 4, 64], F32)
            ltmp, Rltmp = self.sb(es, "p0_ltmp", [128, 2, 64], F32)
            ls, Rls = self.sb(es, "p0_ls", [128, 2], F32)
            if l > 0:
                P.op("pool", lambda e: e.tensor_copy(out=self.gt2p[:], in_=self.modt[:, 5, :]), reads=[self.Rmodt], writes=[self.Rgt2p])
            for g in range(12):
                def grp0_(g):
                    j = g % 2
                    (wt, Rw), (bt, Rb), (pt, Rp) = wa[j], bb[j], pm[j]
                    src = I["w_ada"][l, :, g * 512:(g + 1) * 512].rearrange("(k p) n -> p k n", p=128)
                    P.op("sp", lambda e, wt=wt, src=src: e.dma_start(out=wt[:], in_=src), writes=[Rw], dmakey=f"p0w{j}")
                    bsrc = I["b_ada"][l, g * 512:(g + 1) * 512].partition_broadcast(128)
                    P.op("sp", lambda e, bt=bt, bsrc=bsrc: e.dma_start(out=bt[:], in_=bsrc), writes=[Rb], dmakey=f"p0b{j}")

                    def mm(e, wt=wt, pt=pt):
                        r = None
                        for k in range(8):
                            r = e.matmul(pt[:], lhsT=self.condB[:, k, :], rhs=wt[:, k, :], start=(k == 0), stop=(k == 7))
                        return r
                    P.op("pe", mm, reads=[Rw, self.RcondB], writes=[Rp])
                    dst = self.modt[:, g // 2, (g % 2) * 512:(g % 2 + 1) * 512]
                    P.op("dve", lambda e, dst=dst, pt=pt, bt=bt: e.tensor_tensor(out=dst, in0=pt[:], in1=bt[:], op=ALU.add),
                         reads=[Rp, Rb], writes=[self.Rmodt])
                grp0_(g)
            for (gname, idx) in (("g_mix", 1), ("g_ffn", 4)):
                gsrc = I[gname][l, :].partition_broadcast(128)
                P.op("sp", lambda e, gsrc=gsrc: e.dma_start(out=gb[:], in_=gsrc), writes=[Rgb], dmakey="p0g")
                P.op("dve", lambda e, idx=idx: e.scalar_tensor_tensor(out=self.modt[:, idx, :], in0=self.modt[:, idx, :], scalar=1.0,
                                                                      in1=gb[:], op0=ALU.add, op1=ALU.mult),
                     reads=[Rgb, self.Rmodt], writes=[self.Rmodt])
            for qi, n in enumerate(("lambda_q1", "lambda_k1", "lambda_q2", "lambda_k2")):
                lsrc = I[n][l, :].partition_broadcast(128)
                P.op("sp", lambda e, qi=qi, lsrc=lsrc: e.dma_start(out=lam4[:, qi, :], in_=lsrc), writes=[Rlam4], dmakey=f"p0l{qi}")
            lam_init = 0.8 - 0.6 * math.exp(-0.3 * l)

            def lamf(e):
                e.tensor_tensor(out=ltmp[:, 0, :], in0=lam4[:, 0, :], in1=lam4[:, 1, :], op=ALU.mult)
                return e.tensor_tensor(out=ltmp[:, 1, :], in0=lam4[:, 2, :], in1=lam4[:, 3, :], op=ALU.mult)
            P.op("dve", lamf, reads=[Rlam4], writes=[Rltmp])
            P.op("dve", lambda e: e.tensor_reduce(out=ls[:], in_=ltmp[:], axis=AX.X, op=ALU.add), reads=[Rltmp], writes=[Rls])
            P.op("act", lambda e: e.activation(out=ls[:], in_=ls[:], func=AF.Exp), reads=[Rls], writes=[Rls])
            P.op("dve", lambda e: e.scalar_tensor_tensor(out=self.neglam[:], in0=ls[:, 1:2], scalar=-lam_init, in1=ls[:, 0:1],
                                                         op0=ALU.add, op1=ALU.subtract),
                 reads=[Rls], writes=[self.Rneglam])
            P.flush(self.scr[:])

    def rstd_ops(self, xt, Rx, junk, Rjunk, ss, Rss, n):
        P = self.P
        P.op("act", lambda e: e.activation(out=junk[:], in_=xt, func=AF.Square, accum_out=ss[:]), reads=[Rx], writes=[Rjunk, Rss])
        P.op("act", lambda e: e.activation(out=ss[:], in_=ss[:], func=AF.Ln, scale=1.0 / n, bias=EPS), reads=[Rss], writes=[Rss])
        P.op("act", lambda e: e.activation(out=ss[:], in_=ss[:], func=AF.Exp, scale=-0.5), reads=[Rss], writes=[Rss])

    def phase1(self, l):
        nc, P, I = self.nc, self.P, self.ins
        with contextlib.ExitStack() as es:
            win = [self.sb(es, f"p1_win{k}", [128, INC], BF16) for k in range(8)]
            Rwin = [w[1] for w in win]
            P.op("pool", lambda e: [e.dma_start(out=win[k][0][:], in_=I["w_in"][l, k * 128:(k + 1) * 128, :]) for k in range(8)],
                 writes=Rwin, dmakey="p1win", ndma=8)
            bfb, Rbfb = self.sb(es, "p1_bfb", [128, 8], F32)
            P.op("sp", lambda e: e.dma_start(out=bfb[:], in_=I["b_forget"][l, :].partition_broadcast(128)), writes=[Rbfb], dmakey="p1bfb")
            xs = [self.sb(es, f"p1_xs{j}", [128, D], F32) for j in range(2)]
            tmp, Rtmp = self.sb(es, "p1_tmp", [128, D], F32)
            junk, Rjunk = self.sb(es, "p1_junk", [128, D], BF16)
            hb = [self.sb(es, f"p1_hb{j}", [128, D], BF16) for j in range(2)]
            hT = [self.sb(es, f"p1_hT{j}", [128, 8, 128], BF16) for j in range(2)]
            qk = [self.sb(es, f"p1_qk{j}", [128, 1024], BF16) for j in range(2)]
            vds = [self.sb(es, f"p1_vds{j}", [128, 512], BF16) for j in range(2)]
            vfs = [self.sb(es, f"p1_vfs{j}", [128, 512], BF16) for j in range(2)]
            kf = [self.sb(es, f"p1_kf{j}", [128, 512], BF16) for j in range(2)]
            qfa = [self.sb(es, f"p1_qfa{j}", [128, 8, 67], BF16) for j in range(2)]
            stqk = [self.sb(es, f"p1_stqk{j}", [128, 8, 128], BF16) for j in range(2)]
            stkf = [self.sb(es, f"p1_stkf{j}", [128, 4, 128], BF16) for j in range(2)]
            stqf = [self.sb(es, f"p1_stqf{j}", [128, 8, 128], BF16) for j in range(2)]
            ss = [self.sb(es, f"p1_ss{j}", [128, 1], F32) for j in range(2)]
            rt, Rrt = self.sb(es, "p1_rt", [128, 4, 8, 8], F32)
            fz, Rfz = self.sb(es, "p1_fz", [128, 8], F32)
            fnz, Rfnz = self.sb(es, "p1_fnz", [128, 8], F32)
            fa, Rfa = self.sb(es, "p1_fa", [128, 8], F32)
            fl, Rfl = self.sb(es, "p1_fl", [128, 8], F32)
            cf = [self.sb(es, f"p1_cf{j}", [128, 8], F32) for j in range(2)]
            g8, Rg8 = self.sb(es, "p1_g8", [128, 8], F32)
            r1, Rr1 = self.sb(es, "p1_r1", [128, 8], F32)
            if l > 0:
                y0 = [self.sb(es, f"p1_y0{j}", [128, D], BF16) for j in range(2)]
                y1 = [self.sb(es, f"p1_y1{j}", [128, D], BF16) for j in range(2)]
            tp = [self.ps(es, f"p1_tp{j}", [128, 8, 128], BF16) for j in range(2)]
            pg = [self.ps(es, f"p1_pg{j}", [128, 512], F32) for j in range(3)]
            pff, Rpff = self.ps(es, "p1_pff", [128, 8], F32)
            pcs, Rpcs = self.ps(es, "p1_pcs", [128, 8], F32)
            tpi = [0]
            pgi = [0]

            def stageA(i):
                b = i % 2
                xt, Rx = xs[b]
                rows = slice(i * 128, (i + 1) * 128)
                if l == 0:
                    P.op("sp", lambda e: e.dma_start(out=xt[:], in_=I["x"][rows, :]), writes=[Rx], dmakey=f"p1x{b}")
                    return
                self.combine(i, xt, Rx, y0[b], y1[b], tmp, Rtmp, self.gt2p[:], self.Rgt2p, f"p1x{b}", f"p1y{b}")
                P.op("sp", lambda e: e.dma_start(out=self.xres[rows, :], in_=xt[:]), reads=[Rx], writes=[self.Rxres[b]], dmakey=f"p1xo{b}")

            stageA(0)
            for i in range(NT):
                def tile1_(i):
                    b = i % 2
                    if i + 1 < NT:
                        stageA(i + 1)
                    xt, Rx = xs[b]
                    rows = slice(i * 128, (i + 1) * 128)
                    sst, Rss = ss[b]
                    self.rstd_ops(xt[:], Rx, junk, Rjunk, sst, Rss, D)
                    P.op("dve", lambda e, xt=xt, sst=sst: e.scalar_tensor_tensor(out=tmp[:], in0=xt[:], scalar=sst[:, 0:1], in1=self.modt[:, 1, :],
                                                                               op0=ALU.mult, op1=ALU.mult),
                         reads=[Rx, Rss, self.Rmodt], writes=[Rtmp])
                    hbt, Rhb = hb[b]
                    P.op("dve", lambda e, hbt=hbt: e.tensor_tensor(out=hbt[:], in0=tmp[:], in1=self.modt[:, 0, :], op=ALU.add),
                         reads=[Rtmp, self.Rmodt], writes=[Rhb])
                    tpt, Rtp = tp[tpi[0] % 2]; tpi[0] += 1
                    hTt, RhT = hT[b]

                    def trh(e, hbt=hbt, tpt=tpt):
                        r = None
                        for k in range(8):
                            r = e.transpose(out=tpt[:, k, :], in_=hbt[:, k * 128:(k + 1) * 128], identity=self.ident[:])
                        return r
                    P.op("pe", trh, reads=[Rhb, self.Rident], writes=[Rtp])
                    P.op("act", lambda e, hTt=hTt, tpt=tpt: e.activation(out=hTt[:], in_=tpt[:], func=AF.Copy), reads=[Rtp], writes=[RhT])
                    qkt, Rqk = qk[b]
                    qk3 = qkt[:].rearrange("p (m d) -> p m d", d=64)
                    qfat, Rqfa = qfa[b]
                    for g in range(6):
                        pgt, Rpg = pg[pgi[0] % 3]; pgi[0] += 1

                        def mm(e, g=g, pgt=pgt, hTt=hTt):
                            r = None
                            for k in range(8):
                                r = e.matmul(pgt[:], lhsT=hTt[:, k, :], rhs=win[k][0][:, g * 512:(g + 1) * 512], start=(k == 0), stop=(k == 7))
                            return r
                        P.op("pe", mm, reads=[RhT] + Rwin, writes=[Rpg])
                        pg3 = pgt[:].rearrange("p (m d) -> p m d", d=64)
                        if g < 2:
                            m0 = g * 8
                            P.op("act", lambda e, pg3=pg3, qk3=qk3, m0=m0: e.activation(out=qk3[:, m0:m0 + 8, 16:64], in_=pg3[:, :, 16:64], func=AF.Copy),
                                 reads=[Rpg], writes=[Rqk])
                            cosb = self.cosT[:, i, :].unsqueeze(1).to_broadcast([128, 8, 8])
                            sinb = self.sinT[:, i, :].unsqueeze(1).to_broadcast([128, 8, 8])

                            def rope1(e, pg3=pg3, cosb=cosb, sinb=sinb):
                                e.tensor_tensor(out=rt[:, 0], in0=pg3[:, :, 0:8], in1=cosb, op=ALU.mult)
                                e.tensor_tensor(out=rt[:, 1], in0=pg3[:, :, 8:16], in1=sinb, op=ALU.mult)
                                e.tensor_tensor(out=rt[:, 2], in0=pg3[:, :, 8:16], in1=cosb, op=ALU.mult)
                                return e.tensor_tensor(out=rt[:, 3], in0=pg3[:, :, 0:8], in1=sinb, op=ALU.mult)
                            P.op("dve", rope1, reads=[Rpg, self.RcosT, self.RsinT], writes=[Rrt])

                            def rope2(e, qk3=qk3, m0=m0):
                                e.tensor_tensor(out=qk3[:, m0:m0 + 8, 0:8], in0=rt[:, 0], in1=rt[:, 1], op=ALU.subtract)
                                return e.tensor_tensor(out=qk3[:, m0:m0 + 8, 8:16], in0=rt[:, 2], in1=rt[:, 3], op=ALU.add)
                            P.op("dve", rope2, reads=[Rrt], writes=[Rqk])
                        elif g == 2:
                            vt, Rv = vds[b]
                            P.op("act", lambda e, vt=vt, pgt=pgt: e.activation(out=vt[:], in_=pgt[:], func=AF.Copy), reads=[Rpg], writes=[Rv])
                            P.op("sp", lambda e, vt=vt, rows=rows: e.dma_start(out=self.vd[rows, :], in_=vt[:]), reads=[Rv], writes=[self.Rvd[b]], dmakey=f"p1vd{b}")
                        elif g == 3:
                            P.op("act", lambda e, qfat=qfat, pg3=pg3: e.activation(out=qfat[:, :, 0:64], in_=pg3, func=AF.Copy), reads=[Rpg], writes=[Rqfa])
                        elif g == 4:
                            kt, Rk = kf[b]
                            P.op("act", lambda e, kt=kt, pgt=pgt: e.activation(out=kt[:], in_=pgt[:], func=AF.Copy), reads=[Rpg], writes=[Rk])
                        elif g == 5:
                            vt2, Rv2 = vfs[b]
                            P.op("act", lambda e, vt2=vt2, pgt=pgt: e.activation(out=vt2[:], in_=pgt[:], func=AF.Copy), reads=[Rpg], writes=[Rv2])
                            P.op("sp", lambda e, vt2=vt2, rows=rows: e.dma_start(out=self.vf[rows, :], in_=vt2[:]), reads=[Rv2], writes=[self.Rvf[b]], dmakey=f"p1vf{b}")
                    def mmf(e, hTt=hTt):
                        r = None
                        for k in range(8):
                            r = e.matmul(pff[:], lhsT=hTt[:, k, :], rhs=win[k][0][:, 3072:3080], start=(k == 0), stop=(k == 7))
                        return r
                    P.op("pe", mmf, reads=[RhT] + Rwin, writes=[Rpff])
                    P.op("dve", lambda e: e.tensor_tensor(out=fz[:], in0=pff[:], in1=bfb[:], op=ALU.add), reads=[Rpff, Rbfb], writes=[Rfz])
                    P.op("dve", lambda e: e.tensor_scalar(out=fnz[:], in0=fz[:], scalar1=-1.0, scalar2=None, op0=ALU.mult), reads=[Rfz], writes=[Rfnz])
                    P.op("dve", lambda e: e.tensor_tensor(out=fa[:], in0=fz[:], in1=fnz[:], op=ALU.max), reads=[Rfz, Rfnz], writes=[Rfa])
                    P.op("act", lambda e: e.activation(out=fa[:], in_=fa[:], func=AF.Exp, scale=-1.0), reads=[Rfa], writes=[Rfa])
                    P.op("act", lambda e: e.activation(out=fa[:], in_=fa[:], func=AF.Ln, bias=1.0), reads=[Rfa], writes=[Rfa])
                    P.op("dve", lambda e: e.scalar_tensor_tensor(out=fl[:], in0=fnz[:], scalar=0.0, in1=fa[:], op0=ALU.max, op1=ALU.add),
                         reads=[Rfnz, Rfa], writes=[Rfl])
                    P.op("dve", lambda e: e.tensor_scalar(out=fl[:], in0=fl[:], scalar1=-1.0, scalar2=None, op0=ALU.mult), reads=[Rfl], writes=[Rfl])
                    cft, Rcf = cf[b]
                    cfp, Rcfp = cf[1 - b]

                    def mmc(e, i=i, cfp=cfp):
                        r = e.matmul(pcs[:], lhsT=self.triU[:], rhs=fl[:], start=True, stop=(i == 0))
                        if i > 0:
                            r = e.matmul(pcs[:], lhsT=self.e127[:], rhs=cfp[:], start=False, stop=True)
                        return r
                    P.op("pe", mmc, reads=[Rfl, self.RtriU, self.Re127] + ([Rcfp] if i > 0 else []), writes=[Rpcs])

                    def cfev(e, cft=cft, i=i):
                        e.tensor_copy(out=cft[:], in_=pcs[:])
                        e.tensor_scalar(out=self.ncf[:, i, :], in0=pcs[:], scalar1=-1.0, scalar2=None, op0=ALU.mult)
                        return e.tensor_scalar(out=g8[:], in0=pcs[:], scalar1=8.0, scalar2=None, op0=ALU.mult)
                    P.op("dve", cfev, reads=[Rpcs], writes=[Rcf, self.Rncf, Rg8])
                    P.op("dve", lambda e, qfat=qfat: e.tensor_copy(out=qfat[:, :, 64], in_=g8[:]), reads=[Rg8], writes=[Rqfa])
                    P.op("dve", lambda e, qfat=qfat: e.tensor_tensor(out=r1[:], in0=g8[:], in1=qfat[:, :, 64], op=ALU.subtract), reads=[Rg8, Rqfa], writes=[Rr1])
                    P.op("dve", lambda e, qfat=qfat: e.tensor_copy(out=qfat[:, :, 65], in_=r1[:]), reads=[Rr1], writes=[Rqfa])
                    P.op("dve", lambda e, qfat=qfat: e.tensor_tensor(out=g8[:], in0=r1[:], in1=qfat[:, :, 65], op=ALU.subtract), reads=[Rr1, Rqfa], writes=[Rg8])
                    P.op("dve", lambda e, qfat=qfat: e.tensor_copy(out=qfat[:, :, 66], in_=g8[:]), reads=[Rg8], writes=[Rqfa])
                    tpt, Rtp = tp[tpi[0] % 2]; tpi[0] += 1

                    def trqk(e, qkt=qkt, tpt=tpt):
                        r = None
                        for j in range(8):
                            r = e.transpose(out=tpt[:, j, :], in_=qkt[:, j * 128:(j + 1) * 128], identity=self.ident[:])
                        return r
                    P.op("pe", trqk, reads=[Rqk, self.Rident], writes=[Rtp])
                    st, Rst = stqk[b]
                    P.op("dve", lambda e, st=st, tpt=tpt: e.tensor_copy(out=st[:], in_=tpt[:]), reads=[Rtp], writes=[Rst])
                    P.op("sp", lambda e, st=st, rows=rows: e.dma_start(out=self.qktd[:, :, rows].rearrange("j r t -> r j t"), in_=st[:]),
                         reads=[Rst], writes=[self.Rqktd[b]], dmakey=f"p1sq{b}")
                    tpt2, Rtp2 = tp[tpi[0] % 2]; tpi[0] += 1
                    kt, Rk = kf[b]

                    def trkf(e, kt=kt, tpt2=tpt2):
                        r = None
                        for j in range(4):
                            r = e.transpose(out=tpt2[:, j, :], in_=kt[:, j * 128:(j + 1) * 128], identity=self.ident[:])
                        return r
                    P.op("pe", trkf, reads=[Rk, self.Rident], writes=[Rtp2])
                    st2, Rst2 = stkf[b]
                    P.op("act", lambda e, st2=st2, tpt2=tpt2: e.activation(out=st2[:], in_=tpt2[:, 0:4, :], func=AF.Copy), reads=[Rtp2], writes=[Rst2])
                    P.op("sp", lambda e, st2=st2, rows=rows: e.dma_start(out=self.ktf[:, :, rows].rearrange("j r t -> r j t"), in_=st2[:]),
                         reads=[Rst2], writes=[self.Rktf[b]], dmakey=f"p1sk{b}")
                    tpt3, Rtp3 = tp[tpi[0] % 2]; tpi[0] += 1

                    def trqf(e, qfat=qfat, tpt3=tpt3):
                        r = None
                        for h in range(8):
                            r = e.transpose(out=tpt3[0:67, h, :], in_=qfat[:, h, :], identity=self.ident[:])
                        return r
                    P.op("pe", trqf, reads=[Rqfa, self.Rident], writes=[Rtp3])
                    st3, Rst3 = stqf[b]
                    P.op("dve", lambda e, st3=st3, tpt3=tpt3: e.tensor_copy(out=st3[0:67], in_=tpt3[0:67]), reads=[Rtp3], writes=[Rst3])
                    P.op("sp", lambda e, st3=st3, rows=rows: e.dma_start(out=self.qtf[:, :, rows].rearrange("h r t -> r h t"), in_=st3[0:67]),
                         reads=[Rst3], writes=[self.Rqtf[b]], dmakey=f"p1sf{b}")
                tile1_(i)
            P.flush(self.scr[:])

    def combine(self, i, xt, Rx, y0p, y1p, tmp, Rtmp, gt2, Rgt2, kx, ky):
        P = self.P
        rows = slice(i * 128, (i + 1) * 128)
        (y0t, Ry0), (y1t, Ry1) = y0p, y1p
        P.op("sp", lambda e: e.dma_start(out=xt[:], in_=self.xres[rows, :]), reads=self.Rxres, writes=[Rx], dmakey=kx)
        for k, (yt, Ry) in enumerate(((y0t, Ry0), (y1t, Ry1))):
            P.op("pool", lambda e, yt=yt: e.memset(yt[:], 0.0), writes=[Ry])
            P.op("pool", lambda e, yt=yt, k=k: self.indirect(
                e, out=yt[:, :], out_offset=None, in_=self.ysd[:, :],
                in_offset=bass.IndirectOffsetOnAxis(ap=self.dest[:, i, k:k + 1], axis=0)),
                reads=[self.Rysd, self.Rdest], writes=[Ry], dmakey=ky + str(k))
        P.op("dve", lambda e: e.tensor_scalar(out=tmp[:], in0=y0t[:], scalar1=self.wts[:, i, 0:1], scalar2=None, op0=ALU.mult),
             reads=[Ry0, self.Rwts], writes=[Rtmp])
        P.op("dve", lambda e: e.scalar_tensor_tensor(out=tmp[:], in0=y1t[:], scalar=self.wts[:, i, 1:2], in1=tmp[:], op0=ALU.mult, op1=ALU.add),
             reads=[Ry1, self.Rwts, Rtmp], writes=[Rtmp])
        P.op("dve", lambda e: e.tensor_tensor(out=tmp[:], in0=tmp[:], in1=gt2, op=ALU.mult), reads=[Rtmp, Rgt2], writes=[Rtmp])
        P.op("dve", lambda e: e.tensor_tensor(out=xt[:], in0=xt[:], in1=tmp[:], op=ALU.add), reads=[Rtmp, Rx], writes=[Rx])

    def phase2(self, l):
        nc, P, I = self.nc, self.P, self.ins
        lam_init = 0.8 - 0.6 * math.exp(-0.3 * l)
        with contextlib.ExitStack() as es:
            QT = [self.sb(es, f"p2_QT{j}", [67, T], BF16) for j in range(2)]
            KT = [self.sb(es, f"p2_KT{j}", [67, T], BF16) for j in range(2)]
            VV = [self.sb(es, f"p2_V{j}", [128, NT * 129], BF16) for j in range(2)]
            PT = [self.sb(es, f"p2_PT{j}", [128, 512], BF16) for j in range(4)]
            u1, Ru1 = self.sb(es, "p2_u1", [128, NT, 128], F32)
            ot, Rot = self.sb(es, "p2_ot", [128, 2, 128], F32)
            osq, Rosq = self.sb(es, "p2_osq", [128, 2, 128], F32)
            rden, Rrden = self.sb(es, "p2_rden", [128, 2], F32)
            ssq, Rssq = self.sb(es, "p2_ssq", [128, 2], F32)
            ost = [self.sb(es, f"p2_ost{j}", [128, 2, 128], BF16) for j in range(2)]
            gsub, Rgsub = self.sb(es, "p2_gsub", [128, 128], F32)
            gfox, Rgfox = self.sb(es, "p2_gfox", [128, 64], F32)
            SP_ = [self.ps(es, f"p2_S{j}", [128, 512], F32) for j in range(3)]
            OP = [self.ps(es, f"p2_O{j}", [128, 512], F32) for j in range(4)]
            for kt, Rk in KT:
                P.op("pool", lambda e, kt=kt: e.memset(kt[64:67, :], 1.0), writes=[Rk])
            P.op("sp", lambda e: e.dma_start(out=gsub[:], in_=I["g_subln"][l, :].partition_broadcast(128)), writes=[Rgsub], dmakey="p2g1")
            P.op("sp", lambda e: e.dma_start(out=gfox[:], in_=I["g_fox_out"][l, :].partition_broadcast(128)), writes=[Rgfox], dmakey="p2g2")
            P.op("dve", lambda e: e.tensor_scalar(out=gsub[:], in0=gsub[:], scalar1=float(1.0 - lam_init), scalar2=None, op0=ALU.mult),
                 reads=[Rgsub], writes=[Rgsub])
            jobs = []
            for h in range(4):
                jobs.append(("d", h, 0)); jobs.append(("d", h, 1))
            for h in range(8):
                jobs.append(("f", h, 0))
            vslot = {}
            vcount = [0]

            def load(jn):
                kind, h, c = jobs[jn]
                b = jn % 2
                qt, Rq = QT[b]; kt, Rk = KT[b]
                if kind == "d":
                    m = 2 * h + c
                    P.op("sp", lambda e: e.dma_start(out=qt[0:64, :], in_=self.qktd[m // 2, (m % 2) * 64:(m % 2) * 64 + 64, :]),
                         reads=self.Rqktd, writes=[Rq], dmakey=f"p2q{b}")
                    mk = 8 + m
                    P.op("sp", lambda e: e.dma_start(out=kt[0:64, :], in_=self.qktd[mk // 2, (mk % 2) * 64:(mk % 2) * 64 + 64, :]),
                         reads=self.Rqktd, writes=[Rk], dmakey=f"p2k{b}")
                    if c == 0:
                        vb = vcount[0] % 2; vcount[0] += 1
                        vslot[(kind, h)] = vb
                        vt, Rv = VV[vb]
                        v3 = vt[:].rearrange("p (n d) -> p n d", d=129)
                        P.op("sp", lambda e: e.dma_start(out=v3[:, :, 0:128], in_=self.vd[:, h * 128:(h + 1) * 128].rearrange("(n p) d -> p n d", p=128)),
                             reads=self.Rvd, writes=[Rv], dmakey=f"p2v{vb}")
                        P.op("pool", lambda e: e.memset(v3[:, :, 128:129], 1.0), writes=[Rv])
                else:
                    P.op("sp", lambda e: e.dma_start(out=qt[0:67, :], in_=self.qtf[h, :, :]), reads=self.Rqtf, writes=[Rq], dmakey=f"p2q{b}")
                    P.op("sp", lambda e: e.dma_start(out=kt[0:64, :], in_=self.ktf[h // 2, (h % 2) * 64:(h % 2) * 64 + 64, :]),
                         reads=self.Rktf, writes=[Rk], dmakey=f"p2k{b}")
                    vb = vcount[0] % 2; vcount[0] += 1
                    vslot[(kind, h)] = vb
                    vt, Rv = VV[vb]
                    v3 = vt[:, 0:NT * 65].rearrange("p (n d) -> p n d", d=65)
                    P.op("sp", lambda e: e.dma_start(out=v3[:, :, 0:64], in_=self.vf[:, h * 64:(h + 1) * 64].rearrange("(n p) d -> p n d", p=128)),
                         reads=self.Rvf, writes=[Rv], dmakey=f"p2v{vb}")
                    P.op("pool", lambda e: e.memset(v3[:, :, 64:65], 1.0), writes=[Rv])

            sidx = [0]; pidx = [0]; oidx = [0]; osti = [0]
            load(0)
            for jn, (kind, h, c) in enumerate(jobs):
                def job_(jn, kind, h, c):
                    if jn + 1 < len(jobs):
                        load(jn + 1)
                    b = jn % 2
                    qt, Rq = QT[b]; kt, Rk = KT[b]
                    vt, Rv = VV[vslot[(kind, h)]]
                    if kind == "d":
                        dv = 128; KK = 64; mask, Rmask = self.maskD, self.RmaskD
                        v3 = vt[:].rearrange("p (n d) -> p n d", d=129)
                    else:
                        dv = 64; KK = 67; mask, Rmask = self.maskF, self.RmaskF
                        v3 = vt[:, 0:NT * 65].rearrange("p (n d) -> p n d", d=65)
                    for qti in range(NT // 4):
                        def qtile_(qti):
                            Ob = [OP[(oidx[0] * 2) % 4], OP[(oidx[0] * 2 + 1) % 4]]; oidx[0] += 1
                            O3 = [o[0][:, 0:2 * (dv + 1)].rearrange("p (s d) -> p s d", d=dv + 1) for o in Ob]
                            nkb = 4 * qti + 4
                            steps = []
                            for kb in range(nkb):
                                j = kb - 4 * qti
                                qlo = max(j, 0) * 128
                                st, Rs = SP_[sidx[0] % 3]; sidx[0] += 1
                                pt, Rp = PT[pidx[0] % 4]; pidx[0] += 1
                                steps.append((kb, j, qlo, st, Rs, pt, Rp))

                            def rec_S(s):
                                kb, j, qlo, st, Rs, pt, Rp = s

                                def f(e):
                                    r = e.matmul(st[:, qlo:512], lhsT=kt[0:KK, kb * 128:(kb + 1) * 128],
                                                 rhs=qt[0:KK, qti * 512 + qlo:(qti + 1) * 512], start=True, stop=True, skip_group_check=True)
                                    if j >= 0:
                                        r = e.matmul(st[:, j * 128:(j + 1) * 128], lhsT=self.ident[:], rhs=mask[:], start=False, stop=True, skip_group_check=True)
                                    return r
                                P.op("pe", f, reads=[Rq, Rk, self.Rident, Rmask], writes=[Rs])

                            def rec_E(s):
                                kb, j, qlo, st, Rs, pt, Rp = s
                                bias = self.ncf[:, kb, h:h + 1] if kind == "f" else 0.0
                                P.op("act", lambda e: e.activation(out=pt[:, qlo:512], in_=st[:, qlo:512], func=AF.Exp, scale=0.125, bias=bias),
                                     reads=[Rs, self.Rncf], writes=[Rp])

                            def rec_PV(s):
                                kb, j, qlo, st, Rs, pt, Rp = s

                                def f(e):
                                    r = None
                                    for i4 in range(max(j, 0), 4):
                                        r = e.matmul(O3[i4 // 2][:, i4 % 2, :], lhsT=pt[:, i4 * 128:(i4 + 1) * 128], rhs=v3[:, kb, :],
                                                     start=(kb == 0 and i4 % 2 == 0), stop=(kb == 4 * qti + i4), skip_group_check=True)
                                    return r
                                P.op("pe", f, reads=[Rp, Rv], writes=[Ob[0][1], Ob[1][1]])
                            LA = 2
                            for s_i in range(min(LA, nkb)):
                                rec_S(steps[s_i])
                            for s_i in range(nkb):
                                rec_E(steps[s_i])
                                if s_i + LA < nkb:
                                    rec_S(steps[s_i + LA])
                                rec_PV(steps[s_i])
                            for half in range(2):
                                o3 = O3[half]; Ro = Ob[half][1]
                                rows0 = qti * 512 + half * 256
                                P.op("dve", lambda e, o3=o3: e.tensor_scalar(out=rden[:], in0=o3[:, :, dv], scalar1=1e-30, scalar2=None, op0=ALU.max),
                                     reads=[Ro], writes=[Rrden])
                                P.op("dve", lambda e: e.reciprocal(out=rden[:], in_=rden[:]), reads=[Rrden], writes=[Rrden])
                                rb = rden[:].unsqueeze(2).to_broadcast([128, 2, dv])
                                if kind == "d" and c == 0:
                                    P.op("dve", lambda e, o3=o3, rb=rb, half=half, qti=qti: e.tensor_tensor(out=u1[:, 4 * qti + 2 * half:4 * qti + 2 * half + 2, :], in0=o3[:, :, 0:dv], in1=rb, op=ALU.mult),
                                         reads=[Ro, Rrden], writes=[Ru1])
                                    continue
                                P.op("dve", lambda e, o3=o3, rb=rb: e.tensor_tensor(out=ot[:, :, 0:dv], in0=o3[:, :, 0:dv], in1=rb, op=ALU.mult),
                                     reads=[Ro, Rrden], writes=[Rot])
                                if kind == "d":
                                    P.op("dve", lambda e, half=half, qti=qti: e.scalar_tensor_tensor(out=ot[:], in0=ot[:], scalar=self.neglam[:, 0:1], in1=u1[:, 4 * qti + 2 * half:4 * qti + 2 * half + 2, :],
                                                                                           op0=ALU.mult, op1=ALU.add),
                                         reads=[Rot, Ru1, self.Rneglam], writes=[Rot])
                                P.op("dve", lambda e: e.tensor_tensor(out=osq[:, :, 0:dv], in0=ot[:, :, 0:dv], in1=ot[:, :, 0:dv], op=ALU.mult), reads=[Rot], writes=[Rosq])
                                P.op("dve", lambda e: e.tensor_reduce(out=ssq[:], in_=osq[:, :, 0:dv], axis=AX.X, op=ALU.add), reads=[Rosq], writes=[Rssq])
                                P.op("act", lambda e: e.activation(out=ssq[:], in_=ssq[:], func=AF.Ln, scale=1.0 / dv, bias=EPS), reads=[Rssq], writes=[Rssq])
                                P.op("act", lambda e: e.activation(out=ssq[:], in_=ssq[:], func=AF.Exp, scale=-0.5), reads=[Rssq], writes=[Rssq])
                                sb_ = ssq[:].unsqueeze(2).to_broadcast([128, 2, dv])
                                P.op("dve", lambda e, sb_=sb_: e.tensor_tensor(out=ot[:, :, 0:dv], in0=ot[:, :, 0:dv], in1=sb_, op=ALU.mult), reads=[Rot, Rssq], writes=[Rot])
                                gt = gsub if kind == "d" else gfox
                                Rg = Rgsub if kind == "d" else Rgfox
                                gb = gt[:].unsqueeze(1).to_broadcast([128, 2, dv])
                                ostt, Rost = ost[osti[0] % 2]; ob_ = osti[0] % 2; osti[0] += 1
                                P.op("dve", lambda e, gb=gb, ostt=ostt: e.tensor_tensor(out=ostt[:, :, 0:dv], in0=ot[:, :, 0:dv], in1=gb, op=ALU.mult),
                                     reads=[Rot, Rg], writes=[Rost])
                                col0 = h * 128 if kind == "d" else 512 + h * 64
                                dst = self.ocat[rows0:rows0 + 256, col0:col0 + dv].rearrange("(s p) d -> p s d", p=128)
                                P.op("sp", lambda e, dst=dst, ostt=ostt: e.dma_start(out=dst, in_=ostt[:, :, 0:dv]), reads=[Rost],
                                     writes=[self.Rocat[ob_]], dmakey=f"p2o{ob_}")
                        qtile_(qti)
                job_(jn, kind, h, c)
            P.flush(self.scr[:])

    def phase3a(self, l):
        nc, P, I = self.nc, self.P, self.ins
        xsrc = I["x"] if l == 0 else self.xres
        BIG = 1.0e4
        with contextlib.ExitStack() as es:
            wo = [self.sb(es, f"p3_wo{k}", [128, D], BF16) for k in range(8)]
            Rwo = [w[1] for w in wo]
            P.op("pool", lambda e: [e.dma_start(out=wo[k][0][:], in_=I["w_out"][l, k * 128:(k + 1) * 128, :]) for k in range(8)],
                 writes=Rwo, dmakey="p3wo", ndma=8)
            wr, Rwr = self.sb(es, "p3_wr", [128, 8, 36], F32)
            P.op("sp", lambda e: e.dma_start(out=wr[:, :, 0:4], in_=I["w_router_group"][l].rearrange("(k p) n -> p k n", p=128)), writes=[Rwr], dmakey="p3wr1")
            P.op("sp", lambda e: e.dma_start(out=wr[:, :, 4:36], in_=I["w_router_expert"][l].rearrange("(k p) n -> p k n", p=128)), writes=[Rwr], dmakey="p3wr2")
            rb, Rrb = self.sb(es, "p3_rb", [128, 36], F32)
            P.op("sp", lambda e: e.dma_start(out=rb[:, 0:4], in_=I["b_router_group"][l, :].partition_broadcast(128)), writes=[Rrb], dmakey="p3rb1")
            P.op("sp", lambda e: e.dma_start(out=rb[:, 4:36], in_=I["b_router_expert"][l, :].partition_broadcast(128)), writes=[Rrb], dmakey="p3rb2")
            xs = [self.sb(es, f"p3_xs{j}", [128, D], F32) for j in range(2)]
            oc = [self.sb(es, f"p3_oc{j}", [128, D], BF16) for j in range(2)]
            oT = [self.sb(es, f"p3_oT{j}", [128, 8, 128], BF16) for j in range(2)]
            tmp, Rtmp = self.sb(es, "p3_tmp", [128, D], F32)
            junk, Rjunk = self.sb(es, "p3_junk", [128, D], BF16)
            h2f = [self.sb(es, f"p3_h2f{j}", [128, D], F32) for j in range(2)]
            h2b = [self.sb(es, f"p3_h2b{j}", [128, D], BF16) for j in range(2)]
            h2T, Rh2T = self.sb(es, "p3_h2T", [128, 8, 128], F32)
            ss = [self.sb(es, f"p3_ss{j}", [128, 1], F32) for j in range(2)]
            lg, Rlg = self.sb(es, "p3_lg", [128, 36], F32)
            sm, Rsm = self.sb(es, "p3_sm", [128, 16], F32)
            ohg, Rohg = self.sb(es, "p3_ohg", [128, 4], F32)
            eg, Reg = self.sb(es, "p3_eg", [128, 4], F32)
            msk, Rmsk = self.sb(es, "p3_msk", [128, 32], F32)
            oh, Roh = self.sb(es, "p3_oh", [128, 2, 32], F32)
            ohs, Rohs = self.sb(es, "p3_ohs", [128, 32], F32)
            cum, Rcum = self.sb(es, "p3_cum", [128, 32], F32)
            pos, Rpos = self.sb(es, "p3_pos", [128, 32], F32)
            ovf, Rovf = self.sb(es, "p3_ovf", [128, 32], F32)
            dtmp, Rdtmp = self.sb(es, "p3_dtmp", [128, 2, 32], F32)
            dstf, Rdstf = self.sb(es, "p3_dstf", [128, 2], F32)
            tp, Rtp = self.ps(es, "p3_tp", [128, 8, 128], BF16)
            mx = [self.ps(es, f"p3_mx{j}", [128, 512], F32) for j in range(2)]
            tf = [self.ps(es, f"p3_tf{j}", [128, 4, 128], F32) for j in range(2)]
            plg, Rplg = self.ps(es, "p3_plg", [128, 36], F32)
            prk, Rprk = self.ps(es, "p3_prk", [128, 64], F32)
            P.op("pool", lambda e: e.memset(cum[:], 0.0), writes=[Rcum])

            def loadA(i):
                b = i % 2
                rows = slice(i * 128, (i + 1) * 128)
                P.op("sp", lambda e: e.dma_start(out=xs[b][0][:], in_=xsrc[rows, :]), reads=self.Rxres, writes=[xs[b][1]], dmakey=f"p3x{b}")
                P.op("sp", lambda e: e.dma_start(out=oc[b][0][:], in_=self.ocat[rows, :]), reads=self.Rocat, writes=[oc[b][1]], dmakey=f"p3o{b}")
            loadA(0)
            for i in range(NT):
                def tile3a_(i):
                    b = i % 2
                    rows = slice(i * 128, (i + 1) * 128)
                    if i + 1 < NT:
                        loadA(i + 1)
                    xt, Rx = xs[b]; oct_, Roc = oc[b]; oTt, RoT = oT[b]

                    def tro(e, oct_=oct_):
                        r = None
                        for k in range(8):
                            r = e.transpose(out=tp[:, k, :], in_=oct_[:, k * 128:(k + 1) * 128], identity=self.ident[:])
                        return r
                    P.op("pe", tro, reads=[Roc, self.Rident], writes=[Rtp])
                    P.op("act", lambda e, oTt=oTt: e.activation(out=oTt[:], in_=tp[:], func=AF.Copy), reads=[Rtp], writes=[RoT])
                    for n in range(2):
                        def mm(e, n=n, oTt=oTt):
                            r = None
                            for k in range(8):
                                r = e.matmul(mx[n][0][:], lhsT=oTt[:, k, :], rhs=wo[k][0][:, n * 512:(n + 1) * 512], start=(k == 0), stop=(k == 7))
                            return r
                        P.op("pe", mm, reads=[RoT] + Rwo, writes=[mx[n][1]])
                        P.op("dve", lambda e, n=n: e.tensor_tensor(out=tmp[:, n * 512:(n + 1) * 512], in0=mx[n][0][:], in1=self.modt[:, 2, n * 512:(n + 1) * 512], op=ALU.mult),
                             reads=[mx[n][1], self.Rmodt], writes=[Rtmp])
                    P.op("dve", lambda e, xt=xt: e.tensor_tensor(out=xt[:], in0=xt[:], in1=tmp[:], op=ALU.add), reads=[Rx, Rtmp], writes=[Rx])
                    P.op("sp", lambda e, xt=xt, rows=rows: e.dma_start(out=self.xres[rows, :], in_=xt[:]), reads=[Rx], writes=[self.Rxres[b]], dmakey=f"p3xo{b}")
                    sst, Rss = ss[b]
                    self.rstd_ops(xt[:], Rx, junk, Rjunk, sst, Rss, D)
                    P.op("dve", lambda e, xt=xt, sst=sst: e.scalar_tensor_tensor(out=tmp[:], in0=xt[:], scalar=sst[:, 0:1], in1=self.modt[:, 4, :], op0=ALU.mult, op1=ALU.mult),
                         reads=[Rx, Rss, self.Rmodt], writes=[Rtmp])
                    hf, Rhf = h2f[b]; hbt, Rhb = h2b[b]
                    P.op("dve", lambda e, hf=hf: e.tensor_tensor(out=hf[:], in0=tmp[:], in1=self.modt[:, 3, :], op=ALU.add), reads=[Rtmp, self.Rmodt], writes=[Rhf])
                    P.op("act", lambda e, hf=hf, hbt=hbt: e.activation(out=hbt[:], in_=hf[:], func=AF.Copy), reads=[Rhf], writes=[Rhb])
                    for half in range(2):
                        def trf(e, half=half, hf=hf):
                            r = None
                            for k in range(4):
                                kk = half * 4 + k
                                r = e.transpose(out=tf[half][0][:, k, :], in_=hf[:, kk * 128:(kk + 1) * 128], identity=self.identf[:])
                            return r
                        P.op("pe", trf, reads=[Rhf, self.Ridentf], writes=[tf[half][1]])
                        P.op("dve", lambda e, half=half: e.tensor_copy(out=h2T[:, half * 4:half * 4 + 4, :], in_=tf[half][0][:]), reads=[tf[half][1]], writes=[Rh2T])

                    def mml(e):
                        r = None
                        for k in range(8):
                            r = e.matmul(plg[:], lhsT=h2T[:, k, :], rhs=wr[:, k, :], start=(k == 0), stop=(k == 7))
                        return r
                    P.op("pe", mml, reads=[Rh2T, Rwr], writes=[Rplg])
                    P.op("dve", lambda e: e.tensor_tensor(out=lg[:], in0=plg[:], in1=rb[:], op=ALU.add), reads=[Rplg, Rrb], writes=[Rlg])
                    P.op("dve", lambda e: e.tensor_reduce(out=sm[:, 0:1], in_=lg[:, 0:4], axis=AX.X, op=ALU.max), reads=[Rlg], writes=[Rsm])
                    P.op("dve", lambda e: e.tensor_scalar(out=ohg[:], in0=lg[:, 0:4], scalar1=sm[:, 0:1], scalar2=None, op0=ALU.is_equal), reads=[Rlg, Rsm], writes=[Rohg])
                    P.op("dve", lambda e: e.tensor_scalar(out=sm[:, 1:2], in0=sm[:, 0:1], scalar1=-1.0, scalar2=None, op0=ALU.mult), reads=[Rsm], writes=[Rsm])
                    P.op("act", lambda e: e.activation(out=eg[:], in_=lg[:, 0:4], func=AF.Exp, bias=sm[:, 1:2], accum_out=sm[:, 2:3]), reads=[Rlg, Rsm], writes=[Reg, Rsm])
                    P.op("dve", lambda e: e.reciprocal(out=sm[:, 3:4], in_=sm[:, 2:3]), reads=[Rsm], writes=[Rsm])
                    P.op("dve", lambda e: e.tensor_scalar(out=ohg[:], in0=ohg[:], scalar1=-1.0, scalar2=BIG, op0=ALU.add, op1=ALU.mult), reads=[Rohg], writes=[Rohg])
                    P.op("dve", lambda e: e.tensor_tensor(out=msk[:].rearrange("p (g j) -> p g j", j=8), in0=lg[:, 4:36].rearrange("p (g j) -> p g j", j=8),
                                                          in1=ohg[:].unsqueeze(2).to_broadcast([128, 4, 8]), op=ALU.add), reads=[Rlg, Rohg], writes=[Rmsk])
                    P.op("dve", lambda e: e.tensor_reduce(out=sm[:, 4:5], in_=msk[:], axis=AX.X, op=ALU.max), reads=[Rmsk], writes=[Rsm])
                    P.op("dve", lambda e: e.tensor_scalar(out=oh[:, 0, :], in0=msk[:], scalar1=sm[:, 4:5], scalar2=None, op0=ALU.is_equal), reads=[Rmsk, Rsm], writes=[Roh])
                    P.op("dve", lambda e: e.scalar_tensor_tensor(out=msk[:], in0=oh[:, 0, :], scalar=-BIG, in1=msk[:], op0=ALU.mult, op1=ALU.add), reads=[Roh, Rmsk], writes=[Rmsk])
                    P.op("dve", lambda e: e.tensor_reduce(out=sm[:, 5:6], in_=msk[:], axis=AX.X, op=ALU.max), reads=[Rmsk], writes=[Rsm])
                    P.op("dve", lambda e: e.tensor_scalar(out=oh[:, 1, :], in0=msk[:], scalar1=sm[:, 5:6], scalar2=None, op0=ALU.is_equal), reads=[Rmsk, Rsm], writes=[Roh])
                    P.op("dve", lambda e: e.tensor_tensor(out=sm[:, 6:7], in0=sm[:, 5:6], in1=sm[:, 4:5], op=ALU.subtract), reads=[Rsm], writes=[Rsm])
                    P.op("act", lambda e: e.activation(out=sm[:, 7:8], in_=sm[:, 6:7], func=AF.Exp), reads=[Rsm], writes=[Rsm])
                    P.op("dve", lambda e: e.tensor_scalar(out=sm[:, 8:9], in0=sm[:, 7:8], scalar1=1.0, scalar2=None, op0=ALU.add), reads=[Rsm], writes=[Rsm])
                    P.op("dve", lambda e: e.reciprocal(out=sm[:, 9:10], in_=sm[:, 8:9]), reads=[Rsm], writes=[Rsm])
                    P.op("dve", lambda e: e.tensor_tensor(out=sm[:, 10:11], in0=sm[:, 9:10], in1=sm[:, 7:8], op=ALU.mult), reads=[Rsm], writes=[Rsm])
                    P.op("dve", lambda e, i=i: e.tensor_tensor(out=self.wts[:, i, 0:1], in0=sm[:, 9:10], in1=sm[:, 3:4], op=ALU.mult), reads=[Rsm], writes=[self.Rwts])
                    P.op("dve", lambda e, i=i: e.tensor_tensor(out=self.wts[:, i, 1:2], in0=sm[:, 10:11], in1=sm[:, 3:4], op=ALU.mult), reads=[Rsm], writes=[self.Rwts])
                    P.op("dve", lambda e: e.tensor_tensor(out=ohs[:], in0=oh[:, 0, :], in1=oh[:, 1, :], op=ALU.add), reads=[Roh], writes=[Rohs])

                    def mmr(e):
                        e.matmul(prk[:, 0:32], lhsT=self.triS[:], rhs=ohs[:], start=True, stop=True, skip_group_check=True)
                        return e.matmul(prk[:, 32:64], lhsT=self.ones[:], rhs=ohs[:], start=True, stop=True, skip_group_check=True)
                    P.op("pe", mmr, reads=[Rohs, self.RtriS, self.Rones], writes=[Rprk])
                    P.op("dve", lambda e: e.tensor_tensor(out=pos[:], in0=prk[:, 0:32], in1=cum[:], op=ALU.add), reads=[Rprk, Rcum], writes=[Rpos])
                    P.op("dve", lambda e: e.tensor_tensor(out=cum[:], in0=prk[:, 32:64], in1=cum[:], op=ALU.add), reads=[Rprk, Rcum], writes=[Rcum])
                    P.op("dve", lambda e: e.tensor_scalar(out=ovf[:], in0=pos[:], scalar1=float(CAP), scalar2=1.0e6, op0=ALU.is_ge, op1=ALU.mult), reads=[Rpos], writes=[Rovf])
                    P.op("dve", lambda e: e.tensor_tensor(out=pos[:], in0=pos[:], in1=self.sbase[:], op=ALU.add), reads=[Rpos, self.Rsbase], writes=[Rpos])
                    P.op("dve", lambda e: e.tensor_tensor(out=pos[:], in0=pos[:], in1=ovf[:], op=ALU.add), reads=[Rpos, Rovf], writes=[Rpos])
                    P.op("dve", lambda e: e.tensor_tensor(out=dtmp[:], in0=oh[:], in1=pos[:].unsqueeze(1).to_broadcast([128, 2, 32]), op=ALU.mult), reads=[Roh, Rpos], writes=[Rdtmp])
                    P.op("dve", lambda e: e.tensor_reduce(out=dstf[:], in_=dtmp[:], axis=AX.X, op=ALU.add), reads=[Rdtmp], writes=[Rdstf])
                    P.op("dve", lambda e, i=i: e.tensor_copy(out=self.dest[:, i, :], in_=dstf[:]), reads=[Rdstf], writes=[self.Rdest])
                    for k in range(2):
                        P.op("pool", lambda e, k=k, i=i, hbt=hbt: self.indirect(
                            e, out=self.xsd[:, :], out_offset=bass.IndirectOffsetOnAxis(ap=self.dest[:, i, k:k + 1], axis=0),
                            in_=hbt[:, :], in_offset=None),
                            reads=[Rhb, self.Rdest], writes=[self.Rxsd], dmakey=f"p3sc{k}")
                tile3a_(i)
            NHh = CAP // SEG
            flf, Rflf = self.sb(es, "p3_flf", [128, NE, NHh], F32)
            for hh in range(NHh):
                thr = float(SEG * hh) if COND_THR is None else float(COND_THR * hh)
                P.op("dve", lambda e, hh=hh, thr=thr: e.tensor_scalar(out=flf[:, :, hh], in0=cum[:], scalar1=thr, scalar2=None, op0=ALU.is_gt),
                     reads=[Rcum], writes=[Rflf])
            P.op("dve", lambda e: e.tensor_copy(out=self.flags[:], in_=flf[:].rearrange("p e h -> p (e h)")), reads=[Rflf], writes=[self.Rflags])
            P.flush(self.scr[:])

    def phase3b(self, l):
        nc, P, I = self.nc, self.P, self.ins
        NB = CAP // 128
        NH = CAP // SEG
        BPS = SEG // 128
        with contextlib.ExitStack() as es:
            wg = [[self.sb(es, f"p4_wg{j}_{k}", [128, 512], BF16) for k in range(8)] for j in range(2)]
            wu = [[self.sb(es, f"p4_wu{j}_{k}", [128, 512], BF16) for k in range(8)] for j in range(2)]
            wd = [[self.sb(es, f"p4_wd{j}_{k}", [128, D], BF16) for k in range(4)] for j in range(2)]
            Xt = [self.sb(es, f"p4_X{j}", [128, NB, D], BF16) for j in range(2)]
            XT, RXT = self.sb(es, "p4_XT", [128, 8, CAP], BF16)
            AT, RAT = self.sb(es, "p4_AT", [128, 4, CAP], BF16)
            sg = [self.sb(es, f"p4_sg{j}", [128, 512], F32) for j in range(2)]
            Ys = [self.sb(es, f"p4_Y{j}", [128, D], BF16) for j in range(2)]
            tp = [self.ps(es, f"p4_tp{j}", [128, 8, 128], BF16) for j in range(2)]
            pgt = [self.ps(es, f"p4_pg{j}", [128, 512], F32) for j in range(2)]
            put = [self.ps(es, f"p4_pu{j}", [128, 512], F32) for j in range(2)]
            py = [self.ps(es, f"p4_py{j}", [128, 512], F32) for j in range(2)]
            cnt = dict(tp=0, gu=0, y=0, ys=0)
            RXseg = [[P.res(f"p4_Xseg{j}_{q}") for q in range(CAP // SEG)] for j in range(2)]

            def load(e_):
                j = e_ % 2
                P.op("pool", lambda e: [e.dma_start(out=wg[j][k][0][:], in_=I["w_expert_gate"][l, e_, k * 128:(k + 1) * 128, :]) for k in range(8)],
                     writes=[w[1] for w in wg[j]], dmakey=f"p4g{j}", ndma=8)
                P.op("pool", lambda e: [e.dma_start(out=wu[j][k][0][:], in_=I["w_expert_up"][l, e_, k * 128:(k + 1) * 128, :]) for k in range(8)],
                     writes=[w[1] for w in wu[j]], dmakey=f"p4u{j}", ndma=8)
                P.op("pool", lambda e: [e.dma_start(out=wd[j][k][0][:], in_=I["w_expert_down"][l, e_, k * 128:(k + 1) * 128, :]) for k in range(4)],
                     writes=[w[1] for w in wd[j]], dmakey=f"p4d{j}", ndma=4)
                for sg_ in range(CAP // SEG):
                    if sg_ > 0 and USE_COND:
                        P.cond_begin(self.flags[0:1, e_ * (CAP // SEG) + sg_:e_ * (CAP // SEG) + sg_ + 1])
                    r0_ = e_ * CAP + sg_ * SEG
                    b0_ = sg_ * (SEG // 128)
                    P.op("sp", lambda e, r0_=r0_, b0_=b0_: e.dma_start(out=Xt[j][0][:, b0_:b0_ + SEG // 128, :],
                                                                     in_=self.xsd[r0_:r0_ + SEG, :].rearrange("(b p) d -> p b d", p=128)),
                         reads=[self.Rxsd], writes=[RXseg[j][sg_]], dmakey=f"p4x{j}_{sg_}")
                    if sg_ > 0 and USE_COND:
                        P.cond_end()
            load(0)
            for e_ in range(NE):
                def expert_(e_):
                    j = e_ % 2
                    if e_ + 1 < NE:
                        load(e_ + 1)
                    xt, Rx = Xt[j]
                    Rwg = [w[1] for w in wg[j]]; Rwu = [w[1] for w in wu[j]]; Rwd = [w[1] for w in wd[j]]
                    for half_ in range(NH):
                        if half_ > 0 and USE_COND:
                            P.cond_begin(self.flags[0:1, e_ * NH + half_:e_ * NH + half_ + 1])
                        part_(e_, j, xt, RXseg[j][half_], Rwg, Rwu, Rwd, half_)
                        if half_ > 0 and USE_COND:
                            P.cond_end()

                def part_(e_, j, xt, Rx, Rwg, Rwu, Rwd, half_):
                    blks = range(half_ * BPS, half_ * BPS + BPS)
                    for blk in blks:
                        tpt, Rtp = tp[cnt["tp"] % 2]; cnt["tp"] += 1

                        def trx(e, blk=blk, tpt=tpt):
                            r = None
                            for k in range(8):
                                r = e.transpose(out=tpt[:, k, :], in_=xt[:, blk, k * 128:(k + 1) * 128], identity=self.ident[:])
                            return r
                        P.op("pe", trx, reads=[Rx, self.Rident], writes=[Rtp])
                        eng = "dve" if blk % 2 == 0 else "act"
                        if eng == "dve":
                            P.op("dve", lambda e, blk=blk, tpt=tpt: e.tensor_copy(out=XT[:, :, blk * 128:(blk + 1) * 128], in_=tpt[:]), reads=[Rtp], writes=[RXT])
                        else:
                            P.op("act", lambda e, blk=blk, tpt=tpt: e.activation(out=XT[:, :, blk * 128:(blk + 1) * 128], in_=tpt[:], func=AF.Copy), reads=[Rtp], writes=[RXT])
                    for half in (half_,):
                        cs = slice(half * SEG, (half + 1) * SEG)
                        for m in range(4):
                            (g_, Rg), (u_, Ru) = pgt[cnt["gu"] % 2], put[cnt["gu"] % 2]
                            sgt, Rsg = sg[cnt["gu"] % 2]; cnt["gu"] += 1

                            def mmg(e, m=m, cs=cs, g_=g_):
                                r = None
                                for k in range(8):
                                    r = e.matmul(g_[:, 0:SEG], lhsT=wg[j][k][0][:, m * 128:(m + 1) * 128], rhs=XT[:, k, cs], start=(k == 0), stop=(k == 7))
                                return r
                            P.op("pe", mmg, reads=[RXT] + Rwg, writes=[Rg])

                            def mmu(e, m=m, cs=cs, u_=u_):
                                r = None
                                for k in range(8):
                                    r = e.matmul(u_[:, 0:SEG], lhsT=wu[j][k][0][:, m * 128:(m + 1) * 128], rhs=XT[:, k, cs], start=(k == 0), stop=(k == 7))
                                return r
                            P.op("pe", mmu, reads=[RXT] + Rwu, writes=[Ru])
                            P.op("act", lambda e, g_=g_, sgt=sgt: e.activation(out=sgt[:, 0:SEG], in_=g_[:, 0:SEG], func=AF.Silu), reads=[Rg], writes=[Rsg])
                            P.op("dve", lambda e, m=m, cs=cs, u_=u_, sgt=sgt: e.tensor_tensor(out=AT[:, m, cs], in0=sgt[:, 0:SEG], in1=u_[:, 0:SEG], op=ALU.mult),
                                 reads=[Rsg, Ru], writes=[RAT])
                    for blk in blks:
                        yst, Rys = Ys[cnt["ys"] % 2]; yb = cnt["ys"] % 2; cnt["ys"] += 1
                        for n in range(2):
                            y_, Ry = py[cnt["y"] % 2]; cnt["y"] += 1

                            def mmd(e, blk=blk, n=n, y_=y_):
                                r = None
                                for k in range(4):
                                    r = e.matmul(y_[:], lhsT=AT[:, k, blk * 128:(blk + 1) * 128], rhs=wd[j][k][0][:, n * 512:(n + 1) * 512], start=(k == 0), stop=(k == 3))
                                return r
                            P.op("pe", mmd, reads=[RAT] + Rwd, writes=[Ry])
                            if n == 0:
                                P.op("dve", lambda e, n=n, y_=y_, yst=yst: e.tensor_copy(out=yst[:, n * 512:(n + 1) * 512], in_=y_[:]), reads=[Ry], writes=[Rys])
                            else:
                                P.op("act", lambda e, n=n, y_=y_, yst=yst: e.activation(out=yst[:, n * 512:(n + 1) * 512], in_=y_[:], func=AF.Copy), reads=[Ry], writes=[Rys])
                        r0 = e_ * CAP + blk * 128
                        P.op("sp", lambda e, r0=r0, yst=yst: e.dma_start(out=self.ysd[r0:r0 + 128, :], in_=yst[:]), reads=[Rys], writes=[self.Rysd], dmakey=f"p4y{yb}")
                expert_(e_)
            P.flush(self.scr[:])

    def phase_final(self):
        nc, P, I = self.nc, self.P, self.ins
        with contextlib.ExitStack() as es:
            xs = [self.sb(es, f"pf_xs{j}", [128, D], F32) for j in range(2)]
            y0 = [self.sb(es, f"pf_y0{j}", [128, D], BF16) for j in range(2)]
            y1 = [self.sb(es, f"pf_y1{j}", [128, D], BF16) for j in range(2)]
            tmp, Rtmp = self.sb(es, "pf_tmp", [128, D], F32)
            junk, Rjunk = self.sb(es, "pf_junk", [128, D], BF16)
            ss = [self.sb(es, f"pf_ss{j}", [128, 1], F32) for j in range(2)]
            gf, Rgf = self.sb(es, "pf_gf", [128, D], F32)
            P.op("sp", lambda e: e.dma_start(out=gf[:], in_=I["g_final"].partition_broadcast(128)), writes=[Rgf], dmakey="pfg")
            for i in range(NT):
                def tilef_(i):
                    b = i % 2
                    rows = slice(i * 128, (i + 1) * 128)
                    xt, Rx = xs[b]
                    self.combine(i, xt, Rx, y0[b], y1[b], tmp, Rtmp, self.modt[:, 5, :], self.Rmodt, f"pfx{b}", f"pfy{b}")
                    sst, Rss = ss[b]
                    self.rstd_ops(xt[:], Rx, junk, Rjunk, sst, Rss, D)
                    P.op("dve", lambda e, xt=xt, sst=sst: e.scalar_tensor_tensor(out=xt[:], in0=xt[:], scalar=sst[:, 0:1], in1=gf[:], op0=ALU.mult, op1=ALU.mult),
                         reads=[Rx, Rss, Rgf], writes=[Rx])
                    P.op("sp", lambda e, xt=xt, rows=rows: e.dma_start(out=self.y[rows, :], in_=xt[:]), reads=[Rx], writes=[self.Ry], dmakey=f"pfo{b}")
                tilef_(i)
            P.flush(self.scr[:])


def _layout_inputs(inputs, b):
    m = {}
    m["x"] = np.ascontiguousarray(inputs["x"][b])
    m["cT"] = np.ascontiguousarray(inputs["c"][b].reshape(8, 128).T)
    m["pos"] = np.ascontiguousarray(inputs["positions"][b].reshape(NT, 128).T).astype(np.int32)
    for k in ("w_ada", "b_ada", "g_mix", "w_in", "b_forget", "lambda_q1", "lambda_k1", "lambda_q2", "lambda_k2",
              "g_subln", "g_fox_out", "w_out", "g_ffn", "w_router_group", "b_router_group", "w_router_expert",
              "b_router_expert", "w_expert_gate", "w_expert_up", "w_expert_down", "g_final"):
        m[k] = np.ascontiguousarray(inputs[k])
    return m


def kernel(**inputs):
    kb = K()
    in_maps = [_layout_inputs(inputs, b) for b in range(4)]
    res = run_bass_kernel_spmd(kb.nc, in_maps, core_ids=list(range(4)))
    return np.stack([np.asarray(r["y"]) for r in res.results], axis=0).astype(np.float32)
```

```python
import math, contextlib
import numpy as np
import concourse.bass as bass
import concourse.mybir as mybir
from concourse.bass_utils import run_bass_kernel_spmd

F32 = mybir.dt.float32
BF16 = mybir.dt.bfloat16
I32 = mybir.dt.int32
AF = mybir.ActivationFunctionType
ALU = mybir.AluOpType
AX = mybir.AxisListType

MAXOPS = None
NLIN = 4
USE_COND = True
COND_THR = None
ENGS = ("pe", "act", "dve", "pool", "sp")


class Res:
    __slots__ = ("name", "w", "r", "excl")

    def __init__(self, name, w=None, excl=False):
        self.name = name
        self.w = w
        self.r = []
        self.excl = excl


class Prog:
    def __init__(self, nc):
        self.nc = nc
        self.ops = []
        self.es = contextlib.ExitStack()
        self.engsem = {e: self.es.enter_context(nc.semaphore("s_" + e)) for e in ENGS}
        self.keysem = {}
        self.semByName = {}
        self.cnt = {}
        self.known = {e: {} for e in ENGS}
        self.lastkey = {}
        self.allres = []
        self.barrier_op = None
        self.nflush = 0
        self.ninstr = 0
        self.cur_cond = None
        self.ncond = 0

    def res(self, name, excl=False):
        r = Res(name, self.barrier_op, excl)
        self.allres.append(r)
        return r

    def op(self, eng, fn, reads=(), writes=(), dmakey=None, ndma=1):
        self.total_ops = getattr(self, "total_ops", 0) + 1
        if MAXOPS is not None and self.total_ops > MAXOPS:
            return None
        o = dict(eng=eng, fn=fn, reads=list(reads), writes=list(writes), dmakey=dmakey, ndma=ndma,
                 deps=[], sig=False, done=False, cond=self.cur_cond)
        self.ops.append(o)
        return o

    def cond_begin(self, cnt_ap, key, thr):
        self.ncond += 1
        self.cur_cond = (self.ncond, cnt_ap, key, thr)

    def cond_end(self):
        self.cur_cond = None

    def chain(self, eng, fns, reads=(), writes=()):
        for f in fns:
            self.op(eng, f, reads=reads, writes=writes)

    def flush(self, scratch):
        nc = self.nc
        global MAXOPS
        _mo = MAXOPS
        MAXOPS = None
        bar = self.op("pool", lambda e: e.memset(scratch, 0.0), writes=[self.scratch_res])
        MAXOPS = _mo
        bar["isbar"] = True
        ops = self.ops
        for o in ops:
            deps = {}
            if o.get("isbar"):
                for r in self.allres:
                    if r.w is not None:
                        deps[id(r.w)] = r.w
                    for q in r.r:
                        deps[id(q)] = q
            for r in o["reads"]:
                if r.w is not None:
                    deps[id(r.w)] = r.w
                if r.excl:
                    for q in r.r:
                        if q["eng"] != o["eng"]:
                            deps[id(q)] = q
            for w in o["writes"]:
                if w.w is not None:
                    deps[id(w.w)] = w.w
                for q in w.r:
                    deps[id(q)] = q
            k = o["dmakey"]
            if k is not None:
                if k in self.lastkey:
                    p = self.lastkey[k]
                    deps[id(p)] = p
                self.lastkey[k] = o
            deps.pop(id(o), None)
            o["deps"] = list(deps.values())
            for r in o["reads"]:
                r.r.append(o)
            for w in o["writes"]:
                w.w = o
                w.r = []
        for o in ops:
            for p in o["deps"]:
                if p["eng"] == "pe" and o["eng"] == "pe" and p["dmakey"] is None:
                    continue
                p["sig"] = True
        bar["sig"] = True
        for o in ops:
            k = o["dmakey"]
            if k is not None:
                if k not in self.keysem:
                    pref = "q" if o["eng"] == "pool" else "d"
                    n_ = sum(1 for v in self.keysem.values() if v[0] == pref)
                    name = f"{pref}_{n_}"
                    if name not in self.semByName:
                        self.semByName[name] = self.es.enter_context(nc.semaphore(name))
                    self.keysem[k] = (pref, name)
                else:
                    assert (self.keysem[k][0] == "q") == (o["eng"] == "pool"), k
                o["semname"] = self.keysem[k][1]
                o["sem"] = self.semByName[o["semname"]]
                self.cnt[o["semname"]] = self.cnt.get(o["semname"], 0) + 16 * o["ndma"]
                o["val"] = self.cnt[o["semname"]]
            else:
                o["sem"] = self.engsem[o["eng"]]
                o["semname"] = "s_" + o["eng"]
                if o["sig"]:
                    self.cnt[o["semname"]] = self.cnt.get(o["semname"], 0) + 1
                    o["val"] = self.cnt[o["semname"]]
        per = {e: [] for e in ENGS}
        for o in ops:
            per[o["eng"]].append(o)

        def emit_op(engname, eng, o, known):
            need = {}
            for p in o["deps"]:
                if p["eng"] == "pe" and engname == "pe" and p["dmakey"] is None:
                    continue
                sn = p["semname"]
                if p["val"] > need.get(sn, (None, 0))[1]:
                    need[sn] = (p["sem"], p["val"])
            for sn, (sem, val) in need.items():
                if known.get(sn, 0) >= val:
                    continue
                eng.wait_ge(sem, val)
                known[sn] = val
                self.ninstr += 1
            res = o["fn"](eng)
            if o["dmakey"] is not None:
                lst = res if isinstance(res, (list, tuple)) else [res]
                assert len(lst) == o["ndma"], (len(lst), o["ndma"], o["dmakey"])
                for ins in lst:
                    ins.then_inc(o["sem"], 16)
            elif o["sig"]:
                ins = res[-1] if isinstance(res, (list, tuple)) else res
                ins.then_inc(o["sem"], 1)

        regcache = {e_: {} for e_ in ENGS}

        def run(engname, eng):
            known = self.known[engname]
            lst = per[engname]
            i = 0
            while i < len(lst):
                o = lst[i]
                if o["cond"] is None:
                    emit_op(engname, eng, o, known)
                    i += 1
                    continue
                key = o["cond"][2]
                regions = []
                j = i
                while j < len(lst) and lst[j]["cond"] is not None and lst[j]["cond"][2] == key:
                    cid = lst[j]["cond"]
                    k = j
                    while k < len(lst) and lst[k]["cond"] is cid:
                        k += 1
                    regions.append((cid, lst[j:k]))
                    j = k
                cache = regcache[engname]
                if key not in cache:
                    if len(cache) >= 2:
                        oldk = next(iter(cache))
                        eng.free_register(cache.pop(oldk))
                    self._nreg = getattr(self, "_nreg", 0) + 1
                    reg = eng.alloc_register(f"cf{self._nreg}")
                    eng.reg_load(reg, o["cond"][1])
                    cache[key] = reg
                reg = cache[key]
                saved = dict(known)

                def incs_of(regs):
                    incs = {}
                    for (_, ops_) in regs:
                        for o2 in ops_:
                            n_ = 16 * o2["ndma"] if o2["dmakey"] is not None else (1 if o2["sig"] else 0)
                            if n_:
                                sn = o2["semname"]
                                if sn not in incs:
                                    incs[sn] = [o2["sem"], o2["val"] - n_, 0]
                                incs[sn][2] += n_
                    return incs

                def emit_chain(idx):
                    if idx == len(regions):
                        return
                    cid_, ops_ = regions[idx]
                    with eng.If_lt(reg, cid_[3] + 1):
                        for sn, (sem, base, tot) in incs_of(regions[idx:]).items():
                            if base > 0:
                                eng.wait_ge(sem, base)
                            eng.sem_inc(sem, tot)
                    with eng.Else():
                        for o2 in ops_:
                            emit_op(engname, eng, o2, known)
                        emit_chain(idx + 1)
                emit_chain(0)
                known.clear()
                known.update(saved)
                i = j
            for reg_ in regcache[engname].values():
                eng.free_register(reg_)
            regcache[engname].clear()

        with nc.Block() as block:
            @block.tensor
            def _(e):
                run("pe", e)

            @block.scalar
            def _(e):
                run("act", e)

            @block.vector
            def _(e):
                run("dve", e)

            @block.gpsimd
            def _(e):
                run("pool", e)

            @block.sync
            def _(e):
                run("sp", e)
        for r in self.allres:
            r.w = bar
            r.r = []
        pass
        self.barrier_op = bar
        self.lastkey = {}
        self.keysem = {}
        self.ops = []
        self.nflush += 1

    def finish(self, scratch):
        self.flush(scratch)
        nc = self.nc
        fin = [(sem, self.cnt[name]) for name, sem in self.semByName.items()]
        bar = self.barrier_op
        with nc.Block() as block:
            @block.sync
            def _(e):
                e.wait_ge(bar["sem"], bar["val"])
                for sem, val in fin:
                    e.wait_ge(sem, val)
        self.es.close()

NT = 32
T = NT * 128
D = 1024
NL = 4
NE = 32
CAP = 1536
SEG = 256
NSLOT = NE * CAP
INC = 3080
EPS = 1e-6
NEGM = -240000.0
TWO_PI = 2.0 * math.pi
MAGIC = 12582912.0
INV_FREQ = [500000.0 ** (-(2 * i) / 16.0) for i in range(8)]


class K:
    def __init__(self, nlayers=NL, dbg=False, stop=None):
        self.nl = nlayers
        self.dbg = dbg
        self.stop = stop
        nc = self.nc = bass.Bass("TRN2", target_bir_lowering=False)
        self.P = Prog(nc)
        self.pes = contextlib.ExitStack()
        self.ins = {}
        self.dr = {}
        self.build()

    def din(self, name, shape, dt=F32):
        self.ins[name] = self.nc.dram_tensor(name, list(shape), dt, kind="ExternalInput").ap()
        return self.ins[name]

    def dscr(self, name, shape, dt):
        kind = "ExternalOutput" if self.dbg else "Internal"
        t = self.nc.dram_tensor(name, list(shape), dt, kind=kind).ap()
        self.dr[name] = t
        return t

    def sb(self, es, name, shape, dt):
        self._uid = getattr(self, "_uid", 0) + 1
        name = f"{name}_u{self._uid}"
        t = es.enter_context(self.nc.sbuf_tensor(name, list(shape), dt))
        return t, self.P.res(name)

    def ps(self, es, name, shape, dt):
        self._uid = getattr(self, "_uid", 0) + 1
        name = f"{name}_u{self._uid}"
        n = 1
        for d_ in shape[1:]:
            n *= d_
        full = 512 if dt == F32 else 1024
        assert n <= full
        if n == full:
            t = es.enter_context(self.nc.psum_tensor(name, list(shape), dt))
        else:
            assert len(shape) == 2
            tb = es.enter_context(self.nc.psum_tensor(name, [shape[0], full], dt))
            t = tb[:, 0:shape[1]]
        return t, self.P.res(name, excl=True)

    def indirect(self, e, **kw):
        self._nreg = getattr(self, "_nreg", 0) + 1
        reg = e.alloc_register(f"bnd{self._nreg}")
        e.reg_mov(reg, NSLOT - 1)
        r = e.indirect_dma_start(bounds_check=reg, oob_is_err=False, **kw)
        e.free_register(reg)
        return r

    def dump(self, name, ap, res, shape, dt):
        if not self.dbg:
            return
        t = self.nc.dram_tensor("dbg_" + name, list(shape), dt, kind="ExternalOutput").ap()
        self.P.op("sp", lambda e: e.dma_start(out=t, in_=ap), reads=[res], dmakey="dbg_" + name)

    def build(self):
        nc, P = self.nc, self.P
        I = self.ins
        self.din("x", [T, D]); self.din("cT", [128, 8]); self.din("pos", [128, NT], I32)
        self.din("w_ada", [NLIN, D, 6 * D]); self.din("b_ada", [NLIN, 6 * D]); self.din("g_mix", [NLIN, D])
        self.din("w_in", [NLIN, D, INC]); self.din("b_forget", [NLIN, 8])
        for n in ("lambda_q1", "lambda_k1", "lambda_q2", "lambda_k2"):
            self.din(n, [NLIN, 64])
        self.din("g_subln", [NLIN, 128]); self.din("g_fox_out", [NLIN, 64]); self.din("w_out", [NLIN, D, D])
        self.din("g_ffn", [NLIN, D]); self.din("w_router_group", [NLIN, D, 4]); self.din("b_router_group", [NLIN, 4])
        self.din("w_router_expert", [NLIN, D, 32]); self.din("b_router_expert", [NLIN, 32])
        self.din("w_expert_gate", [NLIN, NE, D, 512]); self.din("w_expert_up", [NLIN, NE, D, 512])
        self.din("w_expert_down", [NLIN, NE, 512, D]); self.din("g_final", [D])
        self.y = nc.dram_tensor("y", [T, D], F32, kind="ExternalOutput").ap()
        self.Ry = P.res("y")
        self.xres = self.dscr("xres", [T, D], F32); self.Rxres = [P.res("xres0"), P.res("xres1")]
        self.qktd = self.dscr("qktd", [8, 128, T], BF16); self.Rqktd = [P.res("qktd0"), P.res("qktd1")]
        self.ktf = self.dscr("ktf", [4, 128, T], BF16); self.Rktf = [P.res("ktf0"), P.res("ktf1")]
        self.qtf = self.dscr("qtf", [8, 67, T], BF16); self.Rqtf = [P.res("qtf0"), P.res("qtf1")]
        self.vd = self.dscr("vd", [T, 512], BF16); self.Rvd = [P.res("vd0"), P.res("vd1")]
        self.vf = self.dscr("vf", [T, 512], BF16); self.Rvf = [P.res("vf0"), P.res("vf1")]
        self.ocat = self.dscr("ocat", [T, D], BF16); self.Rocat = [P.res("ocat0"), P.res("ocat1")]
        self.xsd = self.dscr("xsd", [NSLOT, D], BF16); self.Rxsd = P.res("xsd")
        self.ysd = self.dscr("ysd", [NSLOT, D], BF16); self.Rysd = P.res("ysd")
        self.setup()
        for l in range(self.nl):
            self.phase0(l)
            if l == 0:
                self.dump("modt", self.modt[:], self.Rmodt, [128, 6, D], F32)
                self.dump("cosT", self.cosT[:], self.RcosT, [128, NT, 8], F32)
                self.dump("sinT", self.sinT[:], self.RsinT, [128, NT, 8], F32)
                self.dump("neglam", self.neglam[:], self.Rneglam, [128, 1], F32)
            if self.stop == ("p0", l): break
            self.phase1(l)
            if self.stop == ("p1", l): break
            self.phase2(l)
            if self.stop == ("p2", l): break
            self.phase3a(l)
            if self.stop == ("p3a", l): break
            self.phase3b(l)
            if self.stop == ("p3b", l): break
        else:
            self.phase_final()
        P.finish(self.scr[:])
        self.pes.close()

    def setup(self):
        nc, P, I = self.nc, self.P, self.ins
        pes = self.pes
        self.scr, P.scratch_res = self.sb(pes, "scr", [128, 8], F32)
        self.ident, self.Rident = self.sb(pes, "ident", [128, 128], BF16)
        self.identf, self.Ridentf = self.sb(pes, "identf", [128, 128], F32)
        self.triU, self.RtriU = self.sb(pes, "triU", [128, 128], F32)
        self.triS, self.RtriS = self.sb(pes, "triS", [128, 128], F32)
        self.e127, self.Re127 = self.sb(pes, "e127", [128, 128], F32)
        self.ones, self.Rones = self.sb(pes, "ones", [128, 128], F32)
        self.maskF, self.RmaskF = self.sb(pes, "maskF", [128, 128], BF16)
        self.maskD, self.RmaskD = self.sb(pes, "maskD", [128, 128], BF16)
        self.cosT, self.RcosT = self.sb(pes, "cosT", [128, NT, 8], F32)
        self.sinT, self.RsinT = self.sb(pes, "sinT", [128, NT, 8], F32)
        self.condB, self.RcondB = self.sb(pes, "condB", [128, 8, 128], F32)
        self.modt, self.Rmodt = self.sb(pes, "modt", [128, 6, D], F32)
        self.ncf, self.Rncf = self.sb(pes, "ncf", [128, NT, 8], F32)
        self.dest, self.Rdest = self.sb(pes, "dest", [128, NT, 2], I32)
        self.wts, self.Rwts = self.sb(pes, "wts", [128, NT, 2], F32)
        self.sbase, self.Rsbase = self.sb(pes, "sbase", [128, NE], F32)
        self.neglam, self.Rneglam = self.sb(pes, "neglam", [128, 1], F32)
        self.gt2p, self.Rgt2p = self.sb(pes, "gt2p", [128, D], F32)
        self.cnti, self.Rcnti = self.sb(pes, "cnti", [128, NE], I32)
        with contextlib.ExitStack() as es:
            tmpf, Rtmpf = self.sb(es, "su_tmpf", [128, 128], F32)
            posi, Rposi = self.sb(es, "su_posi", [128, NT], I32)
            posf, Rposf = self.sb(es, "su_posf", [128, NT], F32)
            ang, Rang = self.sb(es, "su_ang", [128, NT, 8], F32)
            a2, Ra2 = self.sb(es, "su_a2", [128, NT, 8], F32)
            kk, Rkk = self.sb(es, "su_kk", [128, NT, 8], F32)
            cs, Rcs = self.sb(es, "su_cs", [128, 8], F32)

            W = [self.Rident, self.Ridentf, self.RtriU, self.RtriS, self.Re127, self.Rones,
                 self.RmaskF, self.RmaskD, self.Rsbase, Rtmpf, P.scratch_res]
            fns = [
                lambda e: e.memset(self.identf[:], 0.0),
                lambda e: e.affine_select(out=self.identf[:], in_=self.identf[:], pattern=[[-1, 128]], compare_op=ALU.not_equal,
                                          fill=1.0, base=0, channel_multiplier=1),
                lambda e: e.tensor_copy(out=self.ident[:], in_=self.identf[:]),
                lambda e: e.memset(self.triU[:], 1.0),
                lambda e: e.affine_select(out=self.triU[:], in_=self.triU[:], pattern=[[1, 128]], compare_op=ALU.is_ge,
                                          fill=0.0, base=0, channel_multiplier=-1),
                lambda e: e.memset(self.triS[:], 1.0),
                lambda e: e.affine_select(out=self.triS[:], in_=self.triS[:], pattern=[[1, 128]], compare_op=ALU.is_gt,
                                          fill=0.0, base=0, channel_multiplier=-1),
                lambda e: e.memset(self.e127[:], 0.0),
                lambda e: e.affine_select(out=self.e127[:], in_=self.e127[:], pattern=[[0, 128]], compare_op=ALU.not_equal,
                                          fill=1.0, base=-127, channel_multiplier=1),
                lambda e: e.memset(self.ones[:], 1.0),
                lambda e: e.memset(tmpf[:], 0.0),
                lambda e: e.affine_select(out=tmpf[:], in_=tmpf[:], pattern=[[1, 128]], compare_op=ALU.is_ge,
                                          fill=NEGM, base=0, channel_multiplier=-1),
                lambda e: e.tensor_copy(out=self.maskF[:], in_=tmpf[:]),
                lambda e: e.memset(self.maskD[:], 0.0),
                lambda e: e.memset(self.maskD[64:128, 0:64], NEGM),
                lambda e: e.memset(self.scr[:], 0.0),
            ]
            for j in range(NE):
                fns.append(lambda e, j=j: e.memset(self.sbase[:, j:j + 1], float(j * CAP)))
            P.chain("pool", fns, writes=W)
            zt, Rzt = self.sb(es, "su_zt", [128, 8192], BF16)
            P.op("pool", lambda e: e.memset(zt[:], 0.0), writes=[Rzt])
            nz = NSLOT // 1024 if self.dbg else 0
            P.op("sp", lambda e: [e.dma_start(out=self.xsd[c * 1024:(c + 1) * 1024, :].rearrange("(p a) d -> p (a d)", p=128), in_=zt[:]) for c in range(nz)],
                 reads=[Rzt], writes=[self.Rxsd], dmakey="su0", ndma=nz) if nz else None
            P.op("sp", lambda e: [e.dma_start(out=self.ysd[c * 1024:(c + 1) * 1024, :].rearrange("(p a) d -> p (a d)", p=128), in_=zt[:]) for c in range(nz)],
                 reads=[Rzt], writes=[self.Rysd], dmakey="su0y", ndma=nz) if nz else None
            P.op("sp", lambda e: e.dma_start(out=posi[:], in_=I["pos"]), writes=[Rposi], dmakey="su1")
            P.op("sp", lambda e: e.dma_start(out=cs[:], in_=I["cT"]), writes=[Rcs], dmakey="su2")

            P.op("dve", lambda e: e.tensor_copy(out=posf[:], in_=posi[:]), reads=[Rposi], writes=[Rposf])

            def angles(e):
                r = None
                for i in range(8):
                    r = e.tensor_scalar(out=ang[:, :, i], in0=posf[:], scalar1=float(INV_FREQ[i]), scalar2=None, op0=ALU.mult)
                return r
            P.op("dve", angles, reads=[Rposf], writes=[Rang])

            def reduce_sin(dst, Rdst, shift):
                P.op("dve", lambda e: e.tensor_scalar(out=a2[:], in0=ang[:], scalar1=float(shift), scalar2=None, op0=ALU.add),
                     reads=[Rang], writes=[Ra2])
                P.op("dve", lambda e: e.tensor_scalar(out=kk[:], in0=a2[:], scalar1=1.0 / TWO_PI, scalar2=MAGIC, op0=ALU.mult, op1=ALU.add),
                     reads=[Ra2], writes=[Rkk])
                P.op("dve", lambda e: e.tensor_scalar(out=kk[:], in0=kk[:], scalar1=MAGIC, scalar2=None, op0=ALU.subtract),
                     reads=[Rkk], writes=[Rkk])
                P.op("dve", lambda e: e.scalar_tensor_tensor(out=a2[:], in0=kk[:], scalar=-TWO_PI, in1=a2[:], op0=ALU.mult, op1=ALU.add),
                     reads=[Rkk, Ra2], writes=[Ra2])
                P.op("dve", lambda e: e.tensor_scalar(out=kk[:], in0=a2[:], scalar1=math.pi, scalar2=-TWO_PI, op0=ALU.is_gt, op1=ALU.mult),
                     reads=[Ra2], writes=[Rkk])
                P.op("dve", lambda e: e.tensor_tensor(out=a2[:], in0=a2[:], in1=kk[:], op=ALU.add), reads=[Ra2, Rkk], writes=[Ra2])
                P.op("dve", lambda e: e.tensor_scalar(out=kk[:], in0=a2[:], scalar1=-math.pi, scalar2=TWO_PI, op0=ALU.is_lt, op1=ALU.mult),
                     reads=[Ra2], writes=[Rkk])
                P.op("dve", lambda e: e.tensor_tensor(out=a2[:], in0=a2[:], in1=kk[:], op=ALU.add), reads=[Ra2, Rkk], writes=[Ra2])
                P.op("dve", lambda e: e.tensor_scalar(out=a2[:], in0=a2[:], scalar1=math.pi, scalar2=-math.pi, op0=ALU.min, op1=ALU.max),
                     reads=[Ra2], writes=[Ra2])
                P.op("act", lambda e: e.activation(out=dst[:], in_=a2[:], func=AF.Sin), reads=[Ra2], writes=[Rdst])
            reduce_sin(self.sinT, self.RsinT, 0.0)
            reduce_sin(self.cosT, self.RcosT, math.pi / 2)
            P.op("act", lambda e: e.activation(out=cs[:], in_=cs[:], func=AF.Silu), reads=[Rcs], writes=[Rcs])
            P.op("dve", lambda e: e.tensor_copy(out=self.condB[:], in_=cs[:].unsqueeze(2).to_broadcast([128, 8, 128])),
                 reads=[Rcs], writes=[self.RcondB])
            P.flush(self.scr[:])

    def phase0(self, l):
        nc, P, I = self.nc, self.P, self.ins
        with contextlib.ExitStack() as es:
            wa = [self.sb(es, f"p0_wa{j}", [128, 8, 512], F32) for j in range(2)]
            bb = [self.sb(es, f"p0_bb{j}", [128, 512], F32) for j in range(2)]
            pm = [self.ps(es, f"p0_pm{j}", [128, 512], F32) for j in range(2)]
            gb, Rgb = self.sb(es, "p0_gb", [128, D], F32)
            lam4, Rlam4 = self.sb(es, "p0_lam", [128, 4, 64], F32)
            ltmp, Rltmp = self.sb(es, "p0_ltmp", [128, 2, 64], F32)
            ls, Rls = self.sb(es, "p0_ls", [128, 2], F32)
            if l > 0:
                P.op("pool", lambda e: e.tensor_copy(out=self.gt2p[:], in_=self.modt[:, 5, :]), reads=[self.Rmodt], writes=[self.Rgt2p])
            for g in range(12):
                def grp0_(g):
                    j = g % 2
                    (wt, Rw), (bt, Rb), (pt, Rp) = wa[j], bb[j], pm[j]
                    src = I["w_ada"][l, :, g * 512:(g + 1) * 512].rearrange("(k p) n -> p k n", p=128)
                    P.op("sp", lambda e, wt=wt, src=src: e.dma_start(out=wt[:], in_=src), writes=[Rw], dmakey=f"p0w{j}")
                    bsrc = I["b_ada"][l, g * 512:(g + 1) * 512].partition_broadcast(128)
                    P.op("sp", lambda e, bt=bt, bsrc=bsrc: e.dma_start(out=bt[:], in_=bsrc), writes=[Rb], dmakey=f"p0b{j}")

                    def mm(e, wt=wt, pt=pt):
                        r = None
                        for k in range(8):
                            r = e.matmul(pt[:], lhsT=self.condB[:, k, :], rhs=wt[:, k, :], start=(k == 0), stop=(k == 7))
                        return r
                    P.op("pe", mm, reads=[Rw, self.RcondB], writes=[Rp])
                    dst = self.modt[:, g // 2, (g % 2) * 512:(g % 2 + 1) * 512]
                    P.op("dve", lambda e, dst=dst, pt=pt, bt=bt: e.tensor_tensor(out=dst, in0=pt[:], in1=bt[:], op=ALU.add),
                         reads=[Rp, Rb], writes=[self.Rmodt])
                grp0_(g)
            for (gname, idx) in (("g_mix", 1), ("g_ffn", 4)):
                gsrc = I[gname][l, :].partition_broadcast(128)
                P.op("sp", lambda e, gsrc=gsrc: e.dma_start(out=gb[:], in_=gsrc), writes=[Rgb], dmakey="p0g")
                P.op("dve", lambda e, idx=idx: e.scalar_tensor_tensor(out=self.modt[:, idx, :], in0=self.modt[:, idx, :], scalar=1.0,
                                                                      in1=gb[:], op0=ALU.add, op1=ALU.mult),
                     reads=[Rgb, self.Rmodt], writes=[self.Rmodt])
            for qi, n in enumerate(("lambda_q1", "lambda_k1", "lambda_q2", "lambda_k2")):
                lsrc = I[n][l, :].partition_broadcast(128)
                P.op("sp", lambda e, qi=qi, lsrc=lsrc: e.dma_start(out=lam4[:, qi, :], in_=lsrc), writes=[Rlam4], dmakey=f"p0l{qi}")
            lam_init = 0.8 - 0.6 * math.exp(-0.3 * l)

            def lamf(e):
                e.tensor_tensor(out=ltmp[:, 0, :], in0=lam4[:, 0, :], in1=lam4[:, 1, :], op=ALU.mult)
                return e.tensor_tensor(out=ltmp[:, 1, :], in0=lam4[:, 2, :], in1=lam4[:, 3, :], op=ALU.mult)
            P.op("dve", lamf, reads=[Rlam4], writes=[Rltmp])
            P.op("dve", lambda e: e.tensor_reduce(out=ls[:], in_=ltmp[:], axis=AX.X, op=ALU.add), reads=[Rltmp], writes=[Rls])
            P.op("act", lambda e: e.activation(out=ls[:], in_=ls[:], func=AF.Exp), reads=[Rls], writes=[Rls])
            P.op("dve", lambda e: e.scalar_tensor_tensor(out=self.neglam[:], in0=ls[:, 1:2], scalar=-lam_init, in1=ls[:, 0:1],
                                                         op0=ALU.add, op1=ALU.subtract),
                 reads=[Rls], writes=[self.Rneglam])
            P.flush(self.scr[:])

    def rstd_ops(self, xt, Rx, junk, Rjunk, ss, Rss, n):
        P = self.P
        P.op("act", lambda e: e.activation(out=junk[:], in_=xt, func=AF.Square, accum_out=ss[:]), reads=[Rx], writes=[Rjunk, Rss])
        P.op("act", lambda e: e.activation(out=ss[:], in_=ss[:], func=AF.Ln, scale=1.0 / n, bias=EPS), reads=[Rss], writes=[Rss])
        P.op("act", lambda e: e.activation(out=ss[:], in_=ss[:], func=AF.Exp, scale=-0.5), reads=[Rss], writes=[Rss])

    def phase1(self, l):
        nc, P, I = self.nc, self.P, self.ins
        with contextlib.ExitStack() as es:
            win = [self.sb(es, f"p1_win{k}", [128, INC], BF16) for k in range(8)]
            Rwin = [w[1] for w in win]
            P.op("pool", lambda e: [e.dma_start(out=win[k][0][:], in_=I["w_in"][l, k * 128:(k + 1) * 128, :]) for k in range(8)],
                 writes=Rwin, dmakey="p1win", ndma=8)
            bfb, Rbfb = self.sb(es, "p1_bfb", [128, 8], F32)
            P.op("sp", lambda e: e.dma_start(out=bfb[:], in_=I["b_forget"][l, :].partition_broadcast(128)), writes=[Rbfb], dmakey="p1bfb")
            xs = [self.sb(es, f"p1_xs{j}", [128, D], F32) for j in range(2)]
            tmp, Rtmp = self.sb(es, "p1_tmp", [128, D], F32)
            junk, Rjunk = self.sb(es, "p1_junk", [128, D], BF16)
            hb = [self.sb(es, f"p1_hb{j}", [128, D], BF16) for j in range(2)]
            hT = [self.sb(es, f"p1_hT{j}", [128, 8, 128], BF16) for j in range(2)]
            qk = [self.sb(es, f"p1_qk{j}", [128, 1024], BF16) for j in range(2)]
            vds = [self.sb(es, f"p1_vds{j}", [128, 512], BF16) for j in range(2)]
            vfs = [self.sb(es, f"p1_vfs{j}", [128, 512], BF16) for j in range(2)]
            kf = [self.sb(es, f"p1_kf{j}", [128, 512], BF16) for j in range(2)]
            qfa = [self.sb(es, f"p1_qfa{j}", [128, 8, 67], BF16) for j in range(2)]
            stqk = [self.sb(es, f"p1_stqk{j}", [128, 8, 128], BF16) for j in range(2)]
            stkf = [self.sb(es, f"p1_stkf{j}", [128, 4, 128], BF16) for j in range(2)]
            stqf = [self.sb(es, f"p1_stqf{j}", [128, 8, 128], BF16) for j in range(2)]
            ss = [self.sb(es, f"p1_ss{j}", [128, 1], F32) for j in range(2)]
            rt, Rrt = self.sb(es, "p1_rt", [128, 4, 8, 8], F32)
            fz, Rfz = self.sb(es, "p1_fz", [128, 8], F32)
            fnz, Rfnz = self.sb(es, "p1_fnz", [128, 8], F32)
            fa, Rfa = self.sb(es, "p1_fa", [128, 8], F32)
            fl, Rfl = self.sb(es, "p1_fl", [128, 8], F32)
            cf = [self.sb(es, f"p1_cf{j}", [128, 8], F32) for j in range(2)]
            g8, Rg8 = self.sb(es, "p1_g8", [128, 8], F32)
            r1, Rr1 = self.sb(es, "p1_r1", [128, 8], F32)
            if l > 0:
                y0 = [self.sb(es, f"p1_y0{j}", [128, D], BF16) for j in range(2)]
                y1 = [self.sb(es, f"p1_y1{j}", [128, D], BF16) for j in range(2)]
            tp = [self.ps(es, f"p1_tp{j}", [128, 8, 128], BF16) for j in range(2)]
            pg = [self.ps(es, f"p1_pg{j}", [128, 512], F32) for j in range(3)]
            pff, Rpff = self.ps(es, "p1_pff", [128, 8], F32)
            pcs, Rpcs = self.ps(es, "p1_pcs", [128, 8], F32)
            tpi = [0]
            pgi = [0]

            def stageA(i):
                b = i % 2
                xt, Rx = xs[b]
                rows = slice(i * 128, (i + 1) * 128)
                if l == 0:
                    P.op("sp", lambda e: e.dma_start(out=xt[:], in_=I["x"][rows, :]), writes=[Rx], dmakey=f"p1x{b}")
                    return
                self.combine(i, xt, Rx, y0[b], y1[b], tmp, Rtmp, self.gt2p[:], self.Rgt2p, f"p1x{b}", f"p1y{b}")
                P.op("sp", lambda e: e.dma_start(out=self.xres[rows, :], in_=xt[:]), reads=[Rx], writes=[self.Rxres[b]], dmakey=f"p1xo{b}")

            stageA(0)
            for i in range(NT):
                def tile1_(i):
                    b = i % 2
                    if i + 1 < NT:
                        stageA(i + 1)
                    xt, Rx = xs[b]
                    rows = slice(i * 128, (i + 1) * 128)
                    sst, Rss = ss[b]
                    self.rstd_ops(xt[:], Rx, junk, Rjunk, sst, Rss, D)
                    P.op("dve", lambda e, xt=xt, sst=sst: e.scalar_tensor_tensor(out=tmp[:], in0=xt[:], scalar=sst[:, 0:1], in1=self.modt[:, 1, :],
                                                                               op0=ALU.mult, op1=ALU.mult),
                         reads=[Rx, Rss, self.Rmodt], writes=[Rtmp])
                    hbt, Rhb = hb[b]
                    P.op("dve", lambda e, hbt=hbt: e.tensor_tensor(out=hbt[:], in0=tmp[:], in1=self.modt[:, 0, :], op=ALU.add),
                         reads=[Rtmp, self.Rmodt], writes=[Rhb])
                    tpt, Rtp = tp[tpi[0] % 2]; tpi[0] += 1
                    hTt, RhT = hT[b]

                    def trh(e, hbt=hbt, tpt=tpt):
                        r = None
                        for k in range(8):
                            r = e.transpose(out=tpt[:, k, :], in_=hbt[:, k * 128:(k + 1) * 128], identity=self.ident[:])
                        return r
                    P.op("pe", trh, reads=[Rhb, self.Rident], writes=[Rtp])
                    P.op("act", lambda e, hTt=hTt, tpt=tpt: e.activation(out=hTt[:], in_=tpt[:], func=AF.Copy), reads=[Rtp], writes=[RhT])
                    qkt, Rqk = qk[b]
                    qk3 = qkt[:].rearrange("p (m d) -> p m d", d=64)
                    qfat, Rqfa = qfa[b]
                    for g in range(6):
                        pgt, Rpg = pg[pgi[0] % 3]; pgi[0] += 1

                        def mm(e, g=g, pgt=pgt, hTt=hTt):
                            r = None
                            for k in range(8):
                                r = e.matmul(pgt[:], lhsT=hTt[:, k, :], rhs=win[k][0][:, g * 512:(g + 1) * 512], start=(k == 0), stop=(k == 7))
                            return r
                        P.op("pe", mm, reads=[RhT] + Rwin, writes=[Rpg])
                        pg3 = pgt[:].rearrange("p (m d) -> p m d", d=64)
                        if g < 2:
                            m0 = g * 8
                            P.op("act", lambda e, pg3=pg3, qk3=qk3, m0=m0: e.activation(out=qk3[:, m0:m0 + 8, 16:64], in_=pg3[:, :, 16:64], func=AF.Copy),
                                 reads=[Rpg], writes=[Rqk])
                            cosb = self.cosT[:, i, :].unsqueeze(1).to_broadcast([128, 8, 8])
                            sinb = self.sinT[:, i, :].unsqueeze(1).to_broadcast([128, 8, 8])

                            def rope1(e, pg3=pg3, cosb=cosb, sinb=sinb):
                                e.tensor_tensor(out=rt[:, 0], in0=pg3[:, :, 0:8], in1=cosb, op=ALU.mult)
                                e.tensor_tensor(out=rt[:, 1], in0=pg3[:, :, 8:16], in1=sinb, op=ALU.mult)
                                e.tensor_tensor(out=rt[:, 2], in0=pg3[:, :, 8:16], in1=cosb, op=ALU.mult)
                                return e.tensor_tensor(out=rt[:, 3], in0=pg3[:, :, 0:8], in1=sinb, op=ALU.mult)
                            P.op("dve", rope1, reads=[Rpg, self.RcosT, self.RsinT], writes=[Rrt])

                            def rope2(e, qk3=qk3, m0=m0):
                                e.tensor_tensor(out=qk3[:, m0:m0 + 8, 0:8], in0=rt[:, 0], in1=rt[:, 1], op=ALU.subtract)
                                return e.tensor_tensor(out=qk3[:, m0:m0 + 8, 8:16], in0=rt[:, 2], in1=rt[:, 3], op=ALU.add)
                            P.op("dve", rope2, reads=[Rrt], writes=[Rqk])
                        elif g == 2:
                            vt, Rv = vds[b]
                            P.op("act", lambda e, vt=vt, pgt=pgt: e.activation(out=vt[:], in_=pgt[:], func=AF.Copy), reads=[Rpg], writes=[Rv])
                            P.op("sp", lambda e, vt=vt, rows=rows: e.dma_start(out=self.vd[rows, :], in_=vt[:]), reads=[Rv], writes=[self.Rvd[b]], dmakey=f"p1vd{b}")
                        elif g == 3:
                            P.op("act", lambda e, qfat=qfat, pg3=pg3: e.activation(out=qfat[:, :, 0:64], in_=pg3, func=AF.Copy), reads=[Rpg], writes=[Rqfa])
                        elif g == 4:
                            kt, Rk = kf[b]
                            P.op("act", lambda e, kt=kt, pgt=pgt: e.activation(out=kt[:], in_=pgt[:], func=AF.Copy), reads=[Rpg], writes=[Rk])
                        elif g == 5:
                            vt2, Rv2 = vfs[b]
                            P.op("act", lambda e, vt2=vt2, pgt=pgt: e.activation(out=vt2[:], in_=pgt[:], func=AF.Copy), reads=[Rpg], writes=[Rv2])
                            P.op("sp", lambda e, vt2=vt2, rows=rows: e.dma_start(out=self.vf[rows, :], in_=vt2[:]), reads=[Rv2], writes=[self.Rvf[b]], dmakey=f"p1vf{b}")
                    def mmf(e, hTt=hTt):
                        r = None
                        for k in range(8):
                            r = e.matmul(pff[:], lhsT=hTt[:, k, :], rhs=win[k][0][:, 3072:3080], start=(k == 0), stop=(k == 7))
                        return r
                    P.op("pe", mmf, reads=[RhT] + Rwin, writes=[Rpff])
                    P.op("dve", lambda e: e.tensor_tensor(out=fz[:], in0=pff[:], in1=bfb[:], op=ALU.add), reads=[Rpff, Rbfb], writes=[Rfz])
                    P.op("dve", lambda e: e.tensor_scalar(out=fnz[:], in0=fz[:], scalar1=-1.0, scalar2=None, op0=ALU.mult), reads=[Rfz], writes=[Rfnz])
                    P.op("dve", lambda e: e.tensor_tensor(out=fa[:], in0=fz[:], in1=fnz[:], op=ALU.max), reads=[Rfz, Rfnz], writes=[Rfa])
                    P.op("act", lambda e: e.activation(out=fa[:], in_=fa[:], func=AF.Exp, scale=-1.0), reads=[Rfa], writes=[Rfa])
                    P.op("act", lambda e: e.activation(out=fa[:], in_=fa[:], func=AF.Ln, bias=1.0), reads=[Rfa], writes=[Rfa])
                    P.op("dve", lambda e: e.scalar_tensor_tensor(out=fl[:], in0=fnz[:], scalar=0.0, in1=fa[:], op0=ALU.max, op1=ALU.add),
                         reads=[Rfnz, Rfa], writes=[Rfl])
                    P.op("dve", lambda e: e.tensor_scalar(out=fl[:], in0=fl[:], scalar1=-1.0, scalar2=None, op0=ALU.mult), reads=[Rfl], writes=[Rfl])
                    cft, Rcf = cf[b]
                    cfp, Rcfp = cf[1 - b]

                    def mmc(e, i=i, cfp=cfp):
                        r = e.matmul(pcs[:], lhsT=self.triU[:], rhs=fl[:], start=True, stop=(i == 0))
                        if i > 0:
                            r = e.matmul(pcs[:], lhsT=self.e127[:], rhs=cfp[:], start=False, stop=True)
                        return r
                    P.op("pe", mmc, reads=[Rfl, self.RtriU, self.Re127] + ([Rcfp] if i > 0 else []), writes=[Rpcs])

                    def cfev(e, cft=cft, i=i):
                        e.tensor_copy(out=cft[:], in_=pcs[:])
                        e.tensor_scalar(out=self.ncf[:, i, :], in0=pcs[:], scalar1=-1.0, scalar2=None, op0=ALU.mult)
                        return e.tensor_scalar(out=g8[:], in0=pcs[:], scalar1=8.0, scalar2=None, op0=ALU.mult)
                    P.op("dve", cfev, reads=[Rpcs], writes=[Rcf, self.Rncf, Rg8])
                    P.op("dve", lambda e, qfat=qfat: e.tensor_copy(out=qfat[:, :, 64], in_=g8[:]), reads=[Rg8], writes=[Rqfa])
                    P.op("dve", lambda e, qfat=qfat: e.tensor_tensor(out=r1[:], in0=g8[:], in1=qfat[:, :, 64], op=ALU.subtract), reads=[Rg8, Rqfa], writes=[Rr1])
                    P.op("dve", lambda e, qfat=qfat: e.tensor_copy(out=qfat[:, :, 65], in_=r1[:]), reads=[Rr1], writes=[Rqfa])
                    P.op("dve", lambda e, qfat=qfat: e.tensor_tensor(out=g8[:], in0=r1[:], in1=qfat[:, :, 65], op=ALU.subtract), reads=[Rr1, Rqfa], writes=[Rg8])
                    P.op("dve", lambda e, qfat=qfat: e.tensor_copy(out=qfat[:, :, 66], in_=g8[:]), reads=[Rg8], writes=[Rqfa])
                    tpt, Rtp = tp[tpi[0] % 2]; tpi[0] += 1

                    def trqk(e, qkt=qkt, tpt=tpt):
                        r = None
                        for j in range(8):
                            r = e.transpose(out=tpt[:, j, :], in_=qkt[:, j * 128:(j + 1) * 128], identity=self.ident[:])
                        return r
                    P.op("pe", trqk, reads=[Rqk, self.Rident], writes=[Rtp])
                    st, Rst = stqk[b]
                    P.op("dve", lambda e, st=st, tpt=tpt: e.tensor_copy(out=st[:], in_=tpt[:]), reads=[Rtp], writes=[Rst])
                    P.op("sp", lambda e, st=st, rows=rows: e.dma_start(out=self.qktd[:, :, rows].rearrange("j r t -> r j t"), in_=st[:]),
                         reads=[Rst], writes=[self.Rqktd[b]], dmakey=f"p1sq{b}")
                    tpt2, Rtp2 = tp[tpi[0] % 2]; tpi[0] += 1
                    kt, Rk = kf[b]

                    def trkf(e, kt=kt, tpt2=tpt2):
                        r = None
                        for j in range(4):
                            r = e.transpose(out=tpt2[:, j, :], in_=kt[:, j * 128:(j + 1) * 128], identity=self.ident[:])
                        return r
                    P.op("pe", trkf, reads=[Rk, self.Rident], writes=[Rtp2])
                    st2, Rst2 = stkf[b]
                    P.op("act", lambda e, st2=st2, tpt2=tpt2: e.activation(out=st2[:], in_=tpt2[:, 0:4, :], func=AF.Copy), reads=[Rtp2], writes=[Rst2])
                    P.op("sp", lambda e, st2=st2, rows=rows: e.dma_start(out=self.ktf[:, :, rows].rearrange("j r t -> r j t"), in_=st2[:]),
                         reads=[Rst2], writes=[self.Rktf[b]], dmakey=f"p1sk{b}")
                    tpt3, Rtp3 = tp[tpi[0] % 2]; tpi[0] += 1

                    def trqf(e, qfat=qfat, tpt3=tpt3):
                        r = None
                        for h in range(8):
                            r = e.transpose(out=tpt3[0:67, h, :], in_=qfat[:, h, :], identity=self.ident[:])
                        return r
                    P.op("pe", trqf, reads=[Rqfa, self.Rident], writes=[Rtp3])
                    st3, Rst3 = stqf[b]
                    P.op("dve", lambda e, st3=st3, tpt3=tpt3: e.tensor_copy(out=st3[0:67], in_=tpt3[0:67]), reads=[Rtp3], writes=[Rst3])
                    P.op("sp", lambda e, st3=st3, rows=rows: e.dma_start(out=self.qtf[:, :, rows].rearrange("h r t -> r h t"), in_=st3[0:67]),
                         reads=[Rst3], writes=[self.Rqtf[b]], dmakey=f"p1sf{b}")
                tile1_(i)
            P.flush(self.scr[:])

    def combine(self, i, xt, Rx, y0p, y1p, tmp, Rtmp, gt2, Rgt2, kx, ky):
        P = self.P
        rows = slice(i * 128, (i + 1) * 128)
        (y0t, Ry0), (y1t, Ry1) = y0p, y1p
        P.op("sp", lambda e: e.dma_start(out=xt[:], in_=self.xres[rows, :]), reads=self.Rxres, writes=[Rx], dmakey=kx)
        for k, (yt, Ry) in enumerate(((y0t, Ry0), (y1t, Ry1))):
            P.op("pool", lambda e, yt=yt: e.memset(yt[:], 0.0), writes=[Ry])
            P.op("pool", lambda e, yt=yt, k=k: self.indirect(
                e, out=yt[:, :], out_offset=None, in_=self.ysd[:, :],
                in_offset=bass.IndirectOffsetOnAxis(ap=self.dest[:, i, k:k + 1], axis=0)),
                reads=[self.Rysd, self.Rdest], writes=[Ry], dmakey=ky + str(k))
        P.op("dve", lambda e: e.tensor_scalar(out=tmp[:], in0=y0t[:], scalar1=self.wts[:, i, 0:1], scalar2=None, op0=ALU.mult),
             reads=[Ry0, self.Rwts], writes=[Rtmp])
        P.op("dve", lambda e: e.scalar_tensor_tensor(out=tmp[:], in0=y1t[:], scalar=self.wts[:, i, 1:2], in1=tmp[:], op0=ALU.mult, op1=ALU.add),
             reads=[Ry1, self.Rwts, Rtmp], writes=[Rtmp])
        P.op("dve", lambda e: e.tensor_tensor(out=tmp[:], in0=tmp[:], in1=gt2, op=ALU.mult), reads=[Rtmp, Rgt2], writes=[Rtmp])
        P.op("dve", lambda e: e.tensor_tensor(out=xt[:], in0=xt[:], in1=tmp[:], op=ALU.add), reads=[Rtmp, Rx], writes=[Rx])

    def phase2(self, l):
        nc, P, I = self.nc, self.P, self.ins
        lam_init = 0.8 - 0.6 * math.exp(-0.3 * l)
        with contextlib.ExitStack() as es:
            QT = [self.sb(es, f"p2_QT{j}", [67, T], BF16) for j in range(2)]
            KT = [self.sb(es, f"p2_KT{j}", [67, T], BF16) for j in range(2)]
            VV = [self.sb(es, f"p2_V{j}", [128, NT * 129], BF16) for j in range(2)]
            PT = [self.sb(es, f"p2_PT{j}", [128, 512], BF16) for j in range(4)]
            u1, Ru1 = self.sb(es, "p2_u1", [128, NT, 128], F32)
            ot, Rot = self.sb(es, "p2_ot", [128, 2, 128], F32)
            osq, Rosq = self.sb(es, "p2_osq", [128, 2, 128], F32)
            rden, Rrden = self.sb(es, "p2_rden", [128, 2], F32)
            ssq, Rssq = self.sb(es, "p2_ssq", [128, 2], F32)
            ost = [self.sb(es, f"p2_ost{j}", [128, 2, 128], BF16) for j in range(2)]
            gsub, Rgsub = self.sb(es, "p2_gsub", [128, 128], F32)
            gfox, Rgfox = self.sb(es, "p2_gfox", [128, 64], F32)
            SP_ = [self.ps(es, f"p2_S{j}", [128, 512], F32) for j in range(3)]
            OP = [self.ps(es, f"p2_O{j}", [128, 512], F32) for j in range(4)]
            for kt, Rk in KT:
                P.op("pool", lambda e, kt=kt: e.memset(kt[64:67, :], 1.0), writes=[Rk])
            P.op("sp", lambda e: e.dma_start(out=gsub[:], in_=I["g_subln"][l, :].partition_broadcast(128)), writes=[Rgsub], dmakey="p2g1")
            P.op("sp", lambda e: e.dma_start(out=gfox[:], in_=I["g_fox_out"][l, :].partition_broadcast(128)), writes=[Rgfox], dmakey="p2g2")
            P.op("dve", lambda e: e.tensor_scalar(out=gsub[:], in0=gsub[:], scalar1=float(1.0 - lam_init), scalar2=None, op0=ALU.mult),
                 reads=[Rgsub], writes=[Rgsub])
            jobs = []
            for h in range(4):
                jobs.append(("d", h, 0)); jobs.append(("d", h, 1))
            for h in range(8):
                jobs.append(("f", h, 0))
            vslot = {}
            vcount = [0]

            def load(jn):
                kind, h, c = jobs[jn]
                b = jn % 2
                qt, Rq = QT[b]; kt, Rk = KT[b]
                if kind == "d":
                    m = 2 * h + c
                    P.op("sp", lambda e: e.dma_start(out=qt[0:64, :], in_=self.qktd[m // 2, (m % 2) * 64:(m % 2) * 64 + 64, :]),
                         reads=self.Rqktd, writes=[Rq], dmakey=f"p2q{b}")
                    mk = 8 + m
                    P.op("sp", lambda e: e.dma_start(out=kt[0:64, :], in_=self.qktd[mk // 2, (mk % 2) * 64:(mk % 2) * 64 + 64, :]),
                         reads=self.Rqktd, writes=[Rk], dmakey=f"p2k{b}")
                    if c == 0:
                        vb = vcount[0] % 2; vcount[0] += 1
                        vslot[(kind, h)] = vb
                        vt, Rv = VV[vb]
                        v3 = vt[:].rearrange("p (n d) -> p n d", d=129)
                        P.op("sp", lambda e: e.dma_start(out=v3[:, :, 0:128], in_=self.vd[:, h * 128:(h + 1) * 128].rearrange("(n p) d -> p n d", p=128)),
                             reads=self.Rvd, writes=[Rv], dmakey=f"p2v{vb}")
                        P.op("pool", lambda e: e.memset(v3[:, :, 128:129], 1.0), writes=[Rv])
                else:
                    P.op("sp", lambda e: e.dma_start(out=qt[0:67, :], in_=self.qtf[h, :, :]), reads=self.Rqtf, writes=[Rq], dmakey=f"p2q{b}")
                    P.op("sp", lambda e: e.dma_start(out=kt[0:64, :], in_=self.ktf[h // 2, (h % 2) * 64:(h % 2) * 64 + 64, :]),
                         reads=self.Rktf, writes=[Rk], dmakey=f"p2k{b}")
                    vb = vcount[0] % 2; vcount[0] += 1
                    vslot[(kind, h)] = vb
                    vt, Rv = VV[vb]
                    v3 = vt[:, 0:NT * 65].rearrange("p (n d) -> p n d", d=65)
                    P.op("sp", lambda e: e.dma_start(out=v3[:, :, 0:64], in_=self.vf[:, h * 64:(h + 1) * 64].rearrange("(n p) d -> p n d", p=128)),
                         reads=self.Rvf, writes=[Rv], dmakey=f"p2v{vb}")
                    P.op("pool", lambda e: e.memset(v3[:, :, 64:65], 1.0), writes=[Rv])

            sidx = [0]; pidx = [0]; oidx = [0]; osti = [0]
            load(0)
            for jn, (kind, h, c) in enumerate(jobs):
                def job_(jn, kind, h, c):
                    if jn + 1 < len(jobs):
                        load(jn + 1)
                    b = jn % 2
                    qt, Rq = QT[b]; kt, Rk = KT[b]
                    vt, Rv = VV[vslot[(kind, h)]]
                    if kind == "d":
                        dv = 128; KK = 64; mask, Rmask = self.maskD, self.RmaskD
                        v3 = vt[:].rearrange("p (n d) -> p n d", d=129)
                    else:
                        dv = 64; KK = 67; mask, Rmask = self.maskF, self.RmaskF
                        v3 = vt[:, 0:NT * 65].rearrange("p (n d) -> p n d", d=65)
                    for qti in range(NT // 4):
                        def qtile_(qti):
                            Ob = [OP[(oidx[0] * 2) % 4], OP[(oidx[0] * 2 + 1) % 4]]; oidx[0] += 1
                            O3 = [o[0][:, 0:2 * (dv + 1)].rearrange("p (s d) -> p s d", d=dv + 1) for o in Ob]
                            nkb = 4 * qti + 4
                            steps = []
                            for kb in range(nkb):
                                j = kb - 4 * qti
                                qlo = max(j, 0) * 128
                                st, Rs = SP_[sidx[0] % 3]; sidx[0] += 1
                                pt, Rp = PT[pidx[0] % 4]; pidx[0] += 1
                                steps.append((kb, j, qlo, st, Rs, pt, Rp))

                            def rec_S(s):
                                kb, j, qlo, st, Rs, pt, Rp = s

                                def f(e):
                                    r = e.matmul(st[:, qlo:512], lhsT=kt[0:KK, kb * 128:(kb + 1) * 128],
                                                 rhs=qt[0:KK, qti * 512 + qlo:(qti + 1) * 512], start=True, stop=True, skip_group_check=True)
                                    if j >= 0:
                                        r = e.matmul(st[:, j * 128:(j + 1) * 128], lhsT=self.ident[:], rhs=mask[:], start=False, stop=True, skip_group_check=True)
                                    return r
                                P.op("pe", f, reads=[Rq, Rk, self.Rident, Rmask], writes=[Rs])

                            def rec_E(s):
                                kb, j, qlo, st, Rs, pt, Rp = s
                                bias = self.ncf[:, kb, h:h + 1] if kind == "f" else 0.0
                                P.op("act", lambda e: e.activation(out=pt[:, qlo:512], in_=st[:, qlo:512], func=AF.Exp, scale=0.125, bias=bias),
                                     reads=[Rs, self.Rncf], writes=[Rp])

                            def rec_PV(s):
                                kb, j, qlo, st, Rs, pt, Rp = s

                                def f(e):
                                    r = None
                                    for i4 in range(max(j, 0), 4):
                                        r = e.matmul(O3[i4 // 2][:, i4 % 2, :], lhsT=pt[:, i4 * 128:(i4 + 1) * 128], rhs=v3[:, kb, :],
                                                     start=(kb == 0 and i4 % 2 == 0), stop=(kb == 4 * qti + i4), skip_group_check=True)
                                    return r
                                P.op("pe", f, reads=[Rp, Rv], writes=[Ob[0][1], Ob[1][1]])
                            LA = 2
                            for s_i in range(min(LA, nkb)):
                                rec_S(steps[s_i])
                            for s_i in range(nkb):
                                rec_E(steps[s_i])
                                if s_i + LA < nkb:
                                    rec_S(steps[s_i + LA])
                                rec_PV(steps[s_i])
                            for half in range(2):
                                o3 = O3[half]; Ro = Ob[half][1]
                                rows0 = qti * 512 + half * 256
                                P.op("dve", lambda e, o3=o3: e.tensor_scalar(out=rden[:], in0=o3[:, :, dv], scalar1=1e-30, scalar2=None, op0=ALU.max),
                                     reads=[Ro], writes=[Rrden])
                                P.op("dve", lambda e: e.reciprocal(out=rden[:], in_=rden[:]), reads=[Rrden], writes=[Rrden])
                                rb = rden[:].unsqueeze(2).to_broadcast([128, 2, dv])
                                if kind == "d" and c == 0:
                                    P.op("dve", lambda e, o3=o3, rb=rb, half=half, qti=qti: e.tensor_tensor(out=u1[:, 4 * qti + 2 * half:4 * qti + 2 * half + 2, :], in0=o3[:, :, 0:dv], in1=rb, op=ALU.mult),
                                         reads=[Ro, Rrden], writes=[Ru1])
                                    continue
                                P.op("dve", lambda e, o3=o3, rb=rb: e.tensor_tensor(out=ot[:, :, 0:dv], in0=o3[:, :, 0:dv], in1=rb, op=ALU.mult),
                                     reads=[Ro, Rrden], writes=[Rot])
                                if kind == "d":
                                    P.op("dve", lambda e, half=half, qti=qti: e.scalar_tensor_tensor(out=ot[:], in0=ot[:], scalar=self.neglam[:, 0:1], in1=u1[:, 4 * qti + 2 * half:4 * qti + 2 * half + 2, :],
                                                                                           op0=ALU.mult, op1=ALU.add),
                                         reads=[Rot, Ru1, self.Rneglam], writes=[Rot])
                                P.op("dve", lambda e: e.tensor_tensor(out=osq[:, :, 0:dv], in0=ot[:, :, 0:dv], in1=ot[:, :, 0:dv], op=ALU.mult), reads=[Rot], writes=[Rosq])
                                P.op("dve", lambda e: e.tensor_reduce(out=ssq[:], in_=osq[:, :, 0:dv], axis=AX.X, op=ALU.add), reads=[Rosq], writes=[Rssq])
                                P.op("act", lambda e: e.activation(out=ssq[:], in_=ssq[:], func=AF.Ln, scale=1.0 / dv, bias=EPS), reads=[Rssq], writes=[Rssq])
                                P.op("act", lambda e: e.activation(out=ssq[:], in_=ssq[:], func=AF.Exp, scale=-0.5), reads=[Rssq], writes=[Rssq])
                                sb_ = ssq[:].unsqueeze(2).to_broadcast([128, 2, dv])
                                P.op("dve", lambda e, sb_=sb_: e.tensor_tensor(out=ot[:, :, 0:dv], in0=ot[:, :, 0:dv], in1=sb_, op=ALU.mult), reads=[Rot, Rssq], writes=[Rot])
                                gt = gsub if kind == "d" else gfox
                                Rg = Rgsub if kind == "d" else Rgfox
                                gb = gt[:].unsqueeze(1).to_broadcast([128, 2, dv])
                                ostt, Rost = ost[osti[0] % 2]; ob_ = osti[0] % 2; osti[0] += 1
                                P.op("dve", lambda e, gb=gb, ostt=ostt: e.tensor_tensor(out=ostt[:, :, 0:dv], in0=ot[:, :, 0:dv], in1=gb, op=ALU.mult),
                                     reads=[Rot, Rg], writes=[Rost])
                                col0 = h * 128 if kind == "d" else 512 + h * 64
                                dst = self.ocat[rows0:rows0 + 256, col0:col0 + dv].rearrange("(s p) d -> p s d", p=128)
                                P.op("sp", lambda e, dst=dst, ostt=ostt: e.dma_start(out=dst, in_=ostt[:, :, 0:dv]), reads=[Rost],
                                     writes=[self.Rocat[ob_]], dmakey=f"p2o{ob_}")
                        qtile_(qti)
                job_(jn, kind, h, c)
            P.flush(self.scr[:])

    def phase3a(self, l):
        nc, P, I = self.nc, self.P, self.ins
        xsrc = I["x"] if l == 0 else self.xres
        BIG = 1.0e4
        with contextlib.ExitStack() as es:
            wo = [self.sb(es, f"p3_wo{k}", [128, D], BF16) for k in range(8)]
            Rwo = [w[1] for w in wo]
            P.op("pool", lambda e: [e.dma_start(out=wo[k][0][:], in_=I["w_out"][l, k * 128:(k + 1) * 128, :]) for k in range(8)],
                 writes=Rwo, dmakey="p3wo", ndma=8)
            wr, Rwr = self.sb(es, "p3_wr", [128, 8, 36], F32)
            P.op("sp", lambda e: e.dma_start(out=wr[:, :, 0:4], in_=I["w_router_group"][l].rearrange("(k p) n -> p k n", p=128)), writes=[Rwr], dmakey="p3wr1")
            P.op("sp", lambda e: e.dma_start(out=wr[:, :, 4:36], in_=I["w_router_expert"][l].rearrange("(k p) n -> p k n", p=128)), writes=[Rwr], dmakey="p3wr2")
            rb, Rrb = self.sb(es, "p3_rb", [128, 36], F32)
            P.op("sp", lambda e: e.dma_start(out=rb[:, 0:4], in_=I["b_router_group"][l, :].partition_broadcast(128)), writes=[Rrb], dmakey="p3rb1")
            P.op("sp", lambda e: e.dma_start(out=rb[:, 4:36], in_=I["b_router_expert"][l, :].partition_broadcast(128)), writes=[Rrb], dmakey="p3rb2")
            xs = [self.sb(es, f"p3_xs{j}", [128, D], F32) for j in range(2)]
            oc = [self.sb(es, f"p3_oc{j}", [128, D], BF16) for j in range(2)]
            oT = [self.sb(es, f"p3_oT{j}", [128, 8, 128], BF16) for j in range(2)]
            tmp, Rtmp = self.sb(es, "p3_tmp", [128, D], F32)
            junk, Rjunk = self.sb(es, "p3_junk", [128, D], BF16)
            h2f = [self.sb(es, f"p3_h2f{j}", [128, D], F32) for j in range(2)]
            h2b = [self.sb(es, f"p3_h2b{j}", [128, D], BF16) for j in range(2)]
            h2T, Rh2T = self.sb(es, "p3_h2T", [128, 8, 128], F32)
            ss = [self.sb(es, f"p3_ss{j}", [128, 1], F32) for j in range(2)]
            lg, Rlg = self.sb(es, "p3_lg", [128, 36], F32)
            sm, Rsm = self.sb(es, "p3_sm", [128, 16], F32)
            ohg, Rohg = self.sb(es, "p3_ohg", [128, 4], F32)
            eg, Reg = self.sb(es, "p3_eg", [128, 4], F32)
            msk, Rmsk = self.sb(es, "p3_msk", [128, 32], F32)
            oh, Roh = self.sb(es, "p3_oh", [128, 2, 32], F32)
            ohs, Rohs = self.sb(es, "p3_ohs", [128, 32], F32)
            cum, Rcum = self.sb(es, "p3_cum", [128, 32], F32)
            pos, Rpos = self.sb(es, "p3_pos", [128, 32], F32)
            ovf, Rovf = self.sb(es, "p3_ovf", [128, 32], F32)
            dtmp, Rdtmp = self.sb(es, "p3_dtmp", [128, 2, 32], F32)
            dstf, Rdstf = self.sb(es, "p3_dstf", [128, 2], F32)
            tp, Rtp = self.ps(es, "p3_tp", [128, 8, 128], BF16)
            mx = [self.ps(es, f"p3_mx{j}", [128, 512], F32) for j in range(2)]
            tf = [self.ps(es, f"p3_tf{j}", [128, 4, 128], F32) for j in range(2)]
            plg, Rplg = self.ps(es, "p3_plg", [128, 36], F32)
            prk, Rprk = self.ps(es, "p3_prk", [128, 64], F32)
            P.op("pool", lambda e: e.memset(cum[:], 0.0), writes=[Rcum])

            def loadA(i):
                b = i % 2
                rows = slice(i * 128, (i + 1) * 128)
                P.op("sp", lambda e: e.dma_start(out=xs[b][0][:], in_=xsrc[rows, :]), reads=self.Rxres, writes=[xs[b][1]], dmakey=f"p3x{b}")
                P.op("sp", lambda e: e.dma_start(out=oc[b][0][:], in_=self.ocat[rows, :]), reads=self.Rocat, writes=[oc[b][1]], dmakey=f"p3o{b}")
            loadA(0)
            for i in range(NT):
                def tile3a_(i):
                    b = i % 2
                    rows = slice(i * 128, (i + 1) * 128)
                    if i + 1 < NT:
                        loadA(i + 1)
                    xt, Rx = xs[b]; oct_, Roc = oc[b]; oTt, RoT = oT[b]

                    def tro(e, oct_=oct_):
                        r = None
                        for k in range(8):
                            r = e.transpose(out=tp[:, k, :], in_=oct_[:, k * 128:(k + 1) * 128], identity=self.ident[:])
                        return r
                    P.op("pe", tro, reads=[Roc, self.Rident], writes=[Rtp])
                    P.op("act", lambda e, oTt=oTt: e.activation(out=oTt[:], in_=tp[:], func=AF.Copy), reads=[Rtp], writes=[RoT])
                    for n in range(2):
                        def mm(e, n=n, oTt=oTt):
                            r = None
                            for k in range(8):
                                r = e.matmul(mx[n][0][:], lhsT=oTt[:, k, :], rhs=wo[k][0][:, n * 512:(n + 1) * 512], start=(k == 0), stop=(k == 7))
                            return r
                        P.op("pe", mm, reads=[RoT] + Rwo, writes=[mx[n][1]])
                        P.op("dve", lambda e, n=n: e.tensor_tensor(out=tmp[:, n * 512:(n + 1) * 512], in0=mx[n][0][:], in1=self.modt[:, 2, n * 512:(n + 1) * 512], op=ALU.mult),
                             reads=[mx[n][1], self.Rmodt], writes=[Rtmp])
                    P.op("dve", lambda e, xt=xt: e.tensor_tensor(out=xt[:], in0=xt[:], in1=tmp[:], op=ALU.add), reads=[Rx, Rtmp], writes=[Rx])
                    P.op("sp", lambda e, xt=xt, rows=rows: e.dma_start(out=self.xres[rows, :], in_=xt[:]), reads=[Rx], writes=[self.Rxres[b]], dmakey=f"p3xo{b}")
                    sst, Rss = ss[b]
                    self.rstd_ops(xt[:], Rx, junk, Rjunk, sst, Rss, D)
                    P.op("dve", lambda e, xt=xt, sst=sst: e.scalar_tensor_tensor(out=tmp[:], in0=xt[:], scalar=sst[:, 0:1], in1=self.modt[:, 4, :], op0=ALU.mult, op1=ALU.mult),
                         reads=[Rx, Rss, self.Rmodt], writes=[Rtmp])
                    hf, Rhf = h2f[b]; hbt, Rhb = h2b[b]
                    P.op("dve", lambda e, hf=hf: e.tensor_tensor(out=hf[:], in0=tmp[:], in1=self.modt[:, 3, :], op=ALU.add), reads=[Rtmp, self.Rmodt], writes=[Rhf])
                    P.op("act", lambda e, hf=hf, hbt=hbt: e.activation(out=hbt[:], in_=hf[:], func=AF.Copy), reads=[Rhf], writes=[Rhb])
                    for half in range(2):
                        def trf(e, half=half, hf=hf):
                            r = None
                            for k in range(4):
                                kk = half * 4 + k
                                r = e.transpose(out=tf[half][0][:, k, :], in_=hf[:, kk * 128:(kk + 1) * 128], identity=self.identf[:])
                            return r
                        P.op("pe", trf, reads=[Rhf, self.Ridentf], writes=[tf[half][1]])
                        P.op("dve", lambda e, half=half: e.tensor_copy(out=h2T[:, half * 4:half * 4 + 4, :], in_=tf[half][0][:]), reads=[tf[half][1]], writes=[Rh2T])

                    def mml(e):
                        r = None
                        for k in range(8):
                            r = e.matmul(plg[:], lhsT=h2T[:, k, :], rhs=wr[:, k, :], start=(k == 0), stop=(k == 7))
                        return r
                    P.op("pe", mml, reads=[Rh2T, Rwr], writes=[Rplg])
                    P.op("dve", lambda e: e.tensor_tensor(out=lg[:], in0=plg[:], in1=rb[:], op=ALU.add), reads=[Rplg, Rrb], writes=[Rlg])
                    P.op("dve", lambda e: e.tensor_reduce(out=sm[:, 0:1], in_=lg[:, 0:4], axis=AX.X, op=ALU.max), reads=[Rlg], writes=[Rsm])
                    P.op("dve", lambda e: e.tensor_scalar(out=ohg[:], in0=lg[:, 0:4], scalar1=sm[:, 0:1], scalar2=None, op0=ALU.is_equal), reads=[Rlg, Rsm], writes=[Rohg])
                    P.op("dve", lambda e: e.tensor_scalar(out=sm[:, 1:2], in0=sm[:, 0:1], scalar1=-1.0, scalar2=None, op0=ALU.mult), reads=[Rsm], writes=[Rsm])
                    P.op("act", lambda e: e.activation(out=eg[:], in_=lg[:, 0:4], func=AF.Exp, bias=sm[:, 1:2], accum_out=sm[:, 2:3]), reads=[Rlg, Rsm], writes=[Reg, Rsm])
                    P.op("dve", lambda e: e.reciprocal(out=sm[:, 3:4], in_=sm[:, 2:3]), reads=[Rsm], writes=[Rsm])
                    P.op("dve", lambda e: e.tensor_scalar(out=ohg[:], in0=ohg[:], scalar1=-1.0, scalar2=BIG, op0=ALU.add, op1=ALU.mult), reads=[Rohg], writes=[Rohg])
                    P.op("dve", lambda e: e.tensor_tensor(out=msk[:].rearrange("p (g j) -> p g j", j=8), in0=lg[:, 4:36].rearrange("p (g j) -> p g j", j=8),
                                                          in1=ohg[:].unsqueeze(2).to_broadcast([128, 4, 8]), op=ALU.add), reads=[Rlg, Rohg], writes=[Rmsk])
                    P.op("dve", lambda e: e.tensor_reduce(out=sm[:, 4:5], in_=msk[:], axis=AX.X, op=ALU.max), reads=[Rmsk], writes=[Rsm])
                    P.op("dve", lambda e: e.tensor_scalar(out=oh[:, 0, :], in0=msk[:], scalar1=sm[:, 4:5], scalar2=None, op0=ALU.is_equal), reads=[Rmsk, Rsm], writes=[Roh])
                    P.op("dve", lambda e: e.scalar_tensor_tensor(out=msk[:], in0=oh[:, 0, :], scalar=-BIG, in1=msk[:], op0=ALU.mult, op1=ALU.add), reads=[Roh, Rmsk], writes=[Rmsk])
                    P.op("dve", lambda e: e.tensor_reduce(out=sm[:, 5:6], in_=msk[:], axis=AX.X, op=ALU.max), reads=[Rmsk], writes=[Rsm])
                    P.op("dve", lambda e: e.tensor_scalar(out=oh[:, 1, :], in0=msk[:], scalar1=sm[:, 5:6], scalar2=None, op0=ALU.is_equal), reads=[Rmsk, Rsm], writes=[Roh])
                    P.op("dve", lambda e: e.tensor_tensor(out=sm[:, 6:7], in0=sm[:, 5:6], in1=sm[:, 4:5], op=ALU.subtract), reads=[Rsm], writes=[Rsm])
                    P.op("act", lambda e: e.activation(out=sm[:, 7:8], in_=sm[:, 6:7], func=AF.Exp), reads=[Rsm], writes=[Rsm])
                    P.op("dve", lambda e: e.tensor_scalar(out=sm[:, 8:9], in0=sm[:, 7:8], scalar1=1.0, scalar2=None, op0=ALU.add), reads=[Rsm], writes=[Rsm])
                    P.op("dve", lambda e: e.reciprocal(out=sm[:, 9:10], in_=sm[:, 8:9]), reads=[Rsm], writes=[Rsm])
                    P.op("dve", lambda e: e.tensor_tensor(out=sm[:, 10:11], in0=sm[:, 9:10], in1=sm[:, 7:8], op=ALU.mult), reads=[Rsm], writes=[Rsm])
                    P.op("dve", lambda e, i=i: e.tensor_tensor(out=self.wts[:, i, 0:1], in0=sm[:, 9:10], in1=sm[:, 3:4], op=ALU.mult), reads=[Rsm], writes=[self.Rwts])
                    P.op("dve", lambda e, i=i: e.tensor_tensor(out=self.wts[:, i, 1:2], in0=sm[:, 10:11], in1=sm[:, 3:4], op=ALU.mult), reads=[Rsm], writes=[self.Rwts])
                    P.op("dve", lambda e: e.tensor_tensor(out=ohs[:], in0=oh[:, 0, :], in1=oh[:, 1, :], op=ALU.add), reads=[Roh], writes=[Rohs])

                    def mmr(e):
                        e.matmul(prk[:, 0:32], lhsT=self.triS[:], rhs=ohs[:], start=True, stop=True, skip_group_check=True)
                        return e.matmul(prk[:, 32:64], lhsT=self.ones[:], rhs=ohs[:], start=True, stop=True, skip_group_check=True)
                    P.op("pe", mmr, reads=[Rohs, self.RtriS, self.Rones], writes=[Rprk])
                    P.op("dve", lambda e: e.tensor_tensor(out=pos[:], in0=prk[:, 0:32], in1=cum[:], op=ALU.add), reads=[Rprk, Rcum], writes=[Rpos])
                    P.op("dve", lambda e: e.tensor_tensor(out=cum[:], in0=prk[:, 32:64], in1=cum[:], op=ALU.add), reads=[Rprk, Rcum], writes=[Rcum])
                    P.op("dve", lambda e: e.tensor_scalar(out=ovf[:], in0=pos[:], scalar1=float(CAP), scalar2=1.0e6, op0=ALU.is_ge, op1=ALU.mult), reads=[Rpos], writes=[Rovf])
                    P.op("dve", lambda e: e.tensor_tensor(out=pos[:], in0=pos[:], in1=self.sbase[:], op=ALU.add), reads=[Rpos, self.Rsbase], writes=[Rpos])
                    P.op("dve", lambda e: e.tensor_tensor(out=pos[:], in0=pos[:], in1=ovf[:], op=ALU.add), reads=[Rpos, Rovf], writes=[Rpos])
                    P.op("dve", lambda e: e.tensor_tensor(out=dtmp[:], in0=oh[:], in1=pos[:].unsqueeze(1).to_broadcast([128, 2, 32]), op=ALU.mult), reads=[Roh, Rpos], writes=[Rdtmp])
                    P.op("dve", lambda e: e.tensor_reduce(out=dstf[:], in_=dtmp[:], axis=AX.X, op=ALU.add), reads=[Rdtmp], writes=[Rdstf])
                    P.op("dve", lambda e, i=i: e.tensor_copy(out=self.dest[:, i, :], in_=dstf[:]), reads=[Rdstf], writes=[self.Rdest])
                    for k in range(2):
                        P.op("pool", lambda e, k=k, i=i, hbt=hbt: self.indirect(
                            e, out=self.xsd[:, :], out_offset=bass.IndirectOffsetOnAxis(ap=self.dest[:, i, k:k + 1], axis=0),
                            in_=hbt[:, :], in_offset=None),
                            reads=[Rhb, self.Rdest], writes=[self.Rxsd], dmakey=f"p3sc{k}")
                tile3a_(i)
            P.op("dve", lambda e: e.tensor_copy(out=self.cnti[:], in_=cum[:]), reads=[Rcum], writes=[self.Rcnti])
            P.flush(self.scr[:])

    def phase3b(self, l):
        nc, P, I = self.nc, self.P, self.ins
        NB = CAP // 128
        NSG = CAP // SEG
        BPS = SEG // 128
        with contextlib.ExitStack() as es:
            wg = [[self.sb(es, f"p4_wg{j}_{k}", [128, 512], BF16) for k in range(8)] for j in range(2)]
            wu = [[self.sb(es, f"p4_wu{j}_{k}", [128, 512], BF16) for k in range(8)] for j in range(2)]
            wd = [[self.sb(es, f"p4_wd{j}_{k}", [128, D], BF16) for k in range(4)] for j in range(2)]
            Xt = [self.sb(es, f"p4_X{j}", [128, NB, D], BF16) for j in range(2)]
            XTb = [self.sb(es, f"p4_XTb{j}", [128, 8, 128], BF16) for j in range(2)]
            ATb = [self.sb(es, f"p4_ATb{j}", [128, 4, 128], BF16) for j in range(2)]
            Atok = [self.sb(es, f"p4_At{j}", [128, 512], BF16) for j in range(2)]
            sg = [self.sb(es, f"p4_sg{j}", [128, 512], F32) for j in range(2)]
            Ys = [self.sb(es, f"p4_Y{j}", [128, D], BF16) for j in range(2)]
            tpX, RtpX = self.ps(es, "p4_tpX", [128, 8, 128], BF16)
            tpA, RtpA = self.ps(es, "p4_tpA", [128, 8, 128], BF16)
            pgt = [self.ps(es, f"p4_pg{j}", [128, 512], F32) for j in range(2)]
            put = [self.ps(es, f"p4_pu{j}", [128, 512], F32) for j in range(2)]
            py = [self.ps(es, f"p4_py{j}", [128, 512], F32) for j in range(2)]
            cnt = dict(b=0, y=0)
            RXseg = [[P.res(f"p4_Xseg{j}_{q}") for q in range(NSG)] for j in range(2)]

            THRU = 128 if COND_THR is None else COND_THR

            def cbegin(e_, blk):
                P.cond_begin(self.cnti[0:1, e_:e_ + 1], (l, e_), THRU * blk)

            def load(e_):
                j = e_ % 2
                P.op("pool", lambda e: [e.dma_start(out=wg[j][k][0][:], in_=I["w_expert_gate"][l, e_, k * 128:(k + 1) * 128, :]) for k in range(8)],
                     writes=[w[1] for w in wg[j]], dmakey=f"p4g{j}", ndma=8)
                P.op("pool", lambda e: [e.dma_start(out=wu[j][k][0][:], in_=I["w_expert_up"][l, e_, k * 128:(k + 1) * 128, :]) for k in range(8)],
                     writes=[w[1] for w in wu[j]], dmakey=f"p4u{j}", ndma=8)
                P.op("pool", lambda e: [e.dma_start(out=wd[j][k][0][:], in_=I["w_expert_down"][l, e_, k * 128:(k + 1) * 128, :]) for k in range(4)],
                     writes=[w[1] for w in wd[j]], dmakey=f"p4d{j}", ndma=4)
                for sg_ in range(NSG):
                    if sg_ > 0:
                        cbegin(e_, sg_ * BPS)
                    r0_ = e_ * CAP + sg_ * SEG
                    b0_ = sg_ * BPS
                    P.op("sp", lambda e, r0_=r0_, b0_=b0_: e.dma_start(out=Xt[j][0][:, b0_:b0_ + BPS, :],
                                                                     in_=self.xsd[r0_:r0_ + SEG, :].rearrange("(b p) d -> p b d", p=128)),
                         reads=[self.Rxsd], writes=[RXseg[j][sg_]], dmakey=f"p4x{j}_{sg_}")
                    if sg_ > 0:
                        P.cond_end()

            def s1_(e_, j, blk):
                xt = Xt[j][0]
                Rx = RXseg[j][blk // BPS]
                Rwg = [w[1] for w in wg[j]]; Rwu = [w[1] for w in wu[j]]
                b2 = cnt["b"] % 2; cnt["b"] += 1
                xtb, Rxtb = XTb[b2]; atok, Ratok = Atok[b2]; sgt, Rsg = sg[b2]
                (g_, Rg), (u_, Ru) = pgt[b2], put[b2]

                def trx(e):
                    r = None
                    for k in range(8):
                        r = e.transpose(out=tpX[:, k, :], in_=xt[:, blk, k * 128:(k + 1) * 128], identity=self.ident[:])
                    return r
                P.op("pe", trx, reads=[Rx, self.Rident], writes=[RtpX])
                P.op("dve", lambda e: e.tensor_copy(out=xtb[:], in_=tpX[:]), reads=[RtpX], writes=[Rxtb])

                def mmg(e):
                    r = None
                    for k in range(8):
                        r = e.matmul(g_[:], lhsT=xtb[:, k, :], rhs=wg[j][k][0][:], start=(k == 0), stop=(k == 7))
                    return r
                P.op("pe", mmg, reads=[Rxtb] + Rwg, writes=[Rg])

                def mmu(e):
                    r = None
                    for k in range(8):
                        r = e.matmul(u_[:], lhsT=xtb[:, k, :], rhs=wu[j][k][0][:], start=(k == 0), stop=(k == 7))
                    return r
                P.op("pe", mmu, reads=[Rxtb] + Rwu, writes=[Ru])
                P.op("act", lambda e: e.activation(out=sgt[:], in_=g_[:], func=AF.Silu), reads=[Rg], writes=[Rsg])
                P.op("dve", lambda e: e.tensor_tensor(out=atok[:], in0=sgt[:], in1=u_[:], op=ALU.mult), reads=[Rsg, Ru], writes=[Ratok])
                return b2

            def s2_(e_, j, blk, b2):
                Rwd = [w[1] for w in wd[j]]
                atb, Ratb = ATb[b2]; atok, Ratok = Atok[b2]

                def tra(e):
                    r = None
                    for m in range(4):
                        r = e.transpose(out=tpA[:, m, :], in_=atok[:, m * 128:(m + 1) * 128], identity=self.ident[:])
                    return r
                P.op("pe", tra, reads=[Ratok, self.Rident], writes=[RtpA])
                P.op("dve", lambda e: e.tensor_copy(out=atb[:], in_=tpA[:, 0:4, :]), reads=[RtpA], writes=[Ratb])
                yst, Rys = Ys[b2]
                for n in range(2):
                    y_, Ry = py[cnt["y"] % 2]; cnt["y"] += 1

                    def mmd(e, n=n, y_=y_):
                        r = None
                        for k in range(4):
                            r = e.matmul(y_[:], lhsT=atb[:, k, :], rhs=wd[j][k][0][:, n * 512:(n + 1) * 512], start=(k == 0), stop=(k == 3))
                        return r
                    P.op("pe", mmd, reads=[Ratb] + Rwd, writes=[Ry])
                    P.op("dve", lambda e, n=n, y_=y_: e.tensor_copy(out=yst[:, n * 512:(n + 1) * 512], in_=y_[:]), reads=[Ry], writes=[Rys])
                r0 = e_ * CAP + blk * 128
                P.op("sp", lambda e: e.dma_start(out=self.ysd[r0:r0 + 128, :], in_=yst[:]), reads=[Rys], writes=[self.Rysd], dmakey=f"p4y{b2}")

            def guarded(e_, blk, fn):
                if blk > 0:
                    cbegin(e_, blk)
                r = fn()
                if blk > 0:
                    P.cond_end()
                return r

            load(0)
            for e_ in range(NE):
                if e_ + 1 < NE:
                    load(e_ + 1)
                for blk in range(NB):
                    def both(e_=e_, blk=blk):
                        b2 = s1_(e_, e_ % 2, blk)
                        s2_(e_, e_ % 2, blk, b2)
                    guarded(e_, blk, both)
            P.flush(self.scr[:])

    def phase_final(self):
        nc, P, I = self.nc, self.P, self.ins
        with contextlib.ExitStack() as es:
            xs = [self.sb(es, f"pf_xs{j}", [128, D], F32) for j in range(2)]
            y0 = [self.sb(es, f"pf_y0{j}", [128, D], BF16) for j in range(2)]
            y1 = [self.sb(es, f"pf_y1{j}", [128, D], BF16) for j in range(2)]
            tmp, Rtmp = self.sb(es, "pf_tmp", [128, D], F32)
            junk, Rjunk = self.sb(es, "pf_junk", [128, D], BF16)
            ss = [self.sb(es, f"pf_ss{j}", [128, 1], F32) for j in range(2)]
            gf, Rgf = self.sb(es, "pf_gf", [128, D], F32)
            P.op("sp", lambda e: e.dma_start(out=gf[:], in_=I["g_final"].partition_broadcast(128)), writes=[Rgf], dmakey="pfg")
            for i in range(NT):
                def tilef_(i):
                    b = i % 2
                    rows = slice(i * 128, (i + 1) * 128)
                    xt, Rx = xs[b]
                    self.combine(i, xt, Rx, y0[b], y1[b], tmp, Rtmp, self.modt[:, 5, :], self.Rmodt, f"pfx{b}", f"pfy{b}")
                    sst, Rss = ss[b]
                    self.rstd_ops(xt[:], Rx, junk, Rjunk, sst, Rss, D)
                    P.op("dve", lambda e, xt=xt, sst=sst: e.scalar_tensor_tensor(out=xt[:], in0=xt[:], scalar=sst[:, 0:1], in1=gf[:], op0=ALU.mult, op1=ALU.mult),
                         reads=[Rx, Rss, Rgf], writes=[Rx])
                    P.op("sp", lambda e, xt=xt, rows=rows: e.dma_start(out=self.y[rows, :], in_=xt[:]), reads=[Rx], writes=[self.Ry], dmakey=f"pfo{b}")
                tilef_(i)
            P.flush(self.scr[:])


def _layout_inputs(inputs, b):
    m = {}
    m["x"] = np.ascontiguousarray(inputs["x"][b])
    m["cT"] = np.ascontiguousarray(inputs["c"][b].reshape(8, 128).T)
    m["pos"] = np.ascontiguousarray(inputs["positions"][b].reshape(NT, 128).T).astype(np.int32)
    for k in ("w_ada", "b_ada", "g_mix", "w_in", "b_forget", "lambda_q1", "lambda_k1", "lambda_q2", "lambda_k2",
              "g_subln", "g_fox_out", "w_out", "g_ffn", "w_router_group", "b_router_group", "w_router_expert",
              "b_router_expert", "w_expert_gate", "w_expert_up", "w_expert_down", "g_final"):
        m[k] = np.ascontiguousarray(inputs[k])
    return m


def kernel(**inputs):
    kb = K()
    in_maps = [_layout_inputs(inputs, b) for b in range(4)]
    res = run_bass_kernel_spmd(kb.nc, in_maps, core_ids=list(range(4)))
    return np.stack([np.asarray(r["y"]) for r in res.results], axis=0).astype(np.float32)
```
